# Optimizing a Trainium2 kernel written in Bass

```python
import math
import jax
import jax.numpy as jnp
from jax import lax
import numpy as np

D_MODEL = 1024
BATCH = 1
SEQ = 16384
DEPTH = 4

N_EVEN = (DEPTH + 1) // 2
N_ODD = DEPTH // 2
EPS = 1e-6
ROPE_THETA = 10000.0
Q_BLOCK = 128

GDN_HEADS = 4
GDN_DK = 128
GDN_DV = 128
GDN_CONV = 4
GDN_CHUNK = 64
GDN_QK = GDN_HEADS * GDN_DK
GDN_V = GDN_HEADS * GDN_DV
GDN_CONV_CH = 2 * GDN_QK + GDN_V
MOBA_HEADS = 4
MOBA_DH = 128
MOBA_BLOCK = 256
MOBA_TOPK = 3
MOBA_W = MOBA_HEADS * MOBA_DH
HY_SIZES = (GDN_CONV_CH, GDN_V, GDN_HEADS, GDN_HEADS, MOBA_W, MOBA_W, MOBA_W)
HY_IN = sum(HY_SIZES)
HY_SPLITS = tuple(int(i) for i in np.cumsum(HY_SIZES)[:-1])
HY_OUT = GDN_V + MOBA_W
MLA_HEADS = 8
MLA_Q_RANK = 512
MLA_KV_RANK = 256
MLA_NOPE = 128
MLA_ROPE = 64
MLA_V = 128
MLA_IN = MLA_Q_RANK + MLA_KV_RANK + MLA_ROPE
PEER_HEADS = 8
PEER_NKEYS = 128
PEER_EXPERTS = PEER_NKEYS * PEER_NKEYS
PEER_DKEY = 256
PEER_TOPK = 16
PEER_TOK_CHUNK = 128

kernel_name = 'hybrid_gdn_moba_mla_peer'


def rmsnorm(x, w):
    xf = x.astype(jnp.float32)
    y = xf * lax.rsqrt(jnp.mean(xf * xf, axis=-1, keepdims=True) + EPS)
    return (y * w.astype(jnp.float32)).astype(x.dtype)


def l2norm(x):
    xf = x.astype(jnp.float32)
    return xf * lax.rsqrt(jnp.sum(xf * xf, axis=-1, keepdims=True) + EPS)


def rope_tables(positions, dim):
    inv_freq = ROPE_THETA ** (-jnp.arange(0, dim, 2, dtype=jnp.float32) / dim)
    ang = positions.astype(jnp.float32)[..., None] * inv_freq
    return jnp.cos(ang), jnp.sin(ang)


def apply_rope(x, cos, sin):
    x1, x2 = jnp.split(x.astype(jnp.float32), 2, axis=-1)
    c = cos[:, :, None, :]
    s = sin[:, :, None, :]
    return jnp.concatenate([x1 * c - x2 * s, x2 * c + x1 * s], axis=-1)


def causal_conv_silu(x, w):
    k_w = w.shape[0]
    s = x.shape[1]
    xp = jnp.pad(x, ((0, 0), (k_w - 1, 0), (0, 0)))
    y = xp[:, 0:s] * w[0]
    for j in range(1, k_w):
        y = y + xp[:, j:j + s] * w[j]
    return jax.nn.silu(y)


def unit_lower_inverse(l_strict):
    n = l_strict.shape[-1]
    eye = jnp.eye(n, dtype=l_strict.dtype)
    neg = -l_strict
    inv = eye + neg
    power = neg
    p = 2
    while p < n:
        power = power @ power
        inv = inv @ (eye + power)
        p *= 2
    return inv


def gated_delta_rule(q, k, v, g, beta):
    bsz, s, nh, dk = q.shape
    dv = v.shape[-1]
    cs = GDN_CHUNK
    nc = s // cs

    def to_chunks(t):
        t = t.reshape((bsz, nc, cs, nh) + t.shape[3:])
        return jnp.moveaxis(t, 3, 1)

    q = to_chunks(q * dk ** -0.5)
    k = to_chunks(k)
    v = to_chunks(v)
    beta = to_chunks(beta)
    g = jnp.cumsum(to_chunks(g), axis=-1)
    causal = jnp.tril(jnp.ones((cs, cs), dtype=bool))
    strict = jnp.tril(jnp.ones((cs, cs), dtype=bool), -1)
    decay = jnp.exp(jnp.where(causal, g[..., :, None] - g[..., None, :], -jnp.inf))
    k_beta = k * beta[..., None]
    l_strict = jnp.where(strict, jnp.einsum('bhncd,bhnmd->bhncm', k_beta, k) * decay, 0.0)
    t_inv = unit_lower_inverse(l_strict)
    u = t_inv @ (v * beta[..., None])
    w = t_inv @ (k_beta * jnp.exp(g)[..., None])
    qk = jnp.einsum('bhncd,bhnmd->bhncm', q, k) * decay
    q_dec = q * jnp.exp(g)[..., None]
    g_last = g[..., -1]
    k_dec = k * jnp.exp(g_last[..., None] - g)[..., None]

    def step(state, inp):
        qd, kd, uc, wc, qkc, gl = inp
        v_new = uc - jnp.einsum('bhck,bhkv->bhcv', wc, state)
        out = jnp.einsum('bhck,bhkv->bhcv', qd, state) + jnp.einsum('bhcm,bhmv->bhcv', qkc, v_new)
        state = state * jnp.exp(gl)[..., None, None] + jnp.einsum('bhck,bhcv->bhkv', kd, v_new)
        return state, out

    xs = tuple(jnp.moveaxis(t, 2, 0) for t in (q_dec, k_dec, u, w, qk, g_last))
    state0 = jnp.zeros((bsz, nh, dk, dv), jnp.float32)
    _, o = lax.scan(step, state0, xs)
    return jnp.transpose(o, (1, 0, 3, 2, 4)).reshape(bsz, s, nh, dv)


def moba_attention(q, k, v):
    bsz, s, nh, d = q.shape
    nb = -(-s // MOBA_BLOCK)
    pad = nb * MOBA_BLOCK - s
    topk = min(MOBA_TOPK, nb)
    qh = jnp.moveaxis(q, 2, 1) * d ** -0.5

    def to_blocks(t):
        t = jnp.pad(jnp.moveaxis(t, 2, 1), ((0, 0), (0, 0), (0, pad), (0, 0)))
        return t.reshape(bsz, nh, nb, MOBA_BLOCK, d)

    k_blk = to_blocks(k)
    v_blk = to_blocks(v)
    k_mean = jnp.mean(k_blk, axis=3)
    b_idx = jnp.arange(bsz)[:, None, None, None]
    h_idx = jnp.arange(nh)[None, :, None, None]
    blk_ids = jnp.arange(nb)

    def one_block(ci):
        start = ci * Q_BLOCK
        own = start // MOBA_BLOCK
        qc = lax.dynamic_slice_in_dim(qh, start, Q_BLOCK, axis=2)
        qpos = start + jnp.arange(Q_BLOCK)
        gate = jnp.einsum('bhqd,bhnd->bhqn', qc, k_mean)
        gate = jnp.where(blk_ids < own, gate, -jnp.inf)
        _, sel = lax.top_k(gate, topk)
        valid = sel < own
        k_sel = k_blk[b_idx, h_idx, sel]
        v_sel = v_blk[b_idx, h_idx, sel]
        s_sel = jnp.einsum('bhqd,bhqtkd->bhqtk', qc, k_sel)
        s_sel = jnp.where(valid[..., None], s_sel, -jnp.inf).reshape(bsz, nh, Q_BLOCK, topk * MOBA_BLOCK)
        k_own = lax.dynamic_index_in_dim(k_blk, own, axis=2, keepdims=False)
        v_own = lax.dynamic_index_in_dim(v_blk, own, axis=2, keepdims=False)
        kpos = own * MOBA_BLOCK + jnp.arange(MOBA_BLOCK)
        s_own = jnp.einsum('bhqd,bhkd->bhqk', qc, k_own)
        s_own = jnp.where(kpos[None, :] <= qpos[:, None], s_own, -jnp.inf)
        p = jax.nn.softmax(jnp.concatenate([s_sel, s_own], axis=-1), axis=-1)
        p_sel = p[..., :topk * MOBA_BLOCK].reshape(bsz, nh, Q_BLOCK, topk, MOBA_BLOCK)
        p_own = p[..., topk * MOBA_BLOCK:]
        return (jnp.einsum('bhqtk,bhqtkd->bhqd', p_sel, v_sel)
                + jnp.einsum('bhqk,bhkd->bhqd', p_own, v_own))

    o = lax.map(one_block, jnp.arange(s // Q_BLOCK))
    return jnp.transpose(o, (1, 0, 3, 2, 4)).reshape(bsz, s, nh, d)


def hybrid_mixer(h, cos, sin, w_in, conv_w, a_log, dt_bias, o_norm, w_out):
    bsz, s, _ = h.shape
    f32 = jnp.float32
    qkv_a, z_a, b_a, a_a, q_b, k_b, v_b = jnp.split(h @ w_in, HY_SPLITS, axis=-1)
    qkv_a = causal_conv_silu(qkv_a, conv_w)
    q_a, k_a, v_a = jnp.split(qkv_a, [GDN_QK, 2 * GDN_QK], axis=-1)
    q_a = l2norm(q_a.reshape(bsz, s, GDN_HEADS, GDN_DK))
    k_a = l2norm(k_a.reshape(bsz, s, GDN_HEADS, GDN_DK))
    v_a = v_a.reshape(bsz, s, GDN_HEADS, GDN_DV).astype(f32)
    beta = jax.nn.sigmoid(b_a.astype(f32))
    g = -jnp.exp(a_log.astype(f32)) * jax.nn.softplus(a_a.astype(f32) + dt_bias.astype(f32))
    o_a = gated_delta_rule(q_a, k_a, v_a, g, beta)
    o_a = rmsnorm(o_a, o_norm) * jax.nn.silu(z_a.reshape(bsz, s, GDN_HEADS, GDN_DV).astype(f32))
    q_b = apply_rope(q_b.reshape(bsz, s, MOBA_HEADS, MOBA_DH), cos, sin)
    k_b = apply_rope(k_b.reshape(bsz, s, MOBA_HEADS, MOBA_DH), cos, sin)
    v_b = v_b.reshape(bsz, s, MOBA_HEADS, MOBA_DH).astype(f32)
    o_b = moba_attention(q_b, k_b, v_b)
    o = jnp.concatenate([o_a.reshape(bsz, s, GDN_V), o_b.reshape(bsz, s, MOBA_W)], axis=-1)
    return o.astype(h.dtype) @ w_out


def mla_mixer(h, cos, sin, w_in, q_norm, kv_norm, w_uq, w_ukv, w_out):
    bsz, s, _ = h.shape
    f32 = jnp.float32
    cq, ckv, k_rope = jnp.split(h @ w_in, [MLA_Q_RANK, MLA_Q_RANK + MLA_KV_RANK], axis=-1)
    q = (rmsnorm(cq, q_norm) @ w_uq).reshape(bsz, s, MLA_HEADS, MLA_NOPE + MLA_ROPE)
    kv = (rmsnorm(ckv, kv_norm) @ w_ukv).reshape(bsz, s, MLA_HEADS, MLA_NOPE + MLA_V)
    q_nope, q_rope = jnp.split(q.astype(f32), [MLA_NOPE], axis=-1)
    k_nope, val = jnp.split(kv.astype(f32), [MLA_NOPE], axis=-1)
    q_rope = apply_rope(q_rope, cos, sin)
    k_rope = apply_rope(k_rope[:, :, None, :], cos, sin)[:, :, 0, :]
    scale = (MLA_NOPE + MLA_ROPE) ** -0.5
    qn = jnp.moveaxis(q_nope, 2, 1) * scale
    qr = jnp.moveaxis(q_rope, 2, 1) * scale
    kn = jnp.moveaxis(k_nope, 2, 1)
    vh = jnp.moveaxis(val, 2, 1)
    kpos = jnp.arange(s)

    def one_block(bi):
        start = bi * Q_BLOCK
        qn_b = lax.dynamic_slice_in_dim(qn, start, Q_BLOCK, axis=2)
        qr_b = lax.dynamic_slice_in_dim(qr, start, Q_BLOCK, axis=2)
        scores = (jnp.einsum('bhqd,bhkd->bhqk', qn_b, kn)
                  + jnp.einsum('bhqr,bkr->bhqk', qr_b, k_rope))
        qpos = start + jnp.arange(Q_BLOCK)
        scores = jnp.where(kpos[None, :] <= qpos[:, None], scores, -jnp.inf)
        return jnp.einsum('bhqk,bhkd->bhqd', jax.nn.softmax(scores, axis=-1), vh)

    o = lax.map(one_block, jnp.arange(s // Q_BLOCK))
    o = jnp.transpose(o, (1, 0, 3, 2, 4)).reshape(bsz, s, MLA_HEADS * MLA_V)
    return o.astype(h.dtype) @ w_out


def peer_ffn(h, w_q, sub_keys, u, v):
    bsz, s, d = h.shape
    f32 = jnp.float32
    q = (h @ w_q).astype(f32).reshape(bsz, s, PEER_HEADS, 2, PEER_DKEY // 2)
    scores = jnp.einsum('bshpd,hpnd->bshpn', q, sub_keys.astype(f32))
    s1, i1 = lax.top_k(scores[..., 0, :], PEER_TOPK)
    s2, i2 = lax.top_k(scores[..., 1, :], PEER_TOPK)
    n_cand = PEER_TOPK * PEER_TOPK
    cand = (s1[..., :, None] + s2[..., None, :]).reshape(bsz, s, PEER_HEADS, n_cand)
    cand_idx = (i1[..., :, None] * PEER_NKEYS + i2[..., None, :]).reshape(bsz, s, PEER_HEADS, n_cand)
    top, pos = lax.top_k(cand, PEER_TOPK)
    expert = jnp.take_along_axis(cand_idx, pos, axis=-1)
    gates = jax.nn.softmax(top, axis=-1)
    n_tok = bsz * s
    n_sel = PEER_HEADS * PEER_TOPK
    n_chunk = n_tok // PEER_TOK_CHUNK
    h_c = h.reshape(n_chunk, PEER_TOK_CHUNK, d)
    e_c = expert.reshape(n_chunk, PEER_TOK_CHUNK, n_sel)
    g_c = gates.reshape(n_chunk, PEER_TOK_CHUNK, n_sel)

    def one_chunk(args):
        hc, ec, gc = args
        act = jax.nn.gelu(jnp.einsum('td,ted->te', hc, u[ec]).astype(f32), approximate=False)
        return jnp.einsum('te,ted->td', (gc * act).astype(v.dtype), v[ec])

    out = lax.map(one_chunk, (h_c, e_c, g_c))
    return out.reshape(bsz, s, d).astype(h.dtype)


def setup_inputs(seed: int = 0) -> dict:
    key = jax.random.key(seed)
    ks = jax.random.split(key, 24)
    f32 = jnp.float32
    D = D_MODEL

    def nrm(k, shape, scale):
        return jax.random.normal(k, shape, f32) * scale

    def gain(k, shape):
        return 1.0 + 0.02 * jax.random.normal(k, shape, f32)

    dt = jnp.exp(jax.random.uniform(ks[9], (N_EVEN, GDN_HEADS), f32, math.log(1e-3), math.log(1e-1)))
    return {
        'x': nrm(ks[0], (BATCH, SEQ, D), 1.0),
        'c': nrm(ks[1], (BATCH, D), 1.0),
        'positions': jnp.broadcast_to(jnp.arange(SEQ, dtype=jnp.int32), (BATCH, SEQ)),
        'mod_w': nrm(ks[2], (DEPTH, D, 6 * D), 0.5 * D ** -0.5),
        'mod_b': nrm(ks[3], (DEPTH, 6 * D), 0.02),
        'norm_mix': gain(ks[4], (DEPTH, D)),
        'norm_ffn': gain(ks[5], (DEPTH, D)),
        'hy_w_in': nrm(ks[6], (N_EVEN, D, HY_IN), D ** -0.5),
        'gdn_conv': nrm(ks[7], (N_EVEN, GDN_CONV, GDN_CONV_CH), GDN_CONV ** -0.5),
        'gdn_a_log': jnp.log(jax.random.uniform(ks[8], (N_EVEN, GDN_HEADS), f32, 1.0, 16.0)),
        'gdn_dt_bias': dt + jnp.log(-jnp.expm1(-dt)),
        'gdn_o_norm': gain(ks[10], (N_EVEN, GDN_DV)),
        'hy_w_out': nrm(ks[11], (N_EVEN, HY_OUT, D), HY_OUT ** -0.5),
        'mla_w_in': nrm(ks[12], (N_ODD, D, MLA_IN), D ** -0.5),
        'mla_q_norm': gain(ks[13], (N_ODD, MLA_Q_RANK)),
        'mla_kv_norm': gain(ks[14], (N_ODD, MLA_KV_RANK)),
        'mla_w_uq': nrm(ks[15], (N_ODD, MLA_Q_RANK, MLA_HEADS * (MLA_NOPE + MLA_ROPE)), MLA_Q_RANK ** -0.5),
        'mla_w_ukv': nrm(ks[16], (N_ODD, MLA_KV_RANK, MLA_HEADS * (MLA_NOPE + MLA_V)), MLA_KV_RANK ** -0.5),
        'mla_w_out': nrm(ks[17], (N_ODD, MLA_HEADS * MLA_V, D), (MLA_HEADS * MLA_V) ** -0.5),
        'peer_w_q': nrm(ks[18], (DEPTH, D, PEER_HEADS * PEER_DKEY), D ** -0.5),
        'peer_sub_keys': nrm(ks[19], (DEPTH, PEER_HEADS, 2, PEER_NKEYS, PEER_DKEY // 2), (PEER_DKEY // 2) ** -0.5),
        'peer_u': nrm(ks[20], (DEPTH, PEER_EXPERTS, D), D ** -0.5),
        'peer_v': nrm(ks[21], (DEPTH, PEER_EXPERTS, D), PEER_HEADS ** -0.5),
        'final_norm': gain(ks[22], (D,)),
    }


def reference(x, c, positions, mod_w, mod_b, norm_mix, norm_ffn, hy_w_in, gdn_conv, gdn_a_log,
              gdn_dt_bias, gdn_o_norm, hy_w_out, mla_w_in, mla_q_norm, mla_kv_norm, mla_w_uq,
              mla_w_ukv, mla_w_out, peer_w_q, peer_sub_keys, peer_u, peer_v, final_norm):
    cond = jax.nn.silu(c)
    cos_b, sin_b = rope_tables(positions, MOBA_DH)
    cos_c, sin_c = rope_tables(positions, MLA_ROPE)
    for layer in range(DEPTH):
        mod = cond @ mod_w[layer] + mod_b[layer]
        sh1, sc1, g1, sh2, sc2, g2 = jnp.split(mod[:, None, :], 6, axis=-1)
        h = rmsnorm(x, norm_mix[layer]) * (1.0 + sc1) + sh1
        i = layer // 2
        if layer % 2 == 0:
            y = hybrid_mixer(h, cos_b, sin_b, hy_w_in[i], gdn_conv[i], gdn_a_log[i], gdn_dt_bias[i],
                             gdn_o_norm[i], hy_w_out[i])
        else:
            y = mla_mixer(h, cos_c, sin_c, mla_w_in[i], mla_q_norm[i], mla_kv_norm[i], mla_w_uq[i],
                          mla_w_ukv[i], mla_w_out[i])
        x = x + g1 * y
        h = rmsnorm(x, norm_ffn[layer]) * (1.0 + sc2) + sh2
        x = x + g2 * peer_ffn(h, peer_w_q[layer], peer_sub_keys[layer], peer_u[layer], peer_v[layer])
    return rmsnorm(x, final_norm)
```

```python
import math
import numpy as np
from contextlib import ExitStack
import concourse.bass as bass
import concourse.mybir as mybir
from concourse.bass_utils import run_bass_kernel_spmd


F32 = mybir.dt.float32
BF16 = mybir.dt.bfloat16
I32 = mybir.dt.int32
AF = mybir.ActivationFunctionType
OP = mybir.AluOpType
AX = mybir.AxisListType


class Buf:
    __slots__ = ("ap", "w", "r", "dsem", "dcnt", "name", "is_dram", "is_psum")

    def __init__(self, ap, name=""):
        self.ap = ap
        self.w = None
        self.r = {}
        self.dsem = None
        self.dcnt = 0
        self.name = name
        self.is_dram = False
        self.is_psum = False

    def __getitem__(self, idx):
        return View(self, self.ap[idx])


class View:
    __slots__ = ("buf", "ap")

    def __init__(self, buf, ap):
        self.buf = buf
        self.ap = ap

    def __getitem__(self, idx):
        return View(self.buf, self.ap[idx])


def _b(x):
    return x.buf if isinstance(x, View) else x


def _ap(x):
    if isinstance(x, (Buf, View)):
        return x.ap
    return x


class Prog:
    ENG = ("pe", "act", "dve", "pool", "sp")

    def __init__(self, nc, stack, same_engine_sync=True):
        self.nc = nc
        self.stack = stack
        self.e = {"pe": nc.tensor, "act": nc.scalar, "dve": nc.vector, "pool": nc.gpsimd, "sp": nc.sync}
        self.sem = {k: stack.enter_context(nc.semaphore("s_" + k)) for k in self.ENG}
        self.cnt = {k: 0 for k in self.ENG}
        self.seen = {k: {} for k in self.ENG}
        self.same = same_engine_sync
        self.ninst = 0
        self.nwait = 0
        self._dsems = []
        self._dbufs = []
        self._free_dsems = []
        self._scopes = []
        self.top = stack

    def sb(self, shape, dt=F32, name=None):
        t = self.stack.enter_context(self.nc.sbuf_tensor(name or f"sb{self.ninst}_{len(self._dsems)}_{np.random.randint(1<<30)}", list(shape), dt))
        return Buf(t.ap() if hasattr(t, "ap") and callable(getattr(t, "ap")) else t[:], name or "")

    def ps(self, shape, dt=F32, name=None):
        t = self.stack.enter_context(self.nc.psum_tensor(name or f"ps{np.random.randint(1<<30)}", list(shape), dt))
        b = Buf(t.ap() if hasattr(t, "ap") and callable(getattr(t, "ap")) else t[:], name or "")
        b.is_psum = True
        return b

    def dram(self, name, shape, dt=F32, kind="Internal"):
        t = self.nc.dram_tensor(name, list(shape), dt, kind=kind)
        b = Buf(t.ap(), name)
        b.is_dram = True
        return b

    def _need(self, eng, dep):
        if dep is None:
            return
        kind, key, count = dep
        if kind == "eng":
            if key == eng and (eng == "pe" or not self.same):
                return
            sem = self.sem[key]
            skey = key
        else:
            sem = key
            skey = ("d", id(key))
        if self.seen[eng].get(skey, 0) >= count:
            return
        self.e[eng].wait_ge(sem, count)
        self.seen[eng][skey] = count
        self.nwait += 1

    def _deps(self, eng, reads, writes):
        for x in reads:
            b = _b(x)
            self._need(eng, b.w)
            if b.is_psum:
                for k, d in b.r.items():
                    if k != eng:
                        self._need(eng, d)
        for x in writes:
            b = _b(x)
            self._need(eng, b.w)
            for d in b.r.values():
                self._need(eng, d)

    def op(self, eng, fn, reads=(), writes=()):
        self._deps(eng, reads, writes)
        inst = fn()
        self.cnt[eng] += 1
        c = self.cnt[eng]
        inst.then_inc(self.sem[eng], 1)
        tag = ("eng", eng, c)
        for x in reads:
            _b(x).r[eng] = tag
        for x in writes:
            b = _b(x)
            b.w = tag
            b.r = {}
        self.ninst += 1
        return inst

    def dma(self, out, in_, q="sp", **kw):
        ob, ib = _b(out), _b(in_)
        self._deps(q, [ib], [ob])
        owner = ib if (ob.is_dram and not ib.is_dram) else ob
        if owner.dsem is None:
            if False:
                pass
            else:
                owner.dsem = self.top.enter_context(self.nc.semaphore(f"d{len(self._dsems)}"))
                owner.dcnt = 0
                self._dsems.append(owner.dsem)
            self._dbufs.append(owner)
            if self._scopes:
                self._scopes[-1].append(owner)
        inst = self.e[q].dma_start(out=_ap(out), in_=_ap(in_), **kw)
        owner.dcnt += 16
        inst.then_inc(owner.dsem, 16)
        tag = ("dma", owner.dsem, owner.dcnt)
        ob.w = tag
        ob.r = {}
        ib.r[("dma", id(owner.dsem))] = tag
        self.ninst += 1
        return inst

    def barrier(self):
        for e in self.ENG:
            for k in self.ENG:
                if k != e and self.cnt[k] > 0:
                    self._need(e, ("eng", k, self.cnt[k]))
        for b in self._dbufs:
            for e in self.ENG:
                if b.dcnt > 0:
                    self._need(e, ("dma", b.dsem, b.dcnt))

    def scope(self):
        return _Scope(self)

    def finish(self, bufs, eng="sp"):
        for b in bufs:
            self._need(eng, b.w)


class _Scope:
    def __init__(self, P):
        self.P = P

    def __enter__(self):
        self.es = ExitStack()
        self.es.__enter__()
        self.prev = self.P.stack
        self.P.stack = self.es
        self.P._scopes.append([])
        return self

    def __exit__(self, *a):
        P = self.P
        P.barrier()
        owners = P._scopes.pop()
        for b in owners:
            P._free_dsems.append((b.dsem, b.dcnt))
            P._dbufs.remove(b)
            b.dsem = None
        P.stack = self.prev
        return self.es.__exit__(*a)


EPS = 1e-6


def build_phase_a(T, O):
    nc = bass.Bass("TRN2", target_bir_lowering=False)
    NT = T // 128
    with ExitStack() as st:
        P = Prog(nc, st)
        x = P.dram("x", [T, 1024], F32, kind="ExternalInput")
        cT = P.dram("cT", [128, 8], F32, kind="ExternalInput")
        modw = P.dram("modw", [1024, 2048], F32, kind="ExternalInput")
        modb = P.dram("modb", [1, 2048], F32, kind="ExternalInput")
        nrm = P.dram("nrm", [1, 1024], F32, kind="ExternalInput")
        W = P.dram("W", [1024, O], F32, kind="ExternalInput")
        ident_d = P.dram("ident", [128, 128], F32, kind="ExternalInput")
        Y = P.dram("Y", [T, O], F32, kind="ExternalOutput")

        ident = P.sb([128, 128], F32, "ident_sb")
        ones_row = P.sb([1, 128], F32, "ones_row")
        A1b = P.sb([128, 1024], F32, "A1b")
        B1b = P.sb([128, 1024], F32, "B1b")
        P1 = P.ps([128, 1024], F32, "P1")
        P2 = P.ps([128, 1024], F32, "P2")
        P.dma(ident, ident_d)
        P.op("dve", lambda: nc.vector.memset(ones_row.ap, 1.0), [], [ones_row])
        with P.scope():
            cond = P.sb([128, 8], F32, "cond")
            modrow = P.sb([1, 2048], F32, "modrow")
            mb = P.sb([1, 2048], F32, "mb")
            nr = P.sb([1, 1024], F32, "nr")
            a1row = P.sb([1, 1024], F32, "a1row")
            mwc = [P.sb([128, 8, 512], F32, f"mwc{i}") for i in range(2)]
            P.dma(cond, cT)
            P.dma(mb, modb, q="act")
            P.dma(nr, nrm, q="act")
            P.op("act", lambda: nc.scalar.activation(out=cond.ap, in_=cond.ap, func=AF.Silu), [cond], [cond])
            mw_v = modw.ap.rearrange("(kc p) o -> p kc o", p=128)
            for ch in range(4):
                buf = mwc[ch % 2]
                P.dma(buf, View(modw, mw_v[:, :, ch * 512:(ch + 1) * 512]), q="sp" if ch % 2 == 0 else "act")
                for kc in range(8):
                    P.op("pe", lambda: nc.tensor.matmul(P1.ap[0:1, 0:512], lhsT=cond.ap[:, kc:kc + 1], rhs=buf.ap[:, kc, :], start=(kc == 0), stop=(kc == 7)), [cond, buf], [P1])
                P.op("dve", lambda: nc.vector.tensor_tensor(out=modrow.ap[0:1, ch * 512:(ch + 1) * 512], in0=P1.ap[0:1, 0:512], in1=mb.ap[0:1, ch * 512:(ch + 1) * 512], op=OP.add), [P1, mb], [modrow])
            P.op("dve", lambda: nc.vector.scalar_tensor_tensor(out=a1row.ap, in0=modrow.ap[0:1, 1024:2048], scalar=1.0, in1=nr.ap, op0=OP.add, op1=OP.mult), [modrow, nr], [a1row])
            b1row = View(modrow, modrow.ap[0:1, 0:1024])
            for (row, dst) in ((a1row, A1b), (b1row, B1b)):
                for c0 in range(0, 1024, 512):
                    rap = _ap(row)[0:1, c0:c0 + 512]
                    P.op("pe", lambda: nc.tensor.matmul(P1.ap[:, 0:512], lhsT=ones_row.ap[0:1, 0:128], rhs=rap, start=True, stop=True), [ones_row, row], [P1])
                    P.op("act", lambda: nc.scalar.copy(out=dst.ap[:, c0:c0 + 512], in_=P1.ap[:, 0:512]), [P1], [dst])
        with P.scope():
            Wsb = P.sb([128, 8, O], BF16, "Wsb")
            W_v = W.ap.rearrange("(kc p) o -> p kc o", p=128)
            for kc in range(8):
                P.dma(View(Wsb, Wsb.ap[:, kc, :]), View(W, W_v[:, kc, :]), q="pool")
            xt = [P.sb([128, 1024], F32, f"xt{i}") for i in range(2)]
            h = P.sb([128, 1024], F32, "h")
            hT = [P.sb([128, 8, 128], BF16, f"hT{i}") for i in range(2)]
            yt = [P.sb([128, 1024], F32, f"yt{i}") for i in range(2)]
            ss = P.sb([128, 2], F32, "ss")
            nyc = 0
            for ti in range(NT):
                tsl = slice(ti * 128, (ti + 1) * 128)
                xb = xt[ti % 2]
                hb = hT[ti % 2]
                P.dma(xb, View(x, x.ap[tsl, :]), q="sp")
                P.op("act", lambda: nc.scalar.activation(out=h.ap, in_=xb.ap, func=AF.Square, accum_out=ss.ap[:, 0:1]), [xb], [h, ss])
                P.op("dve", lambda: nc.vector.tensor_scalar(out=ss.ap[:, 1:2], in0=ss.ap[:, 0:1], scalar1=1.0 / 1024, scalar2=EPS, op0=OP.mult, op1=OP.add), [ss], [ss])
                P.op("act", lambda: nc.scalar.activation(out=ss.ap[:, 1:2], in_=ss.ap[:, 1:2], func=AF.Sqrt), [ss], [ss])
                P.op("dve", lambda: nc.vector.reciprocal(out=ss.ap[:, 1:2], in_=ss.ap[:, 1:2]), [ss], [ss])
                P.op("dve", lambda: nc.vector.scalar_tensor_tensor(out=h.ap, in0=xb.ap, scalar=ss.ap[:, 1:2], in1=A1b.ap, op0=OP.mult, op1=OP.mult), [xb, ss, A1b], [h])
                P.op("pool", lambda: nc.gpsimd.tensor_tensor(out=h.ap, in0=h.ap, in1=B1b.ap, op=OP.add), [h, B1b], [h])
                for kc in range(8):
                    P.op("pe", lambda: nc.tensor.transpose(P1.ap[:, kc * 128:(kc + 1) * 128], h.ap[:, kc * 128:(kc + 1) * 128], ident.ap), [h, ident], [P1])
                P.op("act", lambda: nc.scalar.copy(out=hb.ap.rearrange("p a b -> p (a b)"), in_=P1.ap), [P1], [hb])
                for o0 in range(0, O, 1024):
                    ow = min(1024, O - o0)
                    yb = yt[nyc % 2]
                    nyc += 1
                    for c0 in range(0, ow, 512):
                        cw = min(512, ow - c0)
                        for kc in range(8):
                            P.op("pe", lambda: nc.tensor.matmul(P2.ap[:, c0:c0 + cw], lhsT=hb.ap[:, kc, :], rhs=Wsb.ap[:, kc, o0 + c0:o0 + c0 + cw], start=(kc == 0), stop=(kc == 7)), [hb, Wsb], [P2])
                    if (nyc % 2) == 0:
                        P.op("act", lambda: nc.scalar.copy(out=yb.ap[:, 0:ow], in_=P2.ap[:, 0:ow]), [P2], [yb])
                    else:
                        P.op("dve", lambda: nc.vector.tensor_copy(out=yb.ap[:, 0:ow], in_=P2.ap[:, 0:ow]), [P2], [yb])
                    P.dma(View(Y, Y.ap[tsl, o0:o0 + ow]), View(yb, yb.ap[:, 0:ow]), q="act")
        P.barrier()
        P.finish([Y])
        print("phase A: ninst", P.ninst, "nwait", P.nwait)
    return nc


EPS = 1e-6
TWO_PI = 2.0 * math.pi
C1 = 6.28125
C2 = TWO_PI - C1


def rope_tables(P, nc, pos_i, posf, tmp, tmi, CS, SN, invf, sgn, HP):
    P.op("dve", lambda: nc.vector.tensor_copy(out=posf.ap, in_=pos_i.ap), [pos_i], [posf])
    P.op("dve", lambda: nc.vector.tensor_scalar(out=posf.ap, in0=posf.ap, scalar1=invf.ap[0:HP, 0:1], scalar2=None, op0=OP.mult), [posf, invf], [posf])
    P.op("dve", lambda: nc.vector.tensor_scalar(out=tmi.ap, in0=posf.ap, scalar1=1.0 / TWO_PI, scalar2=None, op0=OP.mult), [posf], [tmi])
    P.op("dve", lambda: nc.vector.tensor_copy(out=tmp.ap, in_=tmi.ap), [tmi], [tmp])
    P.op("dve", lambda: nc.vector.scalar_tensor_tensor(out=posf.ap, in0=tmp.ap, scalar=-C1, in1=posf.ap, op0=OP.mult, op1=OP.add), [tmp, posf], [posf])
    P.op("dve", lambda: nc.vector.scalar_tensor_tensor(out=posf.ap, in0=tmp.ap, scalar=-C2, in1=posf.ap, op0=OP.mult, op1=OP.add), [tmp, posf], [posf])
    P.op("dve", lambda: nc.vector.tensor_scalar(out=tmp.ap, in0=posf.ap, scalar1=math.pi, scalar2=-TWO_PI, op0=OP.is_gt, op1=OP.mult), [posf], [tmp])
    P.op("dve", lambda: nc.vector.tensor_tensor(out=posf.ap, in0=posf.ap, in1=tmp.ap, op=OP.add), [posf, tmp], [posf])
    P.op("dve", lambda: nc.vector.tensor_scalar(out=tmp.ap, in0=posf.ap, scalar1=-math.pi, scalar2=TWO_PI, op0=OP.is_lt, op1=OP.mult), [posf], [tmp])
    P.op("dve", lambda: nc.vector.tensor_tensor(out=posf.ap, in0=posf.ap, in1=tmp.ap, op=OP.add), [posf, tmp], [posf])
    P.op("dve", lambda: nc.vector.tensor_scalar(out=posf.ap, in0=posf.ap, scalar1=math.pi, scalar2=-math.pi, op0=OP.min, op1=OP.max), [posf], [posf])
    P.op("act", lambda: nc.scalar.activation(out=SN.ap, in_=posf.ap, func=AF.Sin), [posf], [SN])
    P.op("dve", lambda: nc.vector.tensor_scalar(out=SN.ap, in0=SN.ap, scalar1=sgn.ap[0:HP, 0:1], scalar2=None, op0=OP.mult), [SN, sgn], [SN])
    P.op("act", lambda: nc.scalar.activation(out=tmp.ap, in_=posf.ap, func=AF.Abs), [posf], [tmp])
    P.op("dve", lambda: nc.vector.tensor_scalar(out=tmp.ap, in0=tmp.ap, scalar1=-1.0, scalar2=math.pi / 2, op0=OP.mult, op1=OP.add), [tmp], [tmp])
    P.op("act", lambda: nc.scalar.activation(out=CS.ap, in_=tmp.ap, func=AF.Sin), [tmp], [CS])


def build_phase_b_mla(S):
    nc = bass.Bass("TRN2", target_bir_lowering=False)
    NG = S // 512
    NB = S // 128
    SCALE = (128 + 64) ** -0.5
    with ExitStack() as st:
        P = Prog(nc, st)
        cqT = P.dram("cqT", [512, S], F32, kind="ExternalInput")
        ckvT = P.dram("ckvT", [256, S], F32, kind="ExternalInput")
        krT = P.dram("krT", [64, S], F32, kind="ExternalInput")
        krTs = P.dram("krTs", [64, S], F32, kind="ExternalInput")
        pos = P.dram("pos", [1, S], I32, kind="ExternalInput")
        qn = P.dram("qn", [128, 4], F32, kind="ExternalInput")
        kvn = P.dram("kvn", [128, 2], F32, kind="ExternalInput")
        wuq_n = P.dram("wuq_n", [512, 128], F32, kind="ExternalInput")
        wuq_r = P.dram("wuq_r", [512, 64], F32, kind="ExternalInput")
        wuq_rs = P.dram("wuq_rs", [512, 64], F32, kind="ExternalInput")
        wukv_k = P.dram("wukv_k", [256, 128], F32, kind="ExternalInput")
        wukv_v = P.dram("wukv_v", [256, 128], F32, kind="ExternalInput")
        cst = P.dram("cst", [128, 2], F32, kind="ExternalInput")
        maskT_d = P.dram("maskT", [128, 128], F32, kind="ExternalInput")
        OT = P.dram("OT", [128, S], F32, kind="ExternalOutput")

        KTn = P.sb([128, S], BF16, "KTn")
        KTr = P.sb([64, S], BF16, "KTr")
        Vsb = P.sb([128, NB, 128], BF16, "Vsb")
        ones_f = P.sb([128, 128], F32, "ones_f")
        ones_b = P.sb([128, 128], BF16, "ones_b")
        maskT = P.sb([128, 128], BF16, "maskT_sb")
        cs = P.sb([128, 2], F32, "cst_sb")
        qn_sb = P.sb([128, 4], F32, "qn_sb")
        kvn_sb = P.sb([128, 2], F32, "kvn_sb")
        Wqn = P.sb([128, 4, 128], BF16, "Wqn")
        Wqr = P.sb([128, 4, 64], BF16, "Wqr")
        Wqrs = P.sb([128, 4, 64], BF16, "Wqrs")
        Wkk = P.sb([128, 2, 128], BF16, "Wkk")
        Wkv = P.sb([128, 2, 128], BF16, "Wkv")
        P.op("dve", lambda: nc.vector.memset(ones_f.ap, 1.0), [], [ones_f])
        P.op("dve", lambda: nc.vector.memset(ones_b.ap, 1.0), [], [ones_b])
        P.dma(maskT, maskT_d, q="pool")
        P.dma(cs, cst); P.dma(qn_sb, qn); P.dma(kvn_sb, kvn)
        P.dma(Wqn, View(wuq_n, wuq_n.ap.rearrange("(kc p) o -> p kc o", p=128)), q="pool")
        P.dma(Wqr, View(wuq_r, wuq_r.ap.rearrange("(kc p) o -> p kc o", p=128)), q="pool")
        P.dma(Wqrs, View(wuq_rs, wuq_rs.ap.rearrange("(kc p) o -> p kc o", p=128)), q="pool")
        P.dma(Wkk, View(wukv_k, wukv_k.ap.rearrange("(kc p) o -> p kc o", p=128)), q="pool")
        P.dma(Wkv, View(wukv_v, wukv_v.ap.rearrange("(kc p) o -> p kc o", p=128)), q="pool")
        invf = View(cs, cs.ap[:, 0:1]); sgn = View(cs, cs.ap[:, 1:2])

        cq_t = P.sb([128, 4, 512], F32, "cq_t")
        ckv_t = P.sb([128, 2, 512], F32, "ckv_t")
        sq = P.sb([128, 4, 512], F32, "sq")
        rs_q = P.sb([128, 512], F32, "rs_q")
        rs_k = P.sb([128, 512], F32, "rs_k")
        cqn = P.sb([128, 4, 512], BF16, "cqn")
        ckvn = P.sb([128, 2, 512], BF16, "ckvn")
        pos_i = P.sb([64, 512], I32, "pos_i")
        posf = P.sb([64, 512], F32, "posf")
        tmp = P.sb([64, 512], F32, "tmp")
        tmi = P.sb([64, 512], I32, "tmi")
        CS = P.sb([64, 512], F32, "CS")
        SN = P.sb([64, 512], F32, "SN")
        kr_t = P.sb([64, 512], F32, "kr_t")
        krs_t = P.sb([64, 512], F32, "krs_t")
        r1 = P.sb([64, 512], F32, "r1")
        r2 = P.sb([64, 512], F32, "r2")
        QTn = P.sb([128, 512], BF16, "QTn")
        QTr = P.sb([64, 512], BF16, "QTr")
        PT = [P.sb([128, 512], BF16, f"PT{i}") for i in range(2)]
        rec = P.sb([128, 512], F32, "rec")
        o_sb = P.sb([128, 512], F32, "o_sb")
        P1 = P.ps([128, 1024], F32, "P1")
        ST = [P.ps([128, 512], F32, f"ST{i}") for i in range(2)]
        OTp = P.ps([128, 512], F32, "OTp")
        DEN = P.ps([128, 512], F32, "DEN")

        cqT_v = cqT.ap.rearrange("(kc p) t -> p kc t", p=128)
        ckvT_v = ckvT.ap.rearrange("(kc p) t -> p kc t", p=128)
        it = 0
        for g in range(NG):
            gs = slice(g * 512, (g + 1) * 512)
            P.dma(cq_t, View(cqT, cqT_v[:, :, gs]), q="sp")
            P.dma(ckv_t, View(ckvT, ckvT_v[:, :, gs]), q="act")
            P.dma(kr_t, View(krT, krT.ap[:, gs]), q="sp")
            P.dma(krs_t, View(krTs, krTs.ap[:, gs]), q="act")
            P.dma(pos_i, View(pos, pos.ap[0:1, gs].to_broadcast([64, 512])), q="sp")
            rope_tables(P, nc, pos_i, posf, tmp, tmi, CS, SN, invf, sgn, 64)
            P.op("act", lambda: nc.scalar.activation(out=sq.ap, in_=cq_t.ap, func=AF.Square), [cq_t], [sq])
            for kc in range(4):
                P.op("pe", lambda: nc.tensor.matmul(P1.ap[:, 0:512], lhsT=ones_f.ap, rhs=sq.ap[:, kc, :], start=(kc == 0), stop=(kc == 3)), [ones_f, sq], [P1])
            P.op("dve", lambda: nc.vector.tensor_scalar(out=rs_q.ap, in0=P1.ap[:, 0:512], scalar1=1.0 / 512, scalar2=EPS, op0=OP.mult, op1=OP.add), [P1], [rs_q])
            P.op("act", lambda: nc.scalar.activation(out=rs_q.ap, in_=rs_q.ap, func=AF.Sqrt), [rs_q], [rs_q])
            P.op("dve", lambda: nc.vector.reciprocal(out=rs_q.ap, in_=rs_q.ap), [rs_q], [rs_q])
            for kc in range(4):
                P.op("dve", lambda: nc.vector.scalar_tensor_tensor(out=cqn.ap[:, kc, :], in0=cq_t.ap[:, kc, :], scalar=qn_sb.ap[:, kc:kc + 1], in1=rs_q.ap, op0=OP.mult, op1=OP.mult), [cq_t, qn_sb, rs_q], [cqn])
            P.op("act", lambda: nc.scalar.activation(out=sq.ap[:, 0:2, :], in_=ckv_t.ap, func=AF.Square), [ckv_t], [sq])
            for kc in range(2):
                P.op("pe", lambda: nc.tensor.matmul(P1.ap[:, 512:1024], lhsT=ones_f.ap, rhs=sq.ap[:, kc, :], start=(kc == 0), stop=(kc == 1)), [ones_f, sq], [P1])
            P.op("dve", lambda: nc.vector.tensor_scalar(out=rs_k.ap, in0=P1.ap[:, 512:1024], scalar1=1.0 / 256, scalar2=EPS, op0=OP.mult, op1=OP.add), [P1], [rs_k])
            P.op("act", lambda: nc.scalar.activation(out=rs_k.ap, in_=rs_k.ap, func=AF.Sqrt), [rs_k], [rs_k])
            P.op("dve", lambda: nc.vector.reciprocal(out=rs_k.ap, in_=rs_k.ap), [rs_k], [rs_k])
            for kc in range(2):
                P.op("dve", lambda: nc.vector.scalar_tensor_tensor(out=ckvn.ap[:, kc, :], in0=ckv_t.ap[:, kc, :], scalar=kvn_sb.ap[:, kc:kc + 1], in1=rs_k.ap, op0=OP.mult, op1=OP.mult), [ckv_t, kvn_sb, rs_k], [ckvn])
            for kc in range(4):
                P.op("pe", lambda: nc.tensor.matmul(P1.ap[:, 0:512], lhsT=Wqn.ap[:, kc, :], rhs=cqn.ap[:, kc, :], start=(kc == 0), stop=(kc == 3)), [Wqn, cqn], [P1])
            P.op("act", lambda: nc.scalar.activation(out=QTn.ap, in_=P1.ap[:, 0:512], func=AF.Copy, scale=SCALE), [P1], [QTn])
            for kc in range(4):
                P.op("pe", lambda: nc.tensor.matmul(P1.ap[0:64, 512:1024], lhsT=Wqr.ap[:, kc, :], rhs=cqn.ap[:, kc, :], start=(kc == 0), stop=(kc == 3)), [Wqr, cqn], [P1])
            P.op("dve", lambda: nc.vector.tensor_tensor(out=r1.ap, in0=P1.ap[0:64, 512:1024], in1=CS.ap, op=OP.mult), [P1, CS], [r1])
            for kc in range(4):
                P.op("pe", lambda: nc.tensor.matmul(P1.ap[0:64, 0:512], lhsT=Wqrs.ap[:, kc, :], rhs=cqn.ap[:, kc, :], start=(kc == 0), stop=(kc == 3)), [Wqrs, cqn], [P1])
            P.op("dve", lambda: nc.vector.tensor_tensor(out=r2.ap, in0=P1.ap[0:64, 0:512], in1=SN.ap, op=OP.mult), [P1, SN], [r2])
            P.op("dve", lambda: nc.vector.tensor_tensor(out=r1.ap, in0=r1.ap, in1=r2.ap, op=OP.add), [r1, r2], [r1])
            P.op("act", lambda: nc.scalar.activation(out=QTr.ap, in_=r1.ap, func=AF.Copy, scale=SCALE), [r1], [QTr])
            for kc in range(2):
                P.op("pe", lambda: nc.tensor.matmul(P1.ap[:, 512:1024], lhsT=Wkk.ap[:, kc, :], rhs=ckvn.ap[:, kc, :], start=(kc == 0), stop=(kc == 1)), [Wkk, ckvn], [P1])
            P.op("act", lambda: nc.scalar.copy(out=KTn.ap[:, gs], in_=P1.ap[:, 512:1024]), [P1], [KTn])
            P.op("dve", lambda: nc.vector.tensor_tensor(out=r1.ap, in0=kr_t.ap, in1=CS.ap, op=OP.mult), [kr_t, CS], [r1])
            P.op("dve", lambda: nc.vector.tensor_tensor(out=r2.ap, in0=krs_t.ap, in1=SN.ap, op=OP.mult), [krs_t, SN], [r2])
            P.op("dve", lambda: nc.vector.tensor_tensor(out=KTr.ap[:, gs], in0=r1.ap, in1=r2.ap, op=OP.add), [r1, r2], [KTr])
            for tt in range(4):
                for kc in range(2):
                    P.op("pe", lambda: nc.tensor.matmul(P1.ap[:, tt * 128:(tt + 1) * 128], lhsT=ckvn.ap[:, kc, tt * 128:(tt + 1) * 128], rhs=Wkv.ap[:, kc, :], start=(kc == 0), stop=(kc == 1)), [ckvn, Wkv], [P1])
            P.op("act", lambda: nc.scalar.copy(out=Vsb.ap[:, g * 4:(g + 1) * 4, :].rearrange("p a b -> p (a b)"), in_=P1.ap[:, 0:512]), [P1], [Vsb])
            nj = 4 * g + 4

            def S_(j):
                b = (it + j) % 2
                c0 = max(0, j - 4 * g) * 128
                ks = slice(j * 128, (j + 1) * 128)
                P.op("pe", lambda: nc.tensor.matmul(ST[b].ap[:, c0:512], lhsT=KTn.ap[:, ks], rhs=QTn.ap[:, c0:512], start=True, stop=False), [KTn, QTn], [ST[b]])
                P.op("pe", lambda: nc.tensor.matmul(ST[b].ap[:, c0:512], lhsT=KTr.ap[:, ks], rhs=QTr.ap[:, c0:512], start=False, stop=True), [KTr, QTr], [ST[b]])

            def EPV_(j):
                b = (it + j) % 2
                c0 = max(0, j - 4 * g) * 128
                P.op("act", lambda: nc.scalar.activation(out=PT[b].ap[:, c0:512], in_=ST[b].ap[:, c0:512], func=AF.Exp), [ST[b]], [PT[b]])
                if j >= 4 * g:
                    P.op("pool", lambda: nc.gpsimd.tensor_tensor(out=PT[b].ap[:, c0:c0 + 128], in0=PT[b].ap[:, c0:c0 + 128], in1=maskT.ap, op=OP.mult), [PT[b], maskT], [PT[b]])
                P.op("pe", lambda: nc.tensor.matmul(OTp.ap[:, c0:512], lhsT=Vsb.ap[:, j, :], rhs=PT[b].ap[:, c0:512], start=(j == 0), stop=(j == nj - 1)), [Vsb, PT[b]], [OTp])
                P.op("pe", lambda: nc.tensor.matmul(DEN.ap[:, c0:512], lhsT=ones_b.ap, rhs=PT[b].ap[:, c0:512], start=(j == 0), stop=(j == nj - 1)), [ones_b, PT[b]], [DEN])

            S_(0)
            for j in range(nj):
                if j + 1 < nj:
                    S_(j + 1)
                EPV_(j)
            it += nj
            P.op("dve", lambda: nc.vector.reciprocal(out=rec.ap, in_=DEN.ap), [DEN], [rec])
            P.op("dve", lambda: nc.vector.tensor_tensor(out=o_sb.ap, in0=OTp.ap, in1=rec.ap, op=OP.mult), [OTp, rec], [o_sb])
            P.dma(View(OT, OT.ap[:, gs]), o_sb, q="sp")
        P.barrier()
        P.finish([OT])
        print("phase B mla: ninst", P.ninst, "nwait", P.nwait)
    return nc


BIG = 30000.0
NEG = -3.0e38


def moba_body(P, nc, S, D):
    NG = S // 512
    NB = S // 128
    NBLK = S // 256
    SCALE = 128 ** -0.5
    qT, qTs, kT, kTs, v, pos = D["qT"], D["qTs"], D["kT"], D["kTs"], D["v"], D["pos"]
    OT = D["OT"]
    KT = P.sb([128, S], BF16, "m_KT")
    Vsb = P.sb([128, NB, 128], BF16, "m_Vsb")
    KMT = P.sb([128, NBLK], F32, "m_KMT")
    SelT = P.sb([NBLK, NBLK, 128], BF16, "m_SelT")
    ones_b = P.sb([128, 128], BF16, "m_ones_b")
    maskT = P.sb([128, 128], BF16, "m_maskT")
    ident = P.sb([128, 128], F32, "m_ident")
    cs = P.sb([128, 2], F32, "m_cst")
    P.op("dve", lambda: nc.vector.memset(ones_b.ap, 1.0), [], [ones_b])
    P.dma(maskT, D["maskT"], q="pool")
    P.dma(SelT, View(D["selT"], D["selT"].ap.rearrange("n (m k) -> n m k", k=128)), q="pool")
    P.dma(ident, D["ident"])
    P.dma(cs, D["cst"])
    v_v = v.ap.rearrange("(b p) d -> p b d", p=128)
    for b0 in range(0, NB, 16):
        b1 = min(NB, b0 + 16)
        P.dma(View(Vsb, Vsb.ap[:, b0:b1, :]), View(v, v_v[:, b0:b1, :]), q="pool")
    invf = View(cs, cs.ap[:, 0:1]); sgn = View(cs, cs.ap[:, 1:2])
    q_t = P.sb([128, 512], F32, "m_q_t"); qs_t = P.sb([128, 512], F32, "m_qs_t")
    k_t = P.sb([128, 512], F32, "m_k_t"); ks_t = P.sb([128, 512], F32, "m_ks_t")
    pos_i = P.sb([128, 512], I32, "m_pos_i"); posf = P.sb([128, 512], F32, "m_posf")
    tmp = P.sb([128, 512], F32, "m_tmp"); tmi = P.sb([128, 512], I32, "m_tmi")
    CS = P.sb([128, 512], F32, "m_CS"); SN = P.sb([128, 512], F32, "m_SN")
    r1 = P.sb([128, 512], F32, "m_r1"); r2 = P.sb([128, 512], F32, "m_r2")
    QTf = P.sb([128, 512], F32, "m_QTf")
    QT = P.sb([128, 512], BF16, "m_QT")
    gate = P.sb([128, NBLK], F32, "m_gate")
    m8 = P.sb([128, 8], F32, "m_m8")
    pen = P.sb([128, NBLK], F32, "m_pen")
    PenT = P.sb([NBLK, 512], BF16, "m_PenT")
    PT = [P.sb([128, 512], BF16, f"m_PT{i}") for i in range(2)]
    rec = P.sb([128, 512], F32, "m_rec")
    o_sb = P.sb([128, 512], F32, "m_o_sb")
    P1 = P.ps([128, 512], F32, "m_P1")
    ST = [P.ps([128, 512], F32, f"m_ST{i}") for i in range(2)]
    OTp = P.ps([128, 512], F32, "m_OTp")
    DEN = P.ps([128, 512], F32, "m_DEN")
    it = 0
    for g in range(NG):
        gs = slice(g * 512, (g + 1) * 512)
        P.dma(q_t, View(qT, qT.ap[:, gs]), q="sp")
        P.dma(qs_t, View(qTs, qTs.ap[:, gs]), q="act")
        P.dma(k_t, View(kT, kT.ap[:, gs]), q="sp")
        P.dma(ks_t, View(kTs, kTs.ap[:, gs]), q="act")
        P.dma(pos_i, View(pos, pos.ap[0:1, gs].to_broadcast([128, 512])), q="sp")
        rope_tables(P, nc, pos_i, posf, tmp, tmi, CS, SN, invf, sgn, 128)
        P.op("dve", lambda: nc.vector.tensor_tensor(out=r1.ap, in0=q_t.ap, in1=CS.ap, op=OP.mult), [q_t, CS], [r1])
        P.op("pool", lambda: nc.gpsimd.tensor_tensor(out=r2.ap, in0=qs_t.ap, in1=SN.ap, op=OP.mult), [qs_t, SN], [r2])
        P.op("dve", lambda: nc.vector.tensor_tensor(out=QTf.ap, in0=r1.ap, in1=r2.ap, op=OP.add), [r1, r2], [QTf])
        P.op("act", lambda: nc.scalar.activation(out=QT.ap, in_=QTf.ap, func=AF.Copy, scale=SCALE), [QTf], [QT])
        P.op("dve", lambda: nc.vector.tensor_tensor(out=r1.ap, in0=k_t.ap, in1=CS.ap, op=OP.mult), [k_t, CS], [r1])
        P.op("pool", lambda: nc.gpsimd.tensor_tensor(out=r2.ap, in0=ks_t.ap, in1=SN.ap, op=OP.mult), [ks_t, SN], [r2])
        P.op("dve", lambda: nc.vector.tensor_tensor(out=r1.ap, in0=r1.ap, in1=r2.ap, op=OP.add), [r1, r2], [r1])
        P.op("act", lambda: nc.scalar.copy(out=KT.ap[:, gs], in_=r1.ap), [r1], [KT])
        P.op("dve", lambda: nc.vector.tensor_reduce(out=KMT.ap[:, 2 * g:2 * g + 2], in_=r1.ap.rearrange("p (n k) -> p n k", k=256), axis=AX.X, op=OP.add), [r1], [KMT])
        P.op("dve", lambda: nc.vector.tensor_scalar(out=KMT.ap[:, 2 * g:2 * g + 2], in0=KMT.ap[:, 2 * g:2 * g + 2], scalar1=1.0 / 256, scalar2=None, op0=OP.mult), [KMT], [KMT])
        for qb in range(4):
            own = 2 * g + qb // 2
            P.op("dve", lambda: nc.vector.memset(gate.ap, NEG), [], [gate])
            if own > 0:
                P.op("pe", lambda: nc.tensor.matmul(P1.ap[:, 0:own], lhsT=QTf.ap[:, qb * 128:(qb + 1) * 128], rhs=KMT.ap[:, 0:own], start=True, stop=True), [QTf, KMT], [P1])
                P.op("dve", lambda: nc.vector.tensor_copy(out=gate.ap[:, 0:own], in_=P1.ap[:, 0:own]), [P1], [gate])
            P.op("dve", lambda: nc.vector.max(out=m8.ap, in_=gate.ap), [gate], [m8])
            P.op("dve", lambda: nc.vector.tensor_scalar(out=m8.ap[:, 2:3], in0=m8.ap[:, 2:3], scalar1=-1.0e30, scalar2=None, op0=OP.max), [m8], [m8])
            P.op("dve", lambda: nc.vector.tensor_scalar(out=pen.ap, in0=gate.ap, scalar1=m8.ap[:, 2:3], scalar2=BIG, op0=OP.is_ge, op1=OP.mult), [gate, m8], [pen])
            P.op("dve", lambda: nc.vector.tensor_scalar(out=pen.ap, in0=pen.ap, scalar1=-BIG, scalar2=None, op0=OP.add), [pen], [pen])
            P.op("dve", lambda: nc.vector.memset(pen.ap[:, own:own + 1], 0.0), [], [pen])
            P.op("pe", lambda: nc.tensor.transpose(P1.ap[0:NBLK, 128:256], pen.ap, ident.ap), [pen, ident], [P1])
            P.op("act", lambda: nc.scalar.copy(out=PenT.ap[:, qb * 128:(qb + 1) * 128], in_=P1.ap[0:NBLK, 128:256]), [P1], [PenT])
        nj = 4 * g + 4

        def S_(j):
            b = (it + j) % 2
            c0 = max(0, j - 4 * g) * 128
            ks = slice(j * 128, (j + 1) * 128)
            n = j // 2
            P.op("pe", lambda: nc.tensor.matmul(ST[b].ap[:, c0:512], lhsT=KT.ap[:, ks], rhs=QT.ap[:, c0:512], start=True, stop=False), [KT, QT], [ST[b]])
            P.op("pe", lambda: nc.tensor.matmul(ST[b].ap[:, c0:512], lhsT=SelT.ap[:, n, :], rhs=PenT.ap[:, c0:512], start=False, stop=True), [SelT, PenT], [ST[b]])

        def EPV_(j):
            b = (it + j) % 2
            c0 = max(0, j - 4 * g) * 128
            P.op("act", lambda: nc.scalar.activation(out=PT[b].ap[:, c0:512], in_=ST[b].ap[:, c0:512], func=AF.Exp), [ST[b]], [PT[b]])
            if j >= 4 * g:
                P.op("pool", lambda: nc.gpsimd.tensor_tensor(out=PT[b].ap[:, c0:c0 + 128], in0=PT[b].ap[:, c0:c0 + 128], in1=maskT.ap, op=OP.mult), [PT[b], maskT], [PT[b]])
            P.op("pe", lambda: nc.tensor.matmul(OTp.ap[:, c0:512], lhsT=Vsb.ap[:, j, :], rhs=PT[b].ap[:, c0:512], start=(j == 0), stop=(j == nj - 1)), [Vsb, PT[b]], [OTp])
            P.op("pe", lambda: nc.tensor.matmul(DEN.ap[:, c0:512], lhsT=ones_b.ap, rhs=PT[b].ap[:, c0:512], start=(j == 0), stop=(j == nj - 1)), [ones_b, PT[b]], [DEN])

        S_(0)
        for j in range(nj):
            if j + 1 < nj:
                S_(j + 1)
            EPV_(j)
        it += nj
        P.op("dve", lambda: nc.vector.reciprocal(out=rec.ap, in_=DEN.ap), [DEN], [rec])
        P.op("dve", lambda: nc.vector.tensor_tensor(out=o_sb.ap, in0=OTp.ap, in1=rec.ap, op=OP.mult), [OTp, rec], [o_sb])
        P.dma(View(OT, OT.ap[:, gs]), o_sb, q="sp")


def build_phase_b_moba(S):
    nc = bass.Bass("TRN2", target_bir_lowering=False)
    NBLK = S // 256
    with ExitStack() as st:
        P = Prog(nc, st)
        D = {}
        for nm in ("qT", "qTs", "kT", "kTs"):
            D[nm] = P.dram(nm, [128, S], F32, kind="ExternalInput")
        D["v"] = P.dram("v", [S, 128], F32, kind="ExternalInput")
        D["pos"] = P.dram("pos", [1, S], I32, kind="ExternalInput")
        D["cst"] = P.dram("cst", [128, 2], F32, kind="ExternalInput")
        D["maskT"] = P.dram("maskT", [128, 128], F32, kind="ExternalInput")
        D["selT"] = P.dram("selT", [NBLK, NBLK * 128], F32, kind="ExternalInput")
        D["ident"] = P.dram("ident", [128, 128], F32, kind="ExternalInput")
        D["OT"] = P.dram("OT", [128, S], F32, kind="ExternalOutput")
        with P.scope():
            moba_body(P, nc, S, D)
        P.barrier()
        P.finish([D["OT"]])
        print("phase B moba: ninst", P.ninst, "nwait", P.nwait)
    return nc


EPS = 1e-6


def gdn_body(P, nc, S, D):
    NG = S // 512
    NCH = S // 128
    op = P.op
    V = nc.vector
    A = nc.scalar
    PE = nc.tensor
    ident = P.sb([128, 128], F32, "g_ident"); tri = P.sb([128, 128], F32, "g_tri")
    maskS = P.sb([128, 128], F32, "g_maskS"); maskI = P.sb([128, 128], F32, "g_maskI")
    ones_f = P.sb([128, 128], F32, "g_ones")
    cw = P.sb([128, 12], F32, "g_cw"); hc = P.sb([128, 2], F32, "g_hc"); onb = P.sb([128, 128], F32, "g_onb")
    ba = P.sb([128, NCH, 2], F32, "g_ba")
    beta = P.sb([128, NCH], F32, "g_beta"); nbeta = P.sb([128, NCH], F32, "g_nbeta"); graw = P.sb([128, NCH], F32, "g_graw")
    tmpc = P.sb([128, NCH], F32, "g_tmpc")
    for b_, d_ in ((ident, "ident"), (tri, "tri"), (maskS, "maskS"), (maskI, "maskI"), (cw, "cw"), (hc, "hc"), (onb, "onorm")):
        P.dma(b_, D[d_], q="sp")
    P.dma(ba, D["ba"], q="act")
    op("dve", lambda: V.memset(ones_f.ap, 1.0), [], [ones_f])
    op("act", lambda: A.activation(out=beta.ap, in_=ba.ap[:, :, 0], func=AF.Sigmoid), [ba], [beta])
    op("dve", lambda: V.tensor_scalar(out=nbeta.ap, in0=beta.ap, scalar1=-1.0, scalar2=None, op0=OP.mult), [beta], [nbeta])
    op("act", lambda: A.activation(out=tmpc.ap, in_=ba.ap[:, :, 1], func=AF.Exp, bias=hc.ap[:, 1:2]), [ba, hc], [tmpc])
    op("act", lambda: A.activation(out=tmpc.ap, in_=tmpc.ap, func=AF.Ln, bias=ones_f.ap[:, 0:1]), [tmpc, ones_f], [tmpc])
    op("act", lambda: A.activation(out=hc.ap[:, 0:1], in_=hc.ap[:, 0:1], func=AF.Exp), [hc], [hc])
    op("dve", lambda: V.tensor_scalar(out=graw.ap, in0=tmpc.ap, scalar1=hc.ap[:, 0:1], scalar2=-1.0, op0=OP.mult, op1=OP.mult), [tmpc, hc], [graw])

    St = P.sb([128, 128], F32, "g_S")
    op("dve", lambda: V.memset(St.ap, 0.0), [], [St])
    xin = [P.sb([128, 515], F32, f"g_xin{i}") for i in range(3)]
    yc = [P.sb([128, 512], F32, f"g_yc{i}") for i in range(3)]
    sq = P.sb([128, 512], F32, "g_sq")
    rs = P.sb([128, 512], F32, "g_rs")
    names = ["GrB", "Gm_sb", "t1", "Dm", "EG", "Am", "Bm", "A2", "B2", "Ym", "qk", "qkT", "kbg", "kd", "vb", "wT", "u", "qdT", "vnew", "zt", "ot", "o2"]
    W = {n: P.sb([128, 128], F32, "g_" + n) for n in names}
    cols = P.sb([128, 8], F32, "g_cols")
    PG = P.ps([128, 512], F32, "g_PG"); PTr = P.ps([128, 512], F32, "g_PTr"); PD = P.ps([128, 512], F32, "g_PD")
    PW = P.ps([128, 512], F32, "g_PW"); PS = P.ps([128, 512], F32, "g_PS"); PC = P.ps([128, 512], F32, "g_PC")
    c_ = lambda i: slice(i * 128, (i + 1) * 128)
    xs = (D["xq"], D["xk"], D["xv"])
    z_v = D["z"].ap.rearrange("(n p) d -> n p d", p=128)
    oa_v = D["OA"].ap.rearrange("(n p) d -> n p d", p=128)
    for g in range(NG):
        for i in range(3):
            P.dma(xin[i], View(xs[i], xs[i].ap[:, g * 512:g * 512 + 515]), q="sp" if i != 1 else "act")
            op("dve", lambda: V.tensor_scalar(out=yc[i].ap, in0=xin[i].ap[:, 0:512], scalar1=cw.ap[:, 4 * i:4 * i + 1], scalar2=None, op0=OP.mult), [xin[i], cw], [yc[i]])
            for j in range(1, 4):
                op("dve", lambda: V.scalar_tensor_tensor(out=yc[i].ap, in0=xin[i].ap[:, j:j + 512], scalar=cw.ap[:, 4 * i + j:4 * i + j + 1], in1=yc[i].ap, op0=OP.mult, op1=OP.add), [xin[i], cw, yc[i]], [yc[i]])
            op("act", lambda: A.activation(out=yc[i].ap, in_=yc[i].ap, func=AF.Silu), [yc[i]], [yc[i]])
            if i < 2:
                op("act", lambda: A.activation(out=sq.ap, in_=yc[i].ap, func=AF.Square), [yc[i]], [sq])
                op("pe", lambda: PE.matmul(PC.ap, lhsT=ones_f.ap, rhs=sq.ap, start=True, stop=True), [ones_f, sq], [PC])
                op("dve", lambda: V.tensor_scalar(out=rs.ap, in0=PC.ap, scalar1=EPS, scalar2=None, op0=OP.add), [PC], [rs])
                op("act", lambda: A.activation(out=rs.ap, in_=rs.ap, func=AF.Sqrt), [rs], [rs])
                op("dve", lambda: V.reciprocal(out=rs.ap, in_=rs.ap), [rs], [rs])
                sc_ = (128 ** -0.5) if i == 0 else 1.0
                op("dve", lambda: V.scalar_tensor_tensor(out=yc[i].ap, in0=yc[i].ap, scalar=sc_, in1=rs.ap, op0=OP.mult, op1=OP.mult), [yc[i], rs], [yc[i]])
        for cc in range(4):
            n = g * 4 + cc
            qTn = View(yc[0], yc[0].ap[:, c_(cc)]); kTn = View(yc[1], yc[1].ap[:, c_(cc)]); vTn = View(yc[2], yc[2].ap[:, c_(cc)])
            gr = graw.ap[:, n:n + 1]; be = beta.ap[:, n:n + 1]; nbe = nbeta.ap[:, n:n + 1]
            op("dve", lambda: V.tensor_scalar(out=W["GrB"].ap, in0=ones_f.ap, scalar1=gr, scalar2=None, op0=OP.mult), [ones_f, graw], [W["GrB"]])
            op("pe", lambda: PE.matmul(PG.ap[:, c_(0)], lhsT=W["GrB"].ap, rhs=tri.ap, start=True, stop=True), [W["GrB"], tri], [PG])
            op("pe", lambda: PE.matmul(PG.ap[:, 128:129], lhsT=tri.ap, rhs=gr, start=True, stop=True), [tri, graw], [PG])
            op("pe", lambda: PE.matmul(PG.ap[:, c_(2)], lhsT=kTn.ap, rhs=kTn.ap, start=True, stop=True), [kTn], [PG])
            op("pe", lambda: PE.matmul(PG.ap[:, c_(3)], lhsT=qTn.ap, rhs=kTn.ap, start=True, stop=True), [qTn, kTn], [PG])
            op("act", lambda: A.copy(out=W["Gm_sb"].ap, in_=PG.ap[:, c_(0)]), [PG], [W["Gm_sb"]])
            op("act", lambda: A.copy(out=cols.ap[:, 0:1], in_=PG.ap[:, 128:129]), [PG], [cols])
            op("dve", lambda: V.tensor_scalar(out=W["t1"].ap, in0=W["Gm_sb"].ap, scalar1=cols.ap[:, 0:1], scalar2=0.0, op0=OP.subtract, op1=OP.max), [W["Gm_sb"], cols], [W["t1"]])
            op("act", lambda: A.activation(out=W["Dm"].ap, in_=W["t1"].ap, func=AF.Exp, scale=-1.0), [W["t1"]], [W["Dm"]])
            op("act", lambda: A.activation(out=W["EG"].ap, in_=W["Gm_sb"].ap, func=AF.Exp), [W["Gm_sb"]], [W["EG"]])
            op("act", lambda: A.activation(out=cols.ap[:, 1:2], in_=cols.ap[:, 0:1], func=AF.Exp), [cols], [cols])
            op("act", lambda: A.activation(out=cols.ap[:, 2:3], in_=cols.ap[:, 0:1], func=AF.Exp, scale=-1.0, bias=W["Gm_sb"].ap[:, 127:128]), [cols, W["Gm_sb"]], [cols])
            op("dve", lambda: V.tensor_tensor(out=cols.ap[:, 3:4], in0=cols.ap[:, 1:2], in1=be, op=OP.mult), [cols, beta], [cols])
            op("dve", lambda: V.tensor_tensor(out=W["t1"].ap, in0=PG.ap[:, c_(2)], in1=W["Dm"].ap, op=OP.mult), [PG, W["Dm"]], [W["t1"]])
            op("dve", lambda: V.scalar_tensor_tensor(out=W["Am"].ap, in0=W["t1"].ap, scalar=nbe, in1=maskS.ap, op0=OP.mult, op1=OP.mult), [W["t1"], nbeta, maskS], [W["Am"]])
            op("dve", lambda: V.tensor_tensor(out=W["t1"].ap, in0=PG.ap[:, c_(3)], in1=W["Dm"].ap, op=OP.mult), [PG, W["Dm"]], [W["t1"]])
            op("dve", lambda: V.tensor_tensor(out=W["qk"].ap, in0=W["t1"].ap, in1=maskI.ap, op=OP.mult), [W["t1"], maskI], [W["qk"]])
            op("pe", lambda: PE.transpose(PTr.ap[:, c_(0)], W["Am"].ap, ident.ap), [W["Am"], ident], [PTr])
            op("pe", lambda: PE.transpose(PTr.ap[:, c_(1)], W["qk"].ap, ident.ap), [W["qk"], ident], [PTr])
            op("pe", lambda: PE.transpose(PTr.ap[:, c_(2)], kTn.ap, ident.ap), [kTn, ident], [PTr])
            op("pe", lambda: PE.transpose(PTr.ap[:, c_(3)], vTn.ap, ident.ap), [vTn, ident], [PTr])
            op("act", lambda: A.copy(out=W["Bm"].ap, in_=PTr.ap[:, c_(0)]), [PTr], [W["Bm"]])
            op("act", lambda: A.copy(out=W["qkT"].ap, in_=PTr.ap[:, c_(1)]), [PTr], [W["qkT"]])
            op("act", lambda: A.activation(out=W["kbg"].ap, in_=PTr.ap[:, c_(2)], func=AF.Copy, scale=cols.ap[:, 3:4]), [PTr, cols], [W["kbg"]])
            op("act", lambda: A.activation(out=W["kd"].ap, in_=PTr.ap[:, c_(2)], func=AF.Copy, scale=cols.ap[:, 2:3]), [PTr, cols], [W["kd"]])
            op("act", lambda: A.activation(out=W["vb"].ap, in_=PTr.ap[:, c_(3)], func=AF.Copy, scale=be), [PTr, beta], [W["vb"]])
            op("dve", lambda: V.tensor_tensor(out=W["Ym"].ap, in0=W["Bm"].ap, in1=ident.ap, op=OP.add), [W["Bm"], ident], [W["Ym"]])
            Ac, Bc, An, Bn = W["Am"], W["Bm"], W["A2"], W["B2"]
            for stp in range(6):
                op("pe", lambda: PE.matmul(PD.ap[:, c_(0)], lhsT=Bc.ap, rhs=Ac.ap, start=True, stop=True), [Bc, Ac], [PD])
                if stp < 5:
                    op("pe", lambda: PE.matmul(PD.ap[:, c_(1)], lhsT=Ac.ap, rhs=Bc.ap, start=True, stop=True), [Ac, Bc], [PD])
                op("act", lambda: A.copy(out=An.ap, in_=PD.ap[:, c_(0)]), [PD], [An])
                if stp < 5:
                    op("act", lambda: A.copy(out=Bn.ap, in_=PD.ap[:, c_(1)]), [PD], [Bn])
                op("pe", lambda: PE.matmul(PD.ap[:, c_(2)], lhsT=An.ap, rhs=W["Ym"].ap, start=True, stop=True), [An, W["Ym"]], [PD])
                op("dve", lambda: V.tensor_tensor(out=W["Ym"].ap, in0=W["Ym"].ap, in1=PD.ap[:, c_(2)], op=OP.add), [W["Ym"], PD], [W["Ym"]])
                Ac, Bc, An, Bn = An, Bn, Ac, Bc
            op("pe", lambda: PE.matmul(PW.ap[:, c_(0)], lhsT=W["kbg"].ap, rhs=W["Ym"].ap, start=True, stop=True), [W["kbg"], W["Ym"]], [PW])
            op("pe", lambda: PE.matmul(PW.ap[:, c_(1)], lhsT=W["Ym"].ap, rhs=W["vb"].ap, start=True, stop=True), [W["Ym"], W["vb"]], [PW])
            op("act", lambda: A.copy(out=W["wT"].ap, in_=PW.ap[:, c_(0)]), [PW], [W["wT"]])
            op("act", lambda: A.copy(out=W["u"].ap, in_=PW.ap[:, c_(1)]), [PW], [W["u"]])
            op("dve", lambda: V.tensor_tensor(out=W["qdT"].ap, in0=qTn.ap, in1=W["EG"].ap, op=OP.mult), [qTn, W["EG"]], [W["qdT"]])
            op("pe", lambda: PE.matmul(PS.ap[:, c_(0)], lhsT=W["wT"].ap, rhs=St.ap, start=True, stop=True), [W["wT"], St], [PS])
            op("dve", lambda: V.tensor_tensor(out=W["vnew"].ap, in0=W["u"].ap, in1=PS.ap[:, c_(0)], op=OP.subtract), [W["u"], PS], [W["vnew"]])
            op("pe", lambda: PE.matmul(PS.ap[:, c_(1)], lhsT=W["qdT"].ap, rhs=St.ap, start=True, stop=False), [W["qdT"], St], [PS])
            op("pe", lambda: PE.matmul(PS.ap[:, c_(1)], lhsT=W["qkT"].ap, rhs=W["vnew"].ap, start=False, stop=True), [W["qkT"], W["vnew"]], [PS])
            op("pe", lambda: PE.matmul(PS.ap[:, c_(2)], lhsT=W["kd"].ap, rhs=W["vnew"].ap, start=True, stop=True), [W["kd"], W["vnew"]], [PS])
            op("act", lambda: A.copy(out=W["ot"].ap, in_=PS.ap[:, c_(1)]), [PS], [W["ot"]])
            op("dve", lambda: V.scalar_tensor_tensor(out=St.ap, in0=St.ap, scalar=W["EG"].ap[:, 127:128], in1=PS.ap[:, c_(2)], op0=OP.mult, op1=OP.add), [St, W["EG"], PS], [St])
            P.dma(W["zt"], View(D["z"], z_v[n]), q="act")
            op("act", lambda: A.activation(out=W["o2"].ap, in_=W["ot"].ap, func=AF.Square, accum_out=cols.ap[:, 4:5]), [W["ot"]], [W["o2"], cols])
            op("dve", lambda: V.tensor_scalar(out=cols.ap[:, 5:6], in0=cols.ap[:, 4:5], scalar1=1.0 / 128, scalar2=EPS, op0=OP.mult, op1=OP.add), [cols], [cols])
            op("act", lambda: A.activation(out=cols.ap[:, 5:6], in_=cols.ap[:, 5:6], func=AF.Sqrt), [cols], [cols])
            op("dve", lambda: V.reciprocal(out=cols.ap[:, 5:6], in_=cols.ap[:, 5:6]), [cols], [cols])
            op("act", lambda: A.activation(out=W["zt"].ap, in_=W["zt"].ap, func=AF.Silu), [W["zt"]], [W["zt"]])
            op("dve", lambda: V.scalar_tensor_tensor(out=W["o2"].ap, in0=W["ot"].ap, scalar=cols.ap[:, 5:6], in1=onb.ap, op0=OP.mult, op1=OP.mult), [W["ot"], cols, onb], [W["o2"]])
            op("dve", lambda: V.tensor_tensor(out=W["o2"].ap, in0=W["o2"].ap, in1=W["zt"].ap, op=OP.mult), [W["o2"], W["zt"]], [W["o2"]])
            P.dma(View(D["OA"], oa_v[n]), W["o2"], q="sp")


def build_phase_b_gdn(S):
    nc = bass.Bass("TRN2", target_bir_lowering=False)
    with ExitStack() as st:
        P = Prog(nc, st)
        D = {}
        for nm in ("xq", "xk", "xv"):
            D[nm] = P.dram(nm, [128, S + 3], F32, kind="ExternalInput")
        D["cw"] = P.dram("cw", [128, 12], F32, kind="ExternalInput")
        D["z"] = P.dram("z", [S, 128], F32, kind="ExternalInput")
        D["ba"] = P.dram("ba", [128, S // 128, 2], F32, kind="ExternalInput")
        D["hc"] = P.dram("hc", [128, 2], F32, kind="ExternalInput")
        D["onorm"] = P.dram("onorm", [128, 128], F32, kind="ExternalInput")
        for nm in ("ident", "tri", "maskS", "maskI"):
            D[nm] = P.dram(nm, [128, 128], F32, kind="ExternalInput")
        D["OA"] = P.dram("OA", [S, 128], F32, kind="ExternalOutput")
        with P.scope():
            gdn_body(P, nc, S, D)
        P.barrier()
        P.finish([D["OA"]])
        print("phase B gdn: ninst", P.ninst, "nwait", P.nwait)
    return nc


EPS = 1e-6
NEG = -3.0e38


def bcast_rows(P, nc, ones_row, row, dst, ps, ncols):
    for c0 in range(0, ncols, 512):
        P.op("pe", lambda: nc.tensor.matmul(ps.ap[:, 0:512], lhsT=ones_row.ap[0:1, 0:128], rhs=row.ap[0:1, c0:c0 + 512], start=True, stop=True), [ones_row, row], [ps])
        P.op("act", lambda: nc.scalar.copy(out=dst.ap[:, c0:c0 + 512], in_=ps.ap[:, 0:512]), [ps], [dst])


def build_phase_c(T, final=False, NCH=32):
    nc = bass.Bass("TRN2", target_bir_lowering=False)
    NT = T // 128
    with ExitStack() as st:
        P = Prog(nc, st)
        x = P.dram("x", [T, 1024], F32, kind="ExternalInput")
        oT = P.dram("oT", [1024, T], F32, kind="ExternalInput")
        w_out = P.dram("w_out", [1024, 1024], F32, kind="ExternalInput")
        cT = P.dram("cT", [128, 8], F32, kind="ExternalInput")
        modw = P.dram("modw", [1024, 4096], F32, kind="ExternalInput")
        modb = P.dram("modb", [1, 4096], F32, kind="ExternalInput")
        nrm = P.dram("nrm", [1, 1024], F32, kind="ExternalInput")
        fnrm = P.dram("fnrm", [1, 1024], F32, kind="ExternalInput")
        w_q = P.dram("w_q", [1024, 2048], F32, kind="ExternalInput")
        skT = P.dram("skT", [128, 16, 128], F32, kind="ExternalInput")
        uT = P.dram("uT", [1024, NCH * 512], F32, kind="ExternalInput")
        vv = P.dram("vv", [NCH * 512, 1024], F32, kind="ExternalInput")
        ident_d = P.dram("ident", [128, 128], F32, kind="ExternalInput")
        xo = P.dram("xo", [T, 1024], F32, kind="ExternalOutput")
        sc_s = P.dram("sc_s", [NT, 128, 3, 8, 128], F32)
        sc_x1 = P.dram("sc_x1", [T, 1024], F32)
        sc_h2T = P.dram("sc_h2T", [NT, 128, 8, 128], BF16)
        uT_bf = P.dram("uT_bf", [1024, NCH * 512], BF16)
        vv_bf = P.dram("vv_bf", [NCH * 512, 1024], BF16)

        ident = P.sb([128, 128], F32, "ident_sb")
        identb = P.sb([128, 128], BF16, "identb")
        ones_row = P.sb([1, 128], F32, "ones_row")
        G1b = P.sb([128, 1024], F32, "G1b")
        A2b = P.sb([128, 1024], F32, "A2b")
        B2b = P.sb([128, 1024], F32, "B2b")
        G2b = P.sb([128, 1024], F32, "G2b")
        FNb = P.sb([128, 1024], F32, "FNb")
        P.dma(ident, ident_d)
        P.op("dve", lambda: nc.vector.tensor_copy(out=identb.ap, in_=ident.ap), [ident], [identb])
        P.op("dve", lambda: nc.vector.memset(ones_row.ap, 1.0), [], [ones_row])

        for i in range(8):
            P.dma(View(uT_bf, uT_bf.ap[i * 128:(i + 1) * 128, :]), View(uT, uT.ap[i * 128:(i + 1) * 128, :]), q="pool")
        NV = NCH * 512 // 8
        for i in range(8):
            P.dma(View(vv_bf, vv_bf.ap[i * NV:(i + 1) * NV, :]), View(vv, vv.ap[i * NV:(i + 1) * NV, :]), q="pool")
        with P.scope():
            P1 = P.ps([128, 1024], F32, "P1a")
            cond = P.sb([128, 8], F32, "cond")
            modrow = P.sb([1, 4096], F32, "modrow")
            mb = P.sb([1, 4096], F32, "mb")
            nr = P.sb([1, 1024], F32, "nr")
            fn = P.sb([1, 1024], F32, "fn")
            a2row = P.sb([1, 1024], F32, "a2row")
            mwc = [P.sb([128, 8, 512], F32, f"mwc{i}") for i in range(2)]
            P.dma(cond, cT)
            P.dma(mb, modb, q="act")
            P.dma(nr, nrm, q="act")
            P.dma(fn, fnrm, q="act")
            P.op("act", lambda: nc.scalar.activation(out=cond.ap, in_=cond.ap, func=AF.Silu), [cond], [cond])
            mw_v = modw.ap.rearrange("(kc p) o -> p kc o", p=128)
            for ch in range(8):
                buf = mwc[ch % 2]
                P.dma(buf, View(modw, mw_v[:, :, ch * 512:(ch + 1) * 512]), q="sp" if ch % 2 == 0 else "act")
                for kc in range(8):
                    P.op("pe", lambda: nc.tensor.matmul(P1.ap[0:1, 0:512], lhsT=cond.ap[:, kc:kc + 1], rhs=buf.ap[:, kc, :], start=(kc == 0), stop=(kc == 7)), [cond, buf], [P1])
                P.op("dve", lambda: nc.vector.tensor_tensor(out=modrow.ap[0:1, ch * 512:(ch + 1) * 512], in0=P1.ap[0:1, 0:512], in1=mb.ap[0:1, ch * 512:(ch + 1) * 512], op=OP.add), [P1, mb], [modrow])
            P.op("dve", lambda: nc.vector.scalar_tensor_tensor(out=a2row.ap, in0=modrow.ap[0:1, 2048:3072], scalar=1.0, in1=nr.ap, op0=OP.add, op1=OP.mult), [modrow, nr], [a2row])
            g1row = View(modrow, modrow.ap[0:1, 0:1024])
            b2row = View(modrow, modrow.ap[0:1, 1024:2048])
            g2row = View(modrow, modrow.ap[0:1, 3072:4096])
            for (row, dst) in ((g1row, G1b), (a2row, A2b), (b2row, B2b), (g2row, G2b), (fn, FNb)):
                for c0 in range(0, 1024, 512):
                    rap = _ap(row)[0:1, c0:c0 + 512]
                    P.op("pe", lambda: nc.tensor.matmul(P1.ap[:, 0:512], lhsT=ones_row.ap[0:1, 0:128], rhs=rap, start=True, stop=True), [ones_row, row], [P1])
                    P.op("act", lambda: nc.scalar.copy(out=dst.ap[:, c0:c0 + 512], in_=P1.ap[:, 0:512]), [P1], [dst])

        with P.scope():
            P1 = P.ps([128, 1024], F32, "P1b")
            wo = P.sb([128, 8, 1024], BF16, "wo")
            wq = P.sb([128, 8, 2048], F32, "wq")
            sk = P.sb([128, 16, 128], F32, "sk")
            P.dma(wo, View(w_out, w_out.ap.rearrange("(kc p) o -> p kc o", p=128)), q="pool")
            for i in range(4):
                P.dma(View(wq, wq.ap[:, 2 * i:2 * i + 2, :]), View(w_q, w_q.ap.rearrange("(kc p) o -> p kc o", p=128)[:, 2 * i:2 * i + 2, :]), q="act" if i % 2 else "sp")
            P.dma(sk, skT, q="sp")
            xt = P.sb([128, 1024], F32, "xt")
            ot = P.sb([128, 8, 128], BF16, "ot")
            h2Tb = P.sb([128, 8, 128], BF16, "h2Tb")
            x1 = P.sb([128, 1024], F32, "x1")
            h2 = P.sb([128, 1024], F32, "h2")
            h2Tf = P.sb([128, 8, 128], F32, "h2Tf")
            qT = P.sb([128, 16, 128], F32, "qT")
            S3 = P.sb([128, 3, 8, 128], F32, "S3")
            s_sb = P.sb([128, 16, 128], F32, "s_sb")
            s_r = P.sb([128, 16, 128], F32, "s_r")
            v16 = P.sb([128, 16, 16], F32, "v16")
            cand = P.sb([128, 8, 256], F32, "cand")
            c16 = P.sb([128, 8, 16], F32, "c16")
            ec = P.sb([128, 8, 16], F32, "ec")
            st4 = P.sb([128, 4, 8], F32, "st4")
            ss = P.sb([128, 2], F32, "ss")
            oT_v = oT.ap.rearrange("(kc p) t -> p kc t", p=128)
            for ti in range(NT):
              if True:
                tsl = slice(ti * 128, (ti + 1) * 128)
                P.dma(xt, View(x, x.ap[tsl, :]), q="sp")
                P.dma(ot, View(oT, oT_v[:, :, tsl]), q="pool")
                for hf in range(2):
                    for kc in range(8):
                        P.op("pe", lambda: nc.tensor.matmul(P1.ap[:, hf * 512:(hf + 1) * 512], lhsT=ot.ap[:, kc, :], rhs=wo.ap[:, kc, hf * 512:(hf + 1) * 512], start=(kc == 0), stop=(kc == 7)), [ot, wo], [P1])
                P.op("dve", lambda: nc.vector.tensor_tensor(out=x1.ap, in0=P1.ap, in1=G1b.ap, op=OP.mult), [P1, G1b], [x1])
                P.op("pool", lambda: nc.gpsimd.tensor_tensor(out=x1.ap, in0=x1.ap, in1=xt.ap, op=OP.add), [x1, xt], [x1])
                P.dma(View(sc_x1, sc_x1.ap[tsl, :]), x1, q="sp")
                P.op("act", lambda: nc.scalar.activation(out=h2.ap, in_=x1.ap, func=AF.Square, accum_out=ss.ap[:, 0:1]), [x1], [h2, ss])
                P.op("dve", lambda: nc.vector.tensor_scalar(out=ss.ap[:, 1:2], in0=ss.ap[:, 0:1], scalar1=1.0 / 1024, scalar2=EPS, op0=OP.mult, op1=OP.add), [ss], [ss])
                P.op("act", lambda: nc.scalar.activation(out=ss.ap[:, 1:2], in_=ss.ap[:, 1:2], func=AF.Sqrt), [ss], [ss])
                P.op("dve", lambda: nc.vector.reciprocal(out=ss.ap[:, 1:2], in_=ss.ap[:, 1:2]), [ss], [ss])
                P.op("dve", lambda: nc.vector.scalar_tensor_tensor(out=h2.ap, in0=x1.ap, scalar=ss.ap[:, 1:2], in1=A2b.ap, op0=OP.mult, op1=OP.mult), [x1, ss, A2b], [h2])
                P.op("pool", lambda: nc.gpsimd.tensor_tensor(out=h2.ap, in0=h2.ap, in1=B2b.ap, op=OP.add), [h2, B2b], [h2])
                for kc in range(8):
                    P.op("pe", lambda: nc.tensor.transpose(P1.ap[:, kc * 128:(kc + 1) * 128], h2.ap[:, kc * 128:(kc + 1) * 128], ident.ap), [h2, ident], [P1])
                P.op("act", lambda: nc.scalar.copy(out=h2Tf.ap.rearrange("p a b -> p (a b)"), in_=P1.ap), [P1], [h2Tf])
                P.op("dve", lambda: nc.vector.tensor_copy(out=h2Tb.ap, in_=h2Tf.ap), [h2Tf], [h2Tb])
                P.dma(View(sc_h2T, sc_h2T.ap[ti]), h2Tb, q="sp")
                for rnd in range(2):
                    for j in range(8):
                        hp = rnd * 8 + j
                        for kc in range(8):
                            P.op("pe", lambda: nc.tensor.matmul(P1.ap[:, j * 128:(j + 1) * 128], lhsT=wq.ap[:, kc, hp * 128:(hp + 1) * 128], rhs=h2Tf.ap[:, kc, :], start=(kc == 0), stop=(kc == 7)), [wq, h2Tf], [P1])
                    P.op("act", lambda: nc.scalar.copy(out=qT.ap[:, rnd * 8:(rnd + 1) * 8, :].rearrange("p a b -> p (a b)"), in_=P1.ap), [P1], [qT])
                for rnd in range(2):
                    for j in range(8):
                        hp = rnd * 8 + j
                        P.op("pe", lambda: nc.tensor.matmul(P1.ap[:, j * 128:(j + 1) * 128], lhsT=qT.ap[:, hp, :], rhs=sk.ap[:, hp, :], start=True, stop=True), [qT, sk], [P1])
                    P.op("act", lambda: nc.scalar.copy(out=s_sb.ap[:, rnd * 8:(rnd + 1) * 8, :].rearrange("p a b -> p (a b)"), in_=P1.ap), [P1], [s_sb])
                for hp in range(16):
                    P.op("dve", lambda: nc.vector.max(out=v16.ap[:, hp, 0:8], in_=s_sb.ap[:, hp, :]), [s_sb], [v16])
                    P.op("dve", lambda: nc.vector.match_replace(out=s_r.ap[:, hp, :], in_to_replace=v16.ap[:, hp, 0:8], in_values=s_sb.ap[:, hp, :], imm_value=NEG), [s_sb, v16], [s_r])
                    P.op("dve", lambda: nc.vector.max(out=v16.ap[:, hp, 8:16], in_=s_r.ap[:, hp, :]), [s_r], [v16])
                v16v = v16.ap.rearrange("p (h two) k -> p h two k", two=2)
                P.op("dve", lambda: nc.vector.tensor_tensor(out=cand.ap.rearrange("p h (i j) -> p h i j", i=16), in0=v16v[:, :, 0, :].unsqueeze(3).to_broadcast([128, 8, 16, 16]), in1=v16v[:, :, 1, :].unsqueeze(2).to_broadcast([128, 8, 16, 16]), op=OP.add), [v16], [cand])
                for h in range(8):
                    P.op("dve", lambda: nc.vector.max(out=c16.ap[:, h, 0:8], in_=cand.ap[:, h, :]), [cand], [c16])
                    P.op("dve", lambda: nc.vector.match_replace(out=s_r.ap.rearrange("p a b -> p (a b)").rearrange("p (h c) -> p h c", h=8)[:, h, :], in_to_replace=c16.ap[:, h, 0:8], in_values=cand.ap[:, h, :], imm_value=NEG), [cand, c16], [s_r])
                    P.op("dve", lambda: nc.vector.max(out=c16.ap[:, h, 8:16], in_=s_r.ap.rearrange("p a b -> p (a b)").rearrange("p (h c) -> p h c", h=8)[:, h, :]), [s_r], [c16])
                P.op("dve", lambda: nc.vector.tensor_tensor(out=ec.ap, in0=c16.ap, in1=c16.ap[:, :, 0:1].to_broadcast([128, 8, 16]), op=OP.subtract), [c16], [ec])
                P.op("act", lambda: nc.scalar.activation(out=ec.ap, in_=ec.ap, func=AF.Exp), [ec], [ec])
                P.op("dve", lambda: nc.vector.tensor_reduce(out=st4.ap[:, 1, :], in_=ec.ap, axis=AX.X, op=OP.add), [ec], [st4])
                P.op("act", lambda: nc.scalar.activation(out=st4.ap[:, 2, :], in_=st4.ap[:, 1, :], func=AF.Ln), [st4], [st4])
                P.op("dve", lambda: nc.vector.tensor_tensor(out=st4.ap[:, 2, :], in0=st4.ap[:, 2, :], in1=c16.ap[:, :, 0], op=OP.add), [st4, c16], [st4])
                P.op("dve", lambda: nc.vector.tensor_scalar(out=st4.ap[:, 3, :], in0=c16.ap[:, :, 15], scalar1=-1e-3, scalar2=None, op0=OP.add), [c16], [st4])
                s_v = s_sb.ap.rearrange("p (h two) n -> p h two n", two=2)
                P.op("dve", lambda: nc.vector.tensor_tensor(out=S3.ap[:, 0], in0=s_v[:, :, 0, :], in1=st4.ap[:, 2, :].unsqueeze(2).to_broadcast([128, 8, 128]), op=OP.subtract), [s_sb, st4], [S3])
                P.op("dve", lambda: nc.vector.tensor_tensor(out=st4.ap[:, 0, :], in0=st4.ap[:, 3, :], in1=st4.ap[:, 2, :], op=OP.subtract), [st4], [st4])
                P.op("dve", lambda: nc.vector.memset(S3.ap[:, 1], 0.0), [], [S3])
                P.op("act", lambda: nc.scalar.activation(out=S3.ap[:, 1, :, 0], in_=st4.ap[:, 0, :], func=AF.Exp), [st4], [S3])
                P.op("pool", lambda: nc.gpsimd.tensor_copy(out=S3.ap[:, 2], in_=s_v[:, :, 1, :]), [s_sb], [S3])
                P.dma(View(sc_s, sc_s.ap[ti]), S3, q="act")

        with P.scope():
            uc = [P.sb([128, 8, 512], BF16, f"uc{i}") for i in range(2)]
            vc = [P.sb([128, 4, 1024], BF16, f"vc{i}") for i in range(3)]
            S3s = [P.sb([128, 3, 8, 128], F32, f"S3b{i}") for i in range(4)]
            x1s = [P.sb([128, 1024], F32, f"x1b{i}") for i in range(4)]
            h2Ts = [P.sb([128, 8, 128], BF16, f"h2Tt{i}") for i in range(4)]
            sumE = [P.sb([128, 8, 4, 128], F32, f"sumE{i}") for i in range(2)]
            Gh = [P.sb([128, 8, 512], BF16, f"Gh{i}") for i in range(2)]
            actT = [P.sb([128, 512], F32, f"actT{i}") for i in range(3)]
            gaT = [P.sb([128, 4, 128], BF16, f"gaT{i}") for i in range(2)]
            xo_t = P.sb([128, 1024], F32, "xo_t")
            junk = P.sb([128, 1024], F32, "junk2")
            ss = P.sb([128, 2], F32, "ss2")
            ACCs = [P.ps([128, 1024], F32, f"ACC{i}") for i in range(2)]
            PA = [P.ps([128, 512], F32, f"PA{i}") for i in range(2)]
            GTs = [P.ps([128, 512], F32, "GT0")] * 2
            PT = P.ps([128, 512], BF16, "PT")
            ga = P.sb([128, 512], BF16, "ga")
            uT_v = uT_bf.ap.rearrange("(kc p) e -> p kc e", p=128)
            vv_v = vv_bf.ap.rearrange("(b p) d -> p b d", p=128)
            assert NT % 2 == 0
            items = []
            for tp in range(NT // 2):
                for ci in range(NCH):
                    items.append((2 * tp, ci, 0))
                    items.append((2 * tp + 1, ci, 1))

            def load_tile(ti):
                P.dma(S3s[ti % 4], View(sc_s, sc_s.ap[ti]), q="sp")
                P.dma(x1s[ti % 4], View(sc_x1, sc_x1.ap[ti * 128:(ti + 1) * 128, :]), q="sp")
                P.dma(h2Ts[ti % 4], View(sc_h2T, sc_h2T.ap[ti]), q="sp")

            def stage1(idx):
                ti, ci, sub = items[idx]
                b = idx % 2
                b3 = idx % 3
                k = idx // 2
                S3 = S3s[ti % 4]
                h2T = h2Ts[ti % 4]
                if ci == 2 and sub == 0 and ti + 2 < NT:
                    load_tile(ti + 2)
                    load_tile(ti + 3)
                if sub == 0:
                    P.dma(uc[k % 2], View(uT_bf, uT_v[:, :, ci * 512:(ci + 1) * 512]), q="sp")
                    P.dma(vc[k % 3], View(vv_bf, vv_v[:, ci * 4:(ci + 1) * 4, :]), q="act")
                for kc in range(8):
                    P.op("pe", lambda: nc.tensor.matmul(PA[b].ap, lhsT=h2T.ap[:, kc, :], rhs=uc[k % 2].ap[:, kc, :], start=(kc == 0), stop=(kc == 7)), [h2T, uc[k % 2]], [PA[b]])
                P.op("act", lambda: nc.scalar.activation(out=actT[b3].ap, in_=PA[b].ap, func=AF.Gelu), [PA[b]], [actT[b3]])
                s1e_b = S3.ap[:, 0, :, ci * 4:(ci + 1) * 4].unsqueeze(3).to_broadcast([128, 8, 4, 128])
                s2_b = S3.ap[:, 2].unsqueeze(2).to_broadcast([128, 8, 4, 128])
                P.op("pool", lambda: nc.gpsimd.tensor_tensor(out=sumE[b].ap, in0=s1e_b, in1=s2_b, op=OP.add), [S3], [sumE[b]])
                P.op("act", lambda: nc.scalar.activation(out=sumE[b].ap, in_=sumE[b].ap, func=AF.Exp), [sumE[b]], [sumE[b]])

            def stage2a(idx):
                ti, ci, sub = items[idx]
                b = idx % 2
                S3 = S3s[ti % 4]
                GT = GTs[b]
                Ev = sumE[b].ap.rearrange("p h a n -> p h (a n)")
                for h in range(8):
                    P.op("dve", lambda: nc.vector.scalar_tensor_tensor(out=Gh[b].ap[:, h, :], in0=Ev[:, h, :], scalar=S3.ap[:, 1, h, 0:1], in1=Ev[:, h, :], op0=OP.is_ge, op1=OP.mult), [sumE[b], S3], [Gh[b]])
                for h in range(8):
                    P.op("pe", lambda: nc.tensor.matmul(GT.ap, lhsT=identb.ap, rhs=Gh[b].ap[:, h, :], start=(h == 0), stop=(h == 7)), [Gh[b], identb], [GT])

            def stage2b(idx):
                ti, ci, sub = items[idx]
                b = idx % 2
                b3 = idx % 3
                k = idx // 2
                GT = GTs[b]
                ACC = ACCs[sub]
                P.op("dve", lambda: nc.vector.tensor_tensor(out=ga.ap, in0=GT.ap, in1=actT[b3].ap, op=OP.mult), [GT, actT[b3]], [ga])
                for bb in range(4):
                    P.op("pe", lambda: nc.tensor.transpose(PT.ap[:, bb * 128:(bb + 1) * 128], ga.ap[:, bb * 128:(bb + 1) * 128], identb.ap), [ga, identb], [PT])
                P.op("act", lambda: nc.scalar.copy(out=gaT[b].ap.rearrange("p a b -> p (a b)"), in_=PT.ap), [PT], [gaT[b]])
                for bb in range(4):
                    for hf in range(2):
                        P.op("pe", lambda: nc.tensor.matmul(ACC.ap[:, hf * 512:(hf + 1) * 512], lhsT=gaT[b].ap[:, bb, :], rhs=vc[k % 3].ap[:, bb, hf * 512:(hf + 1) * 512], start=(ci == 0 and bb == 0), stop=(ci == NCH - 1 and bb == 3)), [gaT[b], vc[k % 3]], [ACC])

            def epilogue(ti, sub):
                x1 = x1s[ti % 4]
                ACC = ACCs[sub]
                P.op("dve", lambda: nc.vector.tensor_tensor(out=xo_t.ap, in0=ACC.ap, in1=G2b.ap, op=OP.mult), [ACC, G2b], [xo_t])
                P.op("pool", lambda: nc.gpsimd.tensor_tensor(out=xo_t.ap, in0=xo_t.ap, in1=x1.ap, op=OP.add), [xo_t, x1], [xo_t])
                if final:
                    P.op("act", lambda: nc.scalar.activation(out=junk.ap, in_=xo_t.ap, func=AF.Square, accum_out=ss.ap[:, 0:1]), [xo_t], [junk, ss])
                    P.op("dve", lambda: nc.vector.tensor_scalar(out=ss.ap[:, 1:2], in0=ss.ap[:, 0:1], scalar1=1.0 / 1024, scalar2=EPS, op0=OP.mult, op1=OP.add), [ss], [ss])
                    P.op("act", lambda: nc.scalar.activation(out=ss.ap[:, 1:2], in_=ss.ap[:, 1:2], func=AF.Sqrt), [ss], [ss])
                    P.op("dve", lambda: nc.vector.reciprocal(out=ss.ap[:, 1:2], in_=ss.ap[:, 1:2]), [ss], [ss])
                    P.op("dve", lambda: nc.vector.scalar_tensor_tensor(out=xo_t.ap, in0=xo_t.ap, scalar=ss.ap[:, 1:2], in1=FNb.ap, op0=OP.mult, op1=OP.mult), [xo_t, ss, FNb], [xo_t])
                P.dma(View(xo, xo.ap[ti * 128:(ti + 1) * 128, :]), xo_t, q="sp")

            load_tile(0)
            load_tile(1)
            NI = len(items)
            stage1(0)
            stage1(1)
            stage2a(0)
            for idx in range(NI):
                if idx + 2 < NI:
                    stage1(idx + 2)
                stage2b(idx)
                if items[idx][1] == NCH - 1:
                    epilogue(items[idx][0], items[idx][2])
                if idx + 1 < NI:
                    stage2a(idx + 1)
        P.barrier()
        P.finish([xo])
        print("phase C: ninst", P.ninst, "nwait", P.nwait)
    return nc


SEQ = 16384
NCORE = 8
TOK = SEQ // NCORE
_PROGS = {}
f32 = np.float32


def build_phase_b_even(S):
    nc = bass.Bass("TRN2", target_bir_lowering=False)
    NBLK = S // 256
    with ExitStack() as st:
        P = Prog(nc, st)
        D = {}
        for nm in ("xq", "xk", "xv"):
            D[nm] = P.dram(nm, [128, S + 3], F32, kind="ExternalInput")
        D["cw"] = P.dram("cw", [128, 12], F32, kind="ExternalInput")
        D["z"] = P.dram("z", [S, 128], F32, kind="ExternalInput")
        D["ba"] = P.dram("ba", [128, S // 128, 2], F32, kind="ExternalInput")
        D["hc"] = P.dram("hc", [128, 2], F32, kind="ExternalInput")
        D["onorm"] = P.dram("onorm", [128, 128], F32, kind="ExternalInput")
        for nm in ("ident", "tri", "maskS", "maskI"):
            D[nm] = P.dram(nm, [128, 128], F32, kind="ExternalInput")
        D["OA"] = P.dram("OA", [S, 128], F32, kind="ExternalOutput")
        for nm in ("qT", "qTs", "kT", "kTs"):
            D[nm] = P.dram(nm, [128, S], F32, kind="ExternalInput")
        D["v"] = P.dram("v", [S, 128], F32, kind="ExternalInput")
        D["pos"] = P.dram("pos", [1, S], I32, kind="ExternalInput")
        D["cst"] = P.dram("cst", [128, 2], F32, kind="ExternalInput")
        D["maskT"] = P.dram("maskT", [128, 128], F32, kind="ExternalInput")
        D["selT"] = P.dram("selT", [NBLK, NBLK * 128], F32, kind="ExternalInput")
        D["OT"] = P.dram("OT", [128, S], F32, kind="ExternalOutput")
        with P.scope():
            gdn_body(P, nc, S, D)
        with P.scope():
            moba_body(P, nc, S, D)
        P.barrier()
        P.finish([D["OA"], D["OT"]])
    return nc


def _prog(key):
    if key not in _PROGS:
        if key == "A_even":
            _PROGS[key] = build_phase_a(TOK, 3592)
        elif key == "A_odd":
            _PROGS[key] = build_phase_a(TOK, 832)
        elif key == "B_even":
            _PROGS[key] = build_phase_b_even(SEQ)
        elif key == "B_odd":
            _PROGS[key] = build_phase_b_mla(SEQ)
        elif key == "C":
            _PROGS[key] = build_phase_c(TOK, final=False)
        elif key == "C_final":
            _PROGS[key] = build_phase_c(TOK, final=True)
    return _PROGS[key]


def _run(key, in_maps):
    res = run_bass_kernel_spmd(_prog(key), in_maps, core_ids=list(range(NCORE)))
    return res.results


def _c(a):
    return np.ascontiguousarray(a)


def kernel(x, c, positions, mod_w, mod_b, norm_mix, norm_ffn, hy_w_in, gdn_conv, gdn_a_log,
           gdn_dt_bias, gdn_o_norm, hy_w_out, mla_w_in, mla_q_norm, mla_kv_norm, mla_w_uq,
           mla_w_ukv, mla_w_out, peer_w_q, peer_sub_keys, peer_u, peer_v, final_norm):
    A_ = lambda a: np.asarray(a)
    x, c, positions, mod_w, mod_b = A_(x), A_(c), A_(positions), A_(mod_w), A_(mod_b)
    norm_mix, norm_ffn, hy_w_in, gdn_conv = A_(norm_mix), A_(norm_ffn), A_(hy_w_in), A_(gdn_conv)
    gdn_a_log, gdn_dt_bias, gdn_o_norm, hy_w_out = A_(gdn_a_log), A_(gdn_dt_bias), A_(gdn_o_norm), A_(hy_w_out)
    mla_w_in, mla_q_norm, mla_kv_norm, mla_w_uq = A_(mla_w_in), A_(mla_q_norm), A_(mla_kv_norm), A_(mla_w_uq)
    mla_w_ukv, mla_w_out, peer_w_q, peer_sub_keys = A_(mla_w_ukv), A_(mla_w_out), A_(peer_w_q), A_(peer_sub_keys)
    peer_u, peer_v, final_norm = A_(peer_u), A_(peer_v), A_(final_norm)
    S = SEQ
    xcur = _c(x[0].astype(f32, copy=False))
    cT = _c(c.reshape(8, 128).T)
    pos = _c(positions.reshape(1, S).astype(np.int32, copy=False))
    I = np.eye(128, dtype=f32)
    tri = np.triu(np.ones((128, 128), f32))
    maskS = np.tril(np.ones((128, 128), f32), -1)
    maskI = np.tril(np.ones((128, 128), f32))
    maskT = np.triu(np.ones((128, 128), f32))
    NBLK = S // 256
    selT = np.kron(np.eye(NBLK, dtype=f32), np.ones((1, 128), f32))
    invf_b = (10000.0 ** (-np.arange(0, 128, 2, dtype=np.float32) / 128)).astype(f32)
    cst_b = np.zeros((128, 2), f32); cst_b[:, 0] = np.concatenate([invf_b, invf_b]); cst_b[:64, 1] = -1; cst_b[64:, 1] = 1
    invf_c = (10000.0 ** (-np.arange(0, 64, 2, dtype=np.float32) / 64)).astype(f32)
    cst_c = np.zeros((128, 2), f32); cst_c[:64, 0] = np.concatenate([invf_c, invf_c]); cst_c[:32, 1] = -1; cst_c[32:64, 1] = 1
    sw = lambda a: _c(np.concatenate([a[a.shape[0] // 2:], a[:a.shape[0] // 2]], 0))

    for l in range(4):
        i = l // 2
        even = (l % 2 == 0)
        W = hy_w_in[i] if even else mla_w_in[i]
        modw_a = _c(mod_w[l][:, 0:2048]); modb_a = _c(mod_b[l][None, 0:2048]); nrm_a = _c(norm_mix[l][None])
        in_maps = [dict(x=xcur[k * TOK:(k + 1) * TOK], cT=cT, modw=modw_a, modb=modb_a, nrm=nrm_a, W=_c(W), ident=I) for k in range(NCORE)]
        res = _run("A_even" if even else "A_odd", in_maps)
        Y = np.concatenate([r["Y"] for r in res], 0)
        if even:
            in_maps = []
            for k in range(NCORE):
                h = k % 4
                def xT(off):
                    a = Y[:, off + h * 128: off + (h + 1) * 128].T
                    return _c(np.concatenate([np.zeros((128, 3), f32), a], 1))
                cw = _c(np.concatenate([gdn_conv[i][:, off + h * 128: off + (h + 1) * 128].T for off in (0, 512, 1024)], 1))
                ba = _c(np.stack([Y[:, 2048 + h], Y[:, 2052 + h]], -1).reshape(S // 128, 128, 2).transpose(1, 0, 2))
                hc = _c(np.tile(np.array([[gdn_a_log[i][h], gdn_dt_bias[i][h]]], f32), (128, 1)))
                qT = _c(Y[:, 2056 + h * 128: 2056 + (h + 1) * 128].T)
                kT = _c(Y[:, 2568 + h * 128: 2568 + (h + 1) * 128].T)
                in_maps.append(dict(xq=xT(0), xk=xT(512), xv=xT(1024), cw=cw, z=_c(Y[:, 1536 + h * 128: 1536 + (h + 1) * 128]),
                                    ba=ba, hc=hc, onorm=_c(np.tile(gdn_o_norm[i][None], (128, 1))), ident=I, tri=tri, maskS=maskS, maskI=maskI,
                                    qT=qT, qTs=sw(qT), kT=kT, kTs=sw(kT), v=_c(Y[:, 3080 + h * 128: 3080 + (h + 1) * 128]),
                                    pos=pos, cst=cst_b, maskT=maskT, selT=selT))
            res = _run("B_even", in_maps)
            oT = np.concatenate([res[h]["OA"].T for h in range(4)] + [res[h]["OT"] for h in range(4)], 0)
            w_out = hy_w_out[i]
        else:
            YT = _c(Y.T)
            qn = _c(mla_q_norm[i].reshape(4, 128).T); kvn = _c(mla_kv_norm[i].reshape(2, 128).T)
            krT = _c(YT[768:832]); krTs = sw(krT)
            in_maps = []
            for h in range(NCORE):
                wq = mla_w_uq[i][:, h * 192:(h + 1) * 192]; wkv = mla_w_ukv[i][:, h * 256:(h + 1) * 256]
                wr = wq[:, 128:]
                in_maps.append(dict(cqT=YT[:512], ckvT=YT[512:768], krT=krT, krTs=krTs, pos=pos, qn=qn, kvn=kvn,
                                    wuq_n=_c(wq[:, :128]), wuq_r=_c(wr), wuq_rs=_c(np.concatenate([wr[:, 32:], wr[:, :32]], 1)),
                                    wukv_k=_c(wkv[:, :128]), wukv_v=_c(wkv[:, 128:]), cst=cst_c, maskT=maskT))
            res = _run("B_odd", in_maps)
            oT = np.concatenate([res[h]["OT"] for h in range(NCORE)], 0)
            w_out = mla_w_out[i]
        modw_c = _c(mod_w[l][:, 2048:6144]); modb_c = _c(mod_b[l][None, 2048:6144])
        skT = _c(peer_sub_keys[l].reshape(16, 128, 128).transpose(2, 0, 1))
        uT = _c(peer_u[l].T)
        vv = _c(peer_v[l])
        in_maps = [dict(x=xcur[k * TOK:(k + 1) * TOK], oT=_c(oT[:, k * TOK:(k + 1) * TOK]), w_out=_c(w_out), cT=cT, modw=modw_c, modb=modb_c,
                        nrm=_c(norm_ffn[l][None]), fnrm=_c(final_norm[None]), w_q=_c(peer_w_q[l]), skT=skT, uT=uT, vv=vv, ident=I) for k in range(NCORE)]
        res = _run("C_final" if l == 3 else "C", in_maps)
        xcur = np.concatenate([r["xo"] for r in res], 0)
    return xcur.reshape(1, S, 1024).astype(f32, copy=False)
```

```python
import math
import numpy as np
from contextlib import ExitStack
import concourse.bass as bass
import concourse.mybir as mybir
from concourse.bass_utils import run_bass_kernel_spmd


F32 = mybir.dt.float32
BF16 = mybir.dt.bfloat16
I32 = mybir.dt.int32
AF = mybir.ActivationFunctionType
OP = mybir.AluOpType
AX = mybir.AxisListType


SAME_ENGINE_SYNC = True


class Buf:
    __slots__ = ("ap", "w", "r", "dsem", "dcnt", "name", "is_dram", "is_psum")

    def __init__(self, ap, name=""):
        self.ap = ap
        self.w = None
        self.r = {}
        self.dsem = None
        self.dcnt = 0
        self.name = name
        self.is_dram = False
        self.is_psum = False

    def __getitem__(self, idx):
        return View(self, self.ap[idx])


class View:
    __slots__ = ("buf", "ap")

    def __init__(self, buf, ap):
        self.buf = buf
        self.ap = ap

    def __getitem__(self, idx):
        return View(self.buf, self.ap[idx])


def _b(x):
    return x.buf if isinstance(x, View) else x


def _ap(x):
    if isinstance(x, (Buf, View)):
        return x.ap
    return x


class Prog:
    ENG = ("pe", "act", "dve", "pool", "sp")

    def __init__(self, nc, stack, same_engine_sync=None):
        self.nc = nc
        self.stack = stack
        self.e = {"pe": nc.tensor, "act": nc.scalar, "dve": nc.vector, "pool": nc.gpsimd, "sp": nc.sync}
        self.sem = {k: stack.enter_context(nc.semaphore("s_" + k)) for k in self.ENG}
        self.cnt = {k: 0 for k in self.ENG}
        self.seen = {k: {} for k in self.ENG}
        self.same = SAME_ENGINE_SYNC if same_engine_sync is None else same_engine_sync
        self.ninst = 0
        self.nwait = 0
        self._dsems = []
        self._dbufs = []
        self._free_dsems = []
        self._scopes = []
        self.top = stack

    def sb(self, shape, dt=F32, name=None):
        t = self.stack.enter_context(self.nc.sbuf_tensor(name or f"sb{self.ninst}_{len(self._dsems)}_{np.random.randint(1<<30)}", list(shape), dt))
        return Buf(t.ap() if hasattr(t, "ap") and callable(getattr(t, "ap")) else t[:], name or "")

    def ps(self, shape, dt=F32, name=None):
        t = self.stack.enter_context(self.nc.psum_tensor(name or f"ps{np.random.randint(1<<30)}", list(shape), dt))
        b = Buf(t.ap() if hasattr(t, "ap") and callable(getattr(t, "ap")) else t[:], name or "")
        b.is_psum = True
        return b

    def dram(self, name, shape, dt=F32, kind="Internal"):
        t = self.nc.dram_tensor(name, list(shape), dt, kind=kind)
        b = Buf(t.ap(), name)
        b.is_dram = True
        return b

    def _need(self, eng, dep):
        if dep is None:
            return
        kind, key, count = dep
        if kind == "eng":
            if key == eng and (eng == "pe" or not self.same):
                return
            sem = self.sem[key]
            skey = key
        else:
            sem = key
            skey = ("d", id(key))
        if self.seen[eng].get(skey, 0) >= count:
            return
        self.e[eng].wait_ge(sem, count)
        self.seen[eng][skey] = count
        self.nwait += 1

    def _deps(self, eng, reads, writes):
        for x in reads:
            b = _b(x)
            self._need(eng, b.w)
            if b.is_psum:
                for k, d in b.r.items():
                    if k != eng:
                        self._need(eng, d)
        for x in writes:
            b = _b(x)
            self._need(eng, b.w)
            for d in b.r.values():
                self._need(eng, d)

    def op(self, eng, fn, reads=(), writes=(), indep=False):
        if indep:
            sv = self.same
            self.same = False
            self._deps(eng, reads, writes)
            self.same = sv
        else:
            self._deps(eng, reads, writes)
        inst = fn()
        self.cnt[eng] += 1
        c = self.cnt[eng]
        inst.then_inc(self.sem[eng], 1)
        tag = ("eng", eng, c)
        for x in reads:
            _b(x).r[eng] = tag
        for x in writes:
            b = _b(x)
            b.w = tag
            b.r = {}
        self.ninst += 1
        return inst

    def dma(self, out, in_, q="sp", **kw):
        ob, ib = _b(out), _b(in_)
        self._deps(q, [ib], [ob])
        owner = ib if (ob.is_dram and not ib.is_dram) else ob
        if owner.dsem is None:
            if False:
                pass
            else:
                owner.dsem = self.top.enter_context(self.nc.semaphore(f"d{len(self._dsems)}"))
                owner.dcnt = 0
                self._dsems.append(owner.dsem)
            self._dbufs.append(owner)
            if self._scopes:
                self._scopes[-1].append(owner)
        inst = self.e[q].dma_start(out=_ap(out), in_=_ap(in_), **kw)
        owner.dcnt += 16
        inst.then_inc(owner.dsem, 16)
        tag = ("dma", owner.dsem, owner.dcnt)
        ob.w = tag
        ob.r = {}
        ib.r[("dma", id(owner.dsem))] = tag
        self.ninst += 1
        return inst

    def barrier(self):
        for e in self.ENG:
            for k in self.ENG:
                if k != e and self.cnt[k] > 0:
                    self._need(e, ("eng", k, self.cnt[k]))
        for b in self._dbufs:
            for e in self.ENG:
                if b.dcnt > 0:
                    self._need(e, ("dma", b.dsem, b.dcnt))

    def scope(self):
        return _Scope(self)

    def finish(self, bufs, eng="sp"):
        for b in bufs:
            self._need(eng, b.w)


class _Scope:
    def __init__(self, P):
        self.P = P

    def __enter__(self):
        self.es = ExitStack()
        self.es.__enter__()
        self.prev = self.P.stack
        self.P.stack = self.es
        self.P._scopes.append([])
        return self

    def __exit__(self, *a):
        P = self.P
        P.barrier()
        owners = P._scopes.pop()
        for b in owners:
            P._free_dsems.append((b.dsem, b.dcnt))
            P._dbufs.remove(b)
            b.dsem = None
        P.stack = self.prev
        return self.es.__exit__(*a)


EPS = 1e-6


def build_phase_a(T, O):
    nc = bass.Bass("TRN2", target_bir_lowering=False)
    NT = T // 128
    with ExitStack() as st:
        P = Prog(nc, st)
        x = P.dram("x", [T, 1024], F32, kind="ExternalInput")
        cT = P.dram("cT", [128, 8], F32, kind="ExternalInput")
        modw = P.dram("modw", [1024, 2048], F32, kind="ExternalInput")
        modb = P.dram("modb", [1, 2048], F32, kind="ExternalInput")
        nrm = P.dram("nrm", [1, 1024], F32, kind="ExternalInput")
        W = P.dram("W", [1024, O], F32, kind="ExternalInput")
        ident_d = P.dram("ident", [128, 128], F32, kind="ExternalInput")
        Y = P.dram("Y", [T, O], F32, kind="ExternalOutput")

        ident = P.sb([128, 128], F32, "ident_sb")
        ones_row = P.sb([1, 128], F32, "ones_row")
        A1b = P.sb([128, 1024], F32, "A1b")
        B1b = P.sb([128, 1024], F32, "B1b")
        P1 = P.ps([128, 1024], F32, "P1")
        P2 = P.ps([128, 1024], F32, "P2")
        P.dma(ident, ident_d)
        P.op("dve", lambda: nc.vector.memset(ones_row.ap, 1.0), [], [ones_row])
        with P.scope():
            cond = P.sb([128, 8], F32, "cond")
            modrow = P.sb([1, 2048], F32, "modrow")
            mb = P.sb([1, 2048], F32, "mb")
            nr = P.sb([1, 1024], F32, "nr")
            a1row = P.sb([1, 1024], F32, "a1row")
            mwc = [P.sb([128, 8, 512], F32, f"mwc{i}") for i in range(2)]
            P.dma(cond, cT)
            P.dma(mb, modb, q="act")
            P.dma(nr, nrm, q="act")
            P.op("act", lambda: nc.scalar.activation(out=cond.ap, in_=cond.ap, func=AF.Silu), [cond], [cond])
            mw_v = modw.ap.rearrange("(kc p) o -> p kc o", p=128)
            for ch in range(4):
                buf = mwc[ch % 2]
                P.dma(buf, View(modw, mw_v[:, :, ch * 512:(ch + 1) * 512]), q="sp" if ch % 2 == 0 else "act")
                for kc in range(8):
                    P.op("pe", lambda: nc.tensor.matmul(P1.ap[0:1, 0:512], lhsT=cond.ap[:, kc:kc + 1], rhs=buf.ap[:, kc, :], start=(kc == 0), stop=(kc == 7)), [cond, buf], [P1])
                P.op("dve", lambda: nc.vector.tensor_tensor(out=modrow.ap[0:1, ch * 512:(ch + 1) * 512], in0=P1.ap[0:1, 0:512], in1=mb.ap[0:1, ch * 512:(ch + 1) * 512], op=OP.add), [P1, mb], [modrow])
            P.op("dve", lambda: nc.vector.scalar_tensor_tensor(out=a1row.ap, in0=modrow.ap[0:1, 1024:2048], scalar=1.0, in1=nr.ap, op0=OP.add, op1=OP.mult), [modrow, nr], [a1row])
            b1row = View(modrow, modrow.ap[0:1, 0:1024])
            for (row, dst) in ((a1row, A1b), (b1row, B1b)):
                for c0 in range(0, 1024, 512):
                    rap = _ap(row)[0:1, c0:c0 + 512]
                    P.op("pe", lambda: nc.tensor.matmul(P1.ap[:, 0:512], lhsT=ones_row.ap[0:1, 0:128], rhs=rap, start=True, stop=True), [ones_row, row], [P1])
                    P.op("act", lambda: nc.scalar.copy(out=dst.ap[:, c0:c0 + 512], in_=P1.ap[:, 0:512]), [P1], [dst])
        with P.scope():
            Wsb = P.sb([128, 8, O], BF16, "Wsb")
            W_v = W.ap.rearrange("(kc p) o -> p kc o", p=128)
            for kc in range(8):
                P.dma(View(Wsb, Wsb.ap[:, kc, :]), View(W, W_v[:, kc, :]), q="pool")
            xt = [P.sb([128, 1024], F32, f"xt{i}") for i in range(2)]
            h = P.sb([128, 1024], F32, "h")
            hT = [P.sb([128, 8, 128], BF16, f"hT{i}") for i in range(2)]
            yt = [P.sb([128, 1024], F32, f"yt{i}") for i in range(2)]
            ss = P.sb([128, 2], F32, "ss")
            nyc = 0
            for ti in range(NT):
                tsl = slice(ti * 128, (ti + 1) * 128)
                xb = xt[ti % 2]
                hb = hT[ti % 2]
                P.dma(xb, View(x, x.ap[tsl, :]), q="sp")
                P.op("act", lambda: nc.scalar.activation(out=h.ap, in_=xb.ap, func=AF.Square, accum_out=ss.ap[:, 0:1]), [xb], [h, ss])
                P.op("dve", lambda: nc.vector.tensor_scalar(out=ss.ap[:, 1:2], in0=ss.ap[:, 0:1], scalar1=1.0 / 1024, scalar2=EPS, op0=OP.mult, op1=OP.add), [ss], [ss])
                P.op("act", lambda: nc.scalar.activation(out=ss.ap[:, 1:2], in_=ss.ap[:, 1:2], func=AF.Sqrt), [ss], [ss])
                P.op("dve", lambda: nc.vector.reciprocal(out=ss.ap[:, 1:2], in_=ss.ap[:, 1:2]), [ss], [ss])
                P.op("dve", lambda: nc.vector.scalar_tensor_tensor(out=h.ap, in0=xb.ap, scalar=ss.ap[:, 1:2], in1=A1b.ap, op0=OP.mult, op1=OP.mult), [xb, ss, A1b], [h])
                P.op("pool", lambda: nc.gpsimd.tensor_tensor(out=h.ap, in0=h.ap, in1=B1b.ap, op=OP.add), [h, B1b], [h])
                for kc in range(8):
                    P.op("pe", lambda: nc.tensor.transpose(P1.ap[:, kc * 128:(kc + 1) * 128], h.ap[:, kc * 128:(kc + 1) * 128], ident.ap), [h, ident], [P1])
                P.op("act", lambda: nc.scalar.copy(out=hb.ap.rearrange("p a b -> p (a b)"), in_=P1.ap), [P1], [hb])
                for o0 in range(0, O, 1024):
                    ow = min(1024, O - o0)
                    yb = yt[nyc % 2]
                    nyc += 1
                    for c0 in range(0, ow, 512):
                        cw = min(512, ow - c0)
                        for kc in range(8):
                            P.op("pe", lambda: nc.tensor.matmul(P2.ap[:, c0:c0 + cw], lhsT=hb.ap[:, kc, :], rhs=Wsb.ap[:, kc, o0 + c0:o0 + c0 + cw], start=(kc == 0), stop=(kc == 7)), [hb, Wsb], [P2])
                    if (nyc % 2) == 0:
                        P.op("act", lambda: nc.scalar.copy(out=yb.ap[:, 0:ow], in_=P2.ap[:, 0:ow]), [P2], [yb])
                    else:
                        P.op("dve", lambda: nc.vector.tensor_copy(out=yb.ap[:, 0:ow], in_=P2.ap[:, 0:ow]), [P2], [yb])
                    P.dma(View(Y, Y.ap[tsl, o0:o0 + ow]), View(yb, yb.ap[:, 0:ow]), q="act")
        P.barrier()
        P.finish([Y])
        print("phase A: ninst", P.ninst, "nwait", P.nwait)
    return nc


EPS = 1e-6
TWO_PI = 2.0 * math.pi
C1 = 6.28125
C2 = TWO_PI - C1


def rope_tables(P, nc, pos_i, posf, tmp, tmi, CS, SN, invf, sgn, HP):
    P.op("dve", lambda: nc.vector.tensor_copy(out=posf.ap, in_=pos_i.ap), [pos_i], [posf])
    P.op("dve", lambda: nc.vector.tensor_scalar(out=posf.ap, in0=posf.ap, scalar1=invf.ap[0:HP, 0:1], scalar2=None, op0=OP.mult), [posf, invf], [posf])
    P.op("dve", lambda: nc.vector.tensor_scalar(out=tmi.ap, in0=posf.ap, scalar1=1.0 / TWO_PI, scalar2=None, op0=OP.mult), [posf], [tmi])
    P.op("dve", lambda: nc.vector.tensor_copy(out=tmp.ap, in_=tmi.ap), [tmi], [tmp])
    P.op("dve", lambda: nc.vector.scalar_tensor_tensor(out=posf.ap, in0=tmp.ap, scalar=-C1, in1=posf.ap, op0=OP.mult, op1=OP.add), [tmp, posf], [posf])
    P.op("dve", lambda: nc.vector.scalar_tensor_tensor(out=posf.ap, in0=tmp.ap, scalar=-C2, in1=posf.ap, op0=OP.mult, op1=OP.add), [tmp, posf], [posf])
    P.op("dve", lambda: nc.vector.tensor_scalar(out=tmp.ap, in0=posf.ap, scalar1=math.pi, scalar2=-TWO_PI, op0=OP.is_gt, op1=OP.mult), [posf], [tmp])
    P.op("dve", lambda: nc.vector.tensor_tensor(out=posf.ap, in0=posf.ap, in1=tmp.ap, op=OP.add), [posf, tmp], [posf])
    P.op("dve", lambda: nc.vector.tensor_scalar(out=tmp.ap, in0=posf.ap, scalar1=-math.pi, scalar2=TWO_PI, op0=OP.is_lt, op1=OP.mult), [posf], [tmp])
    P.op("dve", lambda: nc.vector.tensor_tensor(out=posf.ap, in0=posf.ap, in1=tmp.ap, op=OP.add), [posf, tmp], [posf])
    P.op("dve", lambda: nc.vector.tensor_scalar(out=posf.ap, in0=posf.ap, scalar1=math.pi, scalar2=-math.pi, op0=OP.min, op1=OP.max), [posf], [posf])
    P.op("act", lambda: nc.scalar.activation(out=SN.ap, in_=posf.ap, func=AF.Sin), [posf], [SN])
    P.op("dve", lambda: nc.vector.tensor_scalar(out=SN.ap, in0=SN.ap, scalar1=sgn.ap[0:HP, 0:1], scalar2=None, op0=OP.mult), [SN, sgn], [SN])
    P.op("act", lambda: nc.scalar.activation(out=tmp.ap, in_=posf.ap, func=AF.Abs), [posf], [tmp])
    P.op("dve", lambda: nc.vector.tensor_scalar(out=tmp.ap, in0=tmp.ap, scalar1=-1.0, scalar2=math.pi / 2, op0=OP.mult, op1=OP.add), [tmp], [tmp])
    P.op("act", lambda: nc.scalar.activation(out=CS.ap, in_=tmp.ap, func=AF.Sin), [tmp], [CS])


def build_phase_b_mla(S):
    nc = bass.Bass("TRN2", target_bir_lowering=False)
    NG = S // 512
    NB = S // 128
    SCALE = (128 + 64) ** -0.5
    with ExitStack() as st:
        P = Prog(nc, st)
        cqT = P.dram("cqT", [512, S], F32, kind="ExternalInput")
        ckvT = P.dram("ckvT", [256, S], F32, kind="ExternalInput")
        krT = P.dram("krT", [64, S], F32, kind="ExternalInput")
        krTs = P.dram("krTs", [64, S], F32, kind="ExternalInput")
        pos = P.dram("pos", [1, S], I32, kind="ExternalInput")
        qn = P.dram("qn", [128, 4], F32, kind="ExternalInput")
        kvn = P.dram("kvn", [128, 2], F32, kind="ExternalInput")
        wuq_n = P.dram("wuq_n", [512, 128], F32, kind="ExternalInput")
        wuq_r = P.dram("wuq_r", [512, 64], F32, kind="ExternalInput")
        wuq_rs = P.dram("wuq_rs", [512, 64], F32, kind="ExternalInput")
        wukv_k = P.dram("wukv_k", [256, 128], F32, kind="ExternalInput")
        wukv_v = P.dram("wukv_v", [256, 128], F32, kind="ExternalInput")
        cst = P.dram("cst", [128, 2], F32, kind="ExternalInput")
        maskT_d = P.dram("maskT", [128, 128], F32, kind="ExternalInput")
        OT = P.dram("OT", [128, S], F32, kind="ExternalOutput")

        KTn = P.sb([128, S], BF16, "KTn")
        KTr = P.sb([64, S], BF16, "KTr")
        Vsb = P.sb([128, NB, 128], BF16, "Vsb")
        ones_f = P.sb([128, 128], F32, "ones_f")
        ones_b = P.sb([128, 128], BF16, "ones_b")
        maskT = P.sb([128, 128], BF16, "maskT_sb")
        cs = P.sb([128, 2], F32, "cst_sb")
        qn_sb = P.sb([128, 4], F32, "qn_sb")
        kvn_sb = P.sb([128, 2], F32, "kvn_sb")
        Wqn = P.sb([128, 4, 128], BF16, "Wqn")
        Wqr = P.sb([128, 4, 64], BF16, "Wqr")
        Wqrs = P.sb([128, 4, 64], BF16, "Wqrs")
        Wkk = P.sb([128, 2, 128], BF16, "Wkk")
        Wkv = P.sb([128, 2, 128], BF16, "Wkv")
        P.op("dve", lambda: nc.vector.memset(ones_f.ap, 1.0), [], [ones_f])
        P.op("dve", lambda: nc.vector.memset(ones_b.ap, 1.0), [], [ones_b])
        P.dma(maskT, maskT_d, q="pool")
        P.dma(cs, cst); P.dma(qn_sb, qn); P.dma(kvn_sb, kvn)
        P.dma(Wqn, View(wuq_n, wuq_n.ap.rearrange("(kc p) o -> p kc o", p=128)), q="pool")
        P.dma(Wqr, View(wuq_r, wuq_r.ap.rearrange("(kc p) o -> p kc o", p=128)), q="pool")
        P.dma(Wqrs, View(wuq_rs, wuq_rs.ap.rearrange("(kc p) o -> p kc o", p=128)), q="pool")
        P.dma(Wkk, View(wukv_k, wukv_k.ap.rearrange("(kc p) o -> p kc o", p=128)), q="pool")
        P.dma(Wkv, View(wukv_v, wukv_v.ap.rearrange("(kc p) o -> p kc o", p=128)), q="pool")
        invf = View(cs, cs.ap[:, 0:1]); sgn = View(cs, cs.ap[:, 1:2])

        cq_t = P.sb([128, 4, 512], F32, "cq_t")
        ckv_t = P.sb([128, 2, 512], F32, "ckv_t")
        sq = P.sb([128, 4, 512], F32, "sq")
        rs_q = P.sb([128, 512], F32, "rs_q")
        rs_k = P.sb([128, 512], F32, "rs_k")
        cqn = P.sb([128, 4, 512], BF16, "cqn")
        ckvn = P.sb([128, 2, 512], BF16, "ckvn")
        pos_i = P.sb([64, 512], I32, "pos_i")
        posf = P.sb([64, 512], F32, "posf")
        tmp = P.sb([64, 512], F32, "tmp")
        tmi = P.sb([64, 512], I32, "tmi")
        CS = P.sb([64, 512], F32, "CS")
        SN = P.sb([64, 512], F32, "SN")
        kr_t = P.sb([64, 512], F32, "kr_t")
        krs_t = P.sb([64, 512], F32, "krs_t")
        r1 = P.sb([64, 512], F32, "r1")
        r2 = P.sb([64, 512], F32, "r2")
        QTn = P.sb([128, 512], BF16, "QTn")
        QTr = P.sb([64, 512], BF16, "QTr")
        PT = [P.sb([128, 512], BF16, f"PT{i}") for i in range(2)]
        rec = P.sb([128, 512], F32, "rec")
        o_sb = P.sb([128, 512], F32, "o_sb")
        P1 = P.ps([128, 1024], F32, "P1")
        ST = [P.ps([128, 512], F32, f"ST{i}") for i in range(2)]
        OTp = P.ps([128, 512], F32, "OTp")
        DEN = P.ps([128, 512], F32, "DEN")

        cqT_v = cqT.ap.rearrange("(kc p) t -> p kc t", p=128)
        ckvT_v = ckvT.ap.rearrange("(kc p) t -> p kc t", p=128)
        it = 0
        for g in range(NG):
            gs = slice(g * 512, (g + 1) * 512)
            P.dma(cq_t, View(cqT, cqT_v[:, :, gs]), q="sp")
            P.dma(ckv_t, View(ckvT, ckvT_v[:, :, gs]), q="act")
            P.dma(kr_t, View(krT, krT.ap[:, gs]), q="sp")
            P.dma(krs_t, View(krTs, krTs.ap[:, gs]), q="act")
            P.dma(pos_i, View(pos, pos.ap[0:1, gs].to_broadcast([64, 512])), q="sp")
            rope_tables(P, nc, pos_i, posf, tmp, tmi, CS, SN, invf, sgn, 64)
            P.op("act", lambda: nc.scalar.activation(out=sq.ap, in_=cq_t.ap, func=AF.Square), [cq_t], [sq])
            for kc in range(4):
                P.op("pe", lambda: nc.tensor.matmul(P1.ap[:, 0:512], lhsT=ones_f.ap, rhs=sq.ap[:, kc, :], start=(kc == 0), stop=(kc == 3)), [ones_f, sq], [P1])
            P.op("dve", lambda: nc.vector.tensor_scalar(out=rs_q.ap, in0=P1.ap[:, 0:512], scalar1=1.0 / 512, scalar2=EPS, op0=OP.mult, op1=OP.add), [P1], [rs_q])
            P.op("act", lambda: nc.scalar.activation(out=rs_q.ap, in_=rs_q.ap, func=AF.Sqrt), [rs_q], [rs_q])
            P.op("dve", lambda: nc.vector.reciprocal(out=rs_q.ap, in_=rs_q.ap), [rs_q], [rs_q])
            for kc in range(4):
                P.op("dve", lambda: nc.vector.scalar_tensor_tensor(out=cqn.ap[:, kc, :], in0=cq_t.ap[:, kc, :], scalar=qn_sb.ap[:, kc:kc + 1], in1=rs_q.ap, op0=OP.mult, op1=OP.mult), [cq_t, qn_sb, rs_q], [cqn])
            P.op("act", lambda: nc.scalar.activation(out=sq.ap[:, 0:2, :], in_=ckv_t.ap, func=AF.Square), [ckv_t], [sq])
            for kc in range(2):
                P.op("pe", lambda: nc.tensor.matmul(P1.ap[:, 512:1024], lhsT=ones_f.ap, rhs=sq.ap[:, kc, :], start=(kc == 0), stop=(kc == 1)), [ones_f, sq], [P1])
            P.op("dve", lambda: nc.vector.tensor_scalar(out=rs_k.ap, in0=P1.ap[:, 512:1024], scalar1=1.0 / 256, scalar2=EPS, op0=OP.mult, op1=OP.add), [P1], [rs_k])
            P.op("act", lambda: nc.scalar.activation(out=rs_k.ap, in_=rs_k.ap, func=AF.Sqrt), [rs_k], [rs_k])
            P.op("dve", lambda: nc.vector.reciprocal(out=rs_k.ap, in_=rs_k.ap), [rs_k], [rs_k])
            for kc in range(2):
                P.op("dve", lambda: nc.vector.scalar_tensor_tensor(out=ckvn.ap[:, kc, :], in0=ckv_t.ap[:, kc, :], scalar=kvn_sb.ap[:, kc:kc + 1], in1=rs_k.ap, op0=OP.mult, op1=OP.mult), [ckv_t, kvn_sb, rs_k], [ckvn])
            for kc in range(4):
                P.op("pe", lambda: nc.tensor.matmul(P1.ap[:, 0:512], lhsT=Wqn.ap[:, kc, :], rhs=cqn.ap[:, kc, :], start=(kc == 0), stop=(kc == 3)), [Wqn, cqn], [P1])
            P.op("act", lambda: nc.scalar.activation(out=QTn.ap, in_=P1.ap[:, 0:512], func=AF.Copy, scale=SCALE), [P1], [QTn])
            for kc in range(4):
                P.op("pe", lambda: nc.tensor.matmul(P1.ap[0:64, 512:1024], lhsT=Wqr.ap[:, kc, :], rhs=cqn.ap[:, kc, :], start=(kc == 0), stop=(kc == 3)), [Wqr, cqn], [P1])
            P.op("dve", lambda: nc.vector.tensor_tensor(out=r1.ap, in0=P1.ap[0:64, 512:1024], in1=CS.ap, op=OP.mult), [P1, CS], [r1])
            for kc in range(4):
                P.op("pe", lambda: nc.tensor.matmul(P1.ap[0:64, 0:512], lhsT=Wqrs.ap[:, kc, :], rhs=cqn.ap[:, kc, :], start=(kc == 0), stop=(kc == 3)), [Wqrs, cqn], [P1])
            P.op("dve", lambda: nc.vector.tensor_tensor(out=r2.ap, in0=P1.ap[0:64, 0:512], in1=SN.ap, op=OP.mult), [P1, SN], [r2])
            P.op("dve", lambda: nc.vector.tensor_tensor(out=r1.ap, in0=r1.ap, in1=r2.ap, op=OP.add), [r1, r2], [r1])
            P.op("act", lambda: nc.scalar.activation(out=QTr.ap, in_=r1.ap, func=AF.Copy, scale=SCALE), [r1], [QTr])
            for kc in range(2):
                P.op("pe", lambda: nc.tensor.matmul(P1.ap[:, 512:1024], lhsT=Wkk.ap[:, kc, :], rhs=ckvn.ap[:, kc, :], start=(kc == 0), stop=(kc == 1)), [Wkk, ckvn], [P1])
            P.op("act", lambda: nc.scalar.copy(out=KTn.ap[:, gs], in_=P1.ap[:, 512:1024]), [P1], [KTn])
            P.op("dve", lambda: nc.vector.tensor_tensor(out=r1.ap, in0=kr_t.ap, in1=CS.ap, op=OP.mult), [kr_t, CS], [r1])
            P.op("dve", lambda: nc.vector.tensor_tensor(out=r2.ap, in0=krs_t.ap, in1=SN.ap, op=OP.mult), [krs_t, SN], [r2])
            P.op("dve", lambda: nc.vector.tensor_tensor(out=KTr.ap[:, gs], in0=r1.ap, in1=r2.ap, op=OP.add), [r1, r2], [KTr])
            for tt in range(4):
                for kc in range(2):
                    P.op("pe", lambda: nc.tensor.matmul(P1.ap[:, tt * 128:(tt + 1) * 128], lhsT=ckvn.ap[:, kc, tt * 128:(tt + 1) * 128], rhs=Wkv.ap[:, kc, :], start=(kc == 0), stop=(kc == 1)), [ckvn, Wkv], [P1])
            P.op("act", lambda: nc.scalar.copy(out=Vsb.ap[:, g * 4:(g + 1) * 4, :].rearrange("p a b -> p (a b)"), in_=P1.ap[:, 0:512]), [P1], [Vsb])
            nj = 4 * g + 4

            def S_(j):
                b = (it + j) % 2
                c0 = max(0, j - 4 * g) * 128
                ks = slice(j * 128, (j + 1) * 128)
                P.op("pe", lambda: nc.tensor.matmul(ST[b].ap[:, c0:512], lhsT=KTn.ap[:, ks], rhs=QTn.ap[:, c0:512], start=True, stop=False), [KTn, QTn], [ST[b]])
                P.op("pe", lambda: nc.tensor.matmul(ST[b].ap[:, c0:512], lhsT=KTr.ap[:, ks], rhs=QTr.ap[:, c0:512], start=False, stop=True), [KTr, QTr], [ST[b]])

            def EPV_(j):
                b = (it + j) % 2
                c0 = max(0, j - 4 * g) * 128
                P.op("act", lambda: nc.scalar.activation(out=PT[b].ap[:, c0:512], in_=ST[b].ap[:, c0:512], func=AF.Exp), [ST[b]], [PT[b]])
                if j >= 4 * g:
                    P.op("pool", lambda: nc.gpsimd.tensor_tensor(out=PT[b].ap[:, c0:c0 + 128], in0=PT[b].ap[:, c0:c0 + 128], in1=maskT.ap, op=OP.mult), [PT[b], maskT], [PT[b]])
                P.op("pe", lambda: nc.tensor.matmul(OTp.ap[:, c0:512], lhsT=Vsb.ap[:, j, :], rhs=PT[b].ap[:, c0:512], start=(j == 0), stop=(j == nj - 1)), [Vsb, PT[b]], [OTp])
                P.op("pe", lambda: nc.tensor.matmul(DEN.ap[:, c0:512], lhsT=ones_b.ap, rhs=PT[b].ap[:, c0:512], start=(j == 0), stop=(j == nj - 1)), [ones_b, PT[b]], [DEN])

            S_(0)
            for j in range(nj):
                if j + 1 < nj:
                    S_(j + 1)
                EPV_(j)
            it += nj
            P.op("dve", lambda: nc.vector.reciprocal(out=rec.ap, in_=DEN.ap), [DEN], [rec])
            P.op("dve", lambda: nc.vector.tensor_tensor(out=o_sb.ap, in0=OTp.ap, in1=rec.ap, op=OP.mult), [OTp, rec], [o_sb])
            P.dma(View(OT, OT.ap[:, gs]), o_sb, q="sp")
        P.barrier()
        P.finish([OT])
        print("phase B mla: ninst", P.ninst, "nwait", P.nwait)
    return nc


BIG = 30000.0
NEG = -3.0e38


def moba_body(P, nc, S, D):
    NG = S // 512
    NB = S // 128
    NBLK = S // 256
    SCALE = 128 ** -0.5
    qT, qTs, kT, kTs, v, pos = D["qT"], D["qTs"], D["kT"], D["kTs"], D["v"], D["pos"]
    OT = D["OT"]
    KT = P.sb([128, S], BF16, "m_KT")
    Vsb = P.sb([128, NB, 128], BF16, "m_Vsb")
    KMT = P.sb([128, NBLK], F32, "m_KMT")
    SelT = P.sb([NBLK, NBLK, 128], BF16, "m_SelT")
    ones_b = P.sb([128, 128], BF16, "m_ones_b")
    maskT = P.sb([128, 128], BF16, "m_maskT")
    ident = P.sb([128, 128], F32, "m_ident")
    cs = P.sb([128, 2], F32, "m_cst")
    P.op("dve", lambda: nc.vector.memset(ones_b.ap, 1.0), [], [ones_b])
    P.dma(maskT, D["maskT"], q="pool")
    P.dma(SelT, View(D["selT"], D["selT"].ap.rearrange("n (m k) -> n m k", k=128)), q="pool")
    P.dma(ident, D["ident"])
    P.dma(cs, D["cst"])
    v_v = v.ap.rearrange("(b p) d -> p b d", p=128)
    for b0 in range(0, NB, 16):
        b1 = min(NB, b0 + 16)
        P.dma(View(Vsb, Vsb.ap[:, b0:b1, :]), View(v, v_v[:, b0:b1, :]), q="pool")
    invf = View(cs, cs.ap[:, 0:1]); sgn = View(cs, cs.ap[:, 1:2])
    q_t = P.sb([128, 512], F32, "m_q_t"); qs_t = P.sb([128, 512], F32, "m_qs_t")
    k_t = P.sb([128, 512], F32, "m_k_t"); ks_t = P.sb([128, 512], F32, "m_ks_t")
    pos_i = P.sb([128, 512], I32, "m_pos_i"); posf = P.sb([128, 512], F32, "m_posf")
    tmp = P.sb([128, 512], F32, "m_tmp"); tmi = P.sb([128, 512], I32, "m_tmi")
    CS = P.sb([128, 512], F32, "m_CS"); SN = P.sb([128, 512], F32, "m_SN")
    r1 = P.sb([128, 512], F32, "m_r1"); r2 = P.sb([128, 512], F32, "m_r2")
    QTf = P.sb([128, 512], F32, "m_QTf")
    QT = P.sb([128, 512], BF16, "m_QT")
    gate = P.sb([128, NBLK], F32, "m_gate")
    m8 = P.sb([128, 8], F32, "m_m8")
    pen = P.sb([128, NBLK], F32, "m_pen")
    PenT = P.sb([NBLK, 512], BF16, "m_PenT")
    PT = [P.sb([128, 512], BF16, f"m_PT{i}") for i in range(2)]
    rec = P.sb([128, 512], F32, "m_rec")
    o_sb = P.sb([128, 512], F32, "m_o_sb")
    P1 = P.ps([128, 512], F32, "m_P1")
    ST = [P.ps([128, 512], F32, f"m_ST{i}") for i in range(2)]
    OTp = P.ps([128, 512], F32, "m_OTp")
    DEN = P.ps([128, 512], F32, "m_DEN")
    it = 0
    for g in range(NG):
        gs = slice(g * 512, (g + 1) * 512)
        P.dma(q_t, View(qT, qT.ap[:, gs]), q="sp")
        P.dma(qs_t, View(qTs, qTs.ap[:, gs]), q="act")
        P.dma(k_t, View(kT, kT.ap[:, gs]), q="sp")
        P.dma(ks_t, View(kTs, kTs.ap[:, gs]), q="act")
        P.dma(pos_i, View(pos, pos.ap[0:1, gs].to_broadcast([128, 512])), q="sp")
        rope_tables(P, nc, pos_i, posf, tmp, tmi, CS, SN, invf, sgn, 128)
        P.op("dve", lambda: nc.vector.tensor_tensor(out=r1.ap, in0=q_t.ap, in1=CS.ap, op=OP.mult), [q_t, CS], [r1])
        P.op("pool", lambda: nc.gpsimd.tensor_tensor(out=r2.ap, in0=qs_t.ap, in1=SN.ap, op=OP.mult), [qs_t, SN], [r2])
        P.op("dve", lambda: nc.vector.tensor_tensor(out=QTf.ap, in0=r1.ap, in1=r2.ap, op=OP.add), [r1, r2], [QTf])
        P.op("act", lambda: nc.scalar.activation(out=QT.ap, in_=QTf.ap, func=AF.Copy, scale=SCALE), [QTf], [QT])
        P.op("dve", lambda: nc.vector.tensor_tensor(out=r1.ap, in0=k_t.ap, in1=CS.ap, op=OP.mult), [k_t, CS], [r1])
        P.op("pool", lambda: nc.gpsimd.tensor_tensor(out=r2.ap, in0=ks_t.ap, in1=SN.ap, op=OP.mult), [ks_t, SN], [r2])
        P.op("dve", lambda: nc.vector.tensor_tensor(out=r1.ap, in0=r1.ap, in1=r2.ap, op=OP.add), [r1, r2], [r1])
        P.op("act", lambda: nc.scalar.copy(out=KT.ap[:, gs], in_=r1.ap), [r1], [KT])
        P.op("dve", lambda: nc.vector.tensor_reduce(out=KMT.ap[:, 2 * g:2 * g + 2], in_=r1.ap.rearrange("p (n k) -> p n k", k=256), axis=AX.X, op=OP.add), [r1], [KMT])
        P.op("dve", lambda: nc.vector.tensor_scalar(out=KMT.ap[:, 2 * g:2 * g + 2], in0=KMT.ap[:, 2 * g:2 * g + 2], scalar1=1.0 / 256, scalar2=None, op0=OP.mult), [KMT], [KMT])
        for qb in range(4):
            own = 2 * g + qb // 2
            P.op("dve", lambda: nc.vector.memset(gate.ap, NEG), [], [gate])
            if own > 0:
                P.op("pe", lambda: nc.tensor.matmul(P1.ap[:, 0:own], lhsT=QTf.ap[:, qb * 128:(qb + 1) * 128], rhs=KMT.ap[:, 0:own], start=True, stop=True), [QTf, KMT], [P1])
                P.op("dve", lambda: nc.vector.tensor_copy(out=gate.ap[:, 0:own], in_=P1.ap[:, 0:own]), [P1], [gate])
            P.op("dve", lambda: nc.vector.max(out=m8.ap, in_=gate.ap), [gate], [m8])
            P.op("dve", lambda: nc.vector.tensor_scalar(out=m8.ap[:, 2:3], in0=m8.ap[:, 2:3], scalar1=-1.0e30, scalar2=None, op0=OP.max), [m8], [m8])
            P.op("dve", lambda: nc.vector.tensor_scalar(out=pen.ap, in0=gate.ap, scalar1=m8.ap[:, 2:3], scalar2=BIG, op0=OP.is_ge, op1=OP.mult), [gate, m8], [pen])
            P.op("dve", lambda: nc.vector.tensor_scalar(out=pen.ap, in0=pen.ap, scalar1=-BIG, scalar2=None, op0=OP.add), [pen], [pen])
            P.op("dve", lambda: nc.vector.memset(pen.ap[:, own:own + 1], 0.0), [], [pen])
            P.op("pe", lambda: nc.tensor.transpose(P1.ap[0:NBLK, 128:256], pen.ap, ident.ap), [pen, ident], [P1])
            P.op("act", lambda: nc.scalar.copy(out=PenT.ap[:, qb * 128:(qb + 1) * 128], in_=P1.ap[0:NBLK, 128:256]), [P1], [PenT])
        nj = 4 * g + 4

        def S_(j):
            b = (it + j) % 2
            c0 = max(0, j - 4 * g) * 128
            ks = slice(j * 128, (j + 1) * 128)
            n = j // 2
            P.op("pe", lambda: nc.tensor.matmul(ST[b].ap[:, c0:512], lhsT=KT.ap[:, ks], rhs=QT.ap[:, c0:512], start=True, stop=False), [KT, QT], [ST[b]])
            P.op("pe", lambda: nc.tensor.matmul(ST[b].ap[:, c0:512], lhsT=SelT.ap[:, n, :], rhs=PenT.ap[:, c0:512], start=False, stop=True), [SelT, PenT], [ST[b]])

        def EPV_(j):
            b = (it + j) % 2
            c0 = max(0, j - 4 * g) * 128
            P.op("act", lambda: nc.scalar.activation(out=PT[b].ap[:, c0:512], in_=ST[b].ap[:, c0:512], func=AF.Exp), [ST[b]], [PT[b]])
            if j >= 4 * g:
                P.op("pool", lambda: nc.gpsimd.tensor_tensor(out=PT[b].ap[:, c0:c0 + 128], in0=PT[b].ap[:, c0:c0 + 128], in1=maskT.ap, op=OP.mult), [PT[b], maskT], [PT[b]])
            P.op("pe", lambda: nc.tensor.matmul(OTp.ap[:, c0:512], lhsT=Vsb.ap[:, j, :], rhs=PT[b].ap[:, c0:512], start=(j == 0), stop=(j == nj - 1)), [Vsb, PT[b]], [OTp])
            P.op("pe", lambda: nc.tensor.matmul(DEN.ap[:, c0:512], lhsT=ones_b.ap, rhs=PT[b].ap[:, c0:512], start=(j == 0), stop=(j == nj - 1)), [ones_b, PT[b]], [DEN])

        S_(0)
        for j in range(nj):
            if j + 1 < nj:
                S_(j + 1)
            EPV_(j)
        it += nj
        P.op("dve", lambda: nc.vector.reciprocal(out=rec.ap, in_=DEN.ap), [DEN], [rec])
        P.op("dve", lambda: nc.vector.tensor_tensor(out=o_sb.ap, in0=OTp.ap, in1=rec.ap, op=OP.mult), [OTp, rec], [o_sb])
        P.dma(View(OT, OT.ap[:, gs]), o_sb, q="sp")


def build_phase_b_moba(S):
    nc = bass.Bass("TRN2", target_bir_lowering=False)
    NBLK = S // 256
    with ExitStack() as st:
        P = Prog(nc, st)
        D = {}
        for nm in ("qT", "qTs", "kT", "kTs"):
            D[nm] = P.dram(nm, [128, S], F32, kind="ExternalInput")
        D["v"] = P.dram("v", [S, 128], F32, kind="ExternalInput")
        D["pos"] = P.dram("pos", [1, S], I32, kind="ExternalInput")
        D["cst"] = P.dram("cst", [128, 2], F32, kind="ExternalInput")
        D["maskT"] = P.dram("maskT", [128, 128], F32, kind="ExternalInput")
        D["selT"] = P.dram("selT", [NBLK, NBLK * 128], F32, kind="ExternalInput")
        D["ident"] = P.dram("ident", [128, 128], F32, kind="ExternalInput")
        D["OT"] = P.dram("OT", [128, S], F32, kind="ExternalOutput")
        with P.scope():
            moba_body(P, nc, S, D)
        P.barrier()
        P.finish([D["OT"]])
        print("phase B moba: ninst", P.ninst, "nwait", P.nwait)
    return nc


EPS = 1e-6


def gdn_body(P, nc, S, D):
    NG = S // 512
    NCH = S // 128
    op = P.op
    V = nc.vector
    A = nc.scalar
    PE = nc.tensor
    ident = P.sb([128, 128], F32, "g_ident"); tri = P.sb([128, 128], F32, "g_tri")
    maskS = P.sb([128, 128], F32, "g_maskS"); maskI = P.sb([128, 128], F32, "g_maskI")
    ones_f = P.sb([128, 128], F32, "g_ones")
    cw = P.sb([128, 12], F32, "g_cw"); hc = P.sb([128, 2], F32, "g_hc"); onb = P.sb([128, 128], F32, "g_onb")
    ba = P.sb([128, NCH, 2], F32, "g_ba")
    beta = P.sb([128, NCH], F32, "g_beta"); nbeta = P.sb([128, NCH], F32, "g_nbeta"); graw = P.sb([128, NCH], F32, "g_graw")
    tmpc = P.sb([128, NCH], F32, "g_tmpc")
    for b_, d_ in ((ident, "ident"), (tri, "tri"), (maskS, "maskS"), (maskI, "maskI"), (cw, "cw"), (hc, "hc"), (onb, "onorm")):
        P.dma(b_, D[d_], q="sp")
    P.dma(ba, D["ba"], q="act")
    op("dve", lambda: V.memset(ones_f.ap, 1.0), [], [ones_f])
    op("act", lambda: A.activation(out=beta.ap, in_=ba.ap[:, :, 0], func=AF.Sigmoid), [ba], [beta])
    op("dve", lambda: V.tensor_scalar(out=nbeta.ap, in0=beta.ap, scalar1=-1.0, scalar2=None, op0=OP.mult), [beta], [nbeta])
    op("act", lambda: A.activation(out=tmpc.ap, in_=ba.ap[:, :, 1], func=AF.Exp, bias=hc.ap[:, 1:2]), [ba, hc], [tmpc])
    op("act", lambda: A.activation(out=tmpc.ap, in_=tmpc.ap, func=AF.Ln, bias=ones_f.ap[:, 0:1]), [tmpc, ones_f], [tmpc])
    op("act", lambda: A.activation(out=hc.ap[:, 0:1], in_=hc.ap[:, 0:1], func=AF.Exp), [hc], [hc])
    op("dve", lambda: V.tensor_scalar(out=graw.ap, in0=tmpc.ap, scalar1=hc.ap[:, 0:1], scalar2=-1.0, op0=OP.mult, op1=OP.mult), [tmpc, hc], [graw])

    St = P.sb([128, 128], F32, "g_S")
    op("dve", lambda: V.memset(St.ap, 0.0), [], [St])
    xin = [P.sb([128, 515], F32, f"g_xin{i}") for i in range(3)]
    yc = [P.sb([128, 512], F32, f"g_yc{i}") for i in range(3)]
    sq = P.sb([128, 512], F32, "g_sq")
    rs = P.sb([128, 512], F32, "g_rs")
    names = ["GrB", "Gm_sb", "t1", "Dm", "EG", "Am", "Bm", "A2", "B2", "Ym", "qk", "qkT", "kbg", "kd", "vb", "wT", "u", "qdT", "vnew", "zt", "ot", "o2"]
    W = {n: P.sb([128, 128], F32, "g_" + n) for n in names}
    cols = P.sb([128, 8], F32, "g_cols")
    PG = P.ps([128, 512], F32, "g_PG"); PTr = P.ps([128, 512], F32, "g_PTr"); PD = P.ps([128, 512], F32, "g_PD")
    PW = P.ps([128, 512], F32, "g_PW"); PS = P.ps([128, 512], F32, "g_PS"); PC = P.ps([128, 512], F32, "g_PC")
    c_ = lambda i: slice(i * 128, (i + 1) * 128)
    xs = (D["xq"], D["xk"], D["xv"])
    z_v = D["z"].ap.rearrange("(n p) d -> n p d", p=128)
    oa_v = D["OA"].ap.rearrange("(n p) d -> n p d", p=128)
    for g in range(NG):
        for i in range(3):
            P.dma(xin[i], View(xs[i], xs[i].ap[:, g * 512:g * 512 + 515]), q="sp" if i != 1 else "act")
            op("dve", lambda: V.tensor_scalar(out=yc[i].ap, in0=xin[i].ap[:, 0:512], scalar1=cw.ap[:, 4 * i:4 * i + 1], scalar2=None, op0=OP.mult), [xin[i], cw], [yc[i]])
            for j in range(1, 4):
                op("dve", lambda: V.scalar_tensor_tensor(out=yc[i].ap, in0=xin[i].ap[:, j:j + 512], scalar=cw.ap[:, 4 * i + j:4 * i + j + 1], in1=yc[i].ap, op0=OP.mult, op1=OP.add), [xin[i], cw, yc[i]], [yc[i]])
            op("act", lambda: A.activation(out=yc[i].ap, in_=yc[i].ap, func=AF.Silu), [yc[i]], [yc[i]])
            if i < 2:
                op("act", lambda: A.activation(out=sq.ap, in_=yc[i].ap, func=AF.Square), [yc[i]], [sq])
                op("pe", lambda: PE.matmul(PC.ap, lhsT=ones_f.ap, rhs=sq.ap, start=True, stop=True), [ones_f, sq], [PC])
                op("dve", lambda: V.tensor_scalar(out=rs.ap, in0=PC.ap, scalar1=EPS, scalar2=None, op0=OP.add), [PC], [rs])
                op("act", lambda: A.activation(out=rs.ap, in_=rs.ap, func=AF.Sqrt), [rs], [rs])
                op("dve", lambda: V.reciprocal(out=rs.ap, in_=rs.ap), [rs], [rs])
                sc_ = (128 ** -0.5) if i == 0 else 1.0
                op("dve", lambda: V.scalar_tensor_tensor(out=yc[i].ap, in0=yc[i].ap, scalar=sc_, in1=rs.ap, op0=OP.mult, op1=OP.mult), [yc[i], rs], [yc[i]])
        for cc in range(4):
            n = g * 4 + cc
            qTn = View(yc[0], yc[0].ap[:, c_(cc)]); kTn = View(yc[1], yc[1].ap[:, c_(cc)]); vTn = View(yc[2], yc[2].ap[:, c_(cc)])
            gr = graw.ap[:, n:n + 1]; be = beta.ap[:, n:n + 1]; nbe = nbeta.ap[:, n:n + 1]
            op("dve", lambda: V.tensor_scalar(out=W["GrB"].ap, in0=ones_f.ap, scalar1=gr, scalar2=None, op0=OP.mult), [ones_f, graw], [W["GrB"]])
            op("pe", lambda: PE.matmul(PG.ap[:, c_(0)], lhsT=W["GrB"].ap, rhs=tri.ap, start=True, stop=True), [W["GrB"], tri], [PG])
            op("pe", lambda: PE.matmul(PG.ap[:, 128:129], lhsT=tri.ap, rhs=gr, start=True, stop=True), [tri, graw], [PG])
            op("pe", lambda: PE.matmul(PG.ap[:, c_(2)], lhsT=kTn.ap, rhs=kTn.ap, start=True, stop=True), [kTn], [PG])
            op("pe", lambda: PE.matmul(PG.ap[:, c_(3)], lhsT=qTn.ap, rhs=kTn.ap, start=True, stop=True), [qTn, kTn], [PG])
            op("act", lambda: A.copy(out=W["Gm_sb"].ap, in_=PG.ap[:, c_(0)]), [PG], [W["Gm_sb"]])
            op("act", lambda: A.copy(out=cols.ap[:, 0:1], in_=PG.ap[:, 128:129]), [PG], [cols])
            op("dve", lambda: V.tensor_scalar(out=W["t1"].ap, in0=W["Gm_sb"].ap, scalar1=cols.ap[:, 0:1], scalar2=0.0, op0=OP.subtract, op1=OP.max), [W["Gm_sb"], cols], [W["t1"]])
            op("act", lambda: A.activation(out=W["Dm"].ap, in_=W["t1"].ap, func=AF.Exp, scale=-1.0), [W["t1"]], [W["Dm"]])
            op("act", lambda: A.activation(out=W["EG"].ap, in_=W["Gm_sb"].ap, func=AF.Exp), [W["Gm_sb"]], [W["EG"]])
            op("act", lambda: A.activation(out=cols.ap[:, 1:2], in_=cols.ap[:, 0:1], func=AF.Exp), [cols], [cols])
            op("act", lambda: A.activation(out=cols.ap[:, 2:3], in_=cols.ap[:, 0:1], func=AF.Exp, scale=-1.0, bias=W["Gm_sb"].ap[:, 127:128]), [cols, W["Gm_sb"]], [cols])
            op("dve", lambda: V.tensor_tensor(out=cols.ap[:, 3:4], in0=cols.ap[:, 1:2], in1=be, op=OP.mult), [cols, beta], [cols])
            op("dve", lambda: V.tensor_tensor(out=W["t1"].ap, in0=PG.ap[:, c_(2)], in1=W["Dm"].ap, op=OP.mult), [PG, W["Dm"]], [W["t1"]])
            op("dve", lambda: V.scalar_tensor_tensor(out=W["Am"].ap, in0=W["t1"].ap, scalar=nbe, in1=maskS.ap, op0=OP.mult, op1=OP.mult), [W["t1"], nbeta, maskS], [W["Am"]])
            op("dve", lambda: V.tensor_tensor(out=W["t1"].ap, in0=PG.ap[:, c_(3)], in1=W["Dm"].ap, op=OP.mult), [PG, W["Dm"]], [W["t1"]])
            op("dve", lambda: V.tensor_tensor(out=W["qk"].ap, in0=W["t1"].ap, in1=maskI.ap, op=OP.mult), [W["t1"], maskI], [W["qk"]])
            op("pe", lambda: PE.transpose(PTr.ap[:, c_(0)], W["Am"].ap, ident.ap), [W["Am"], ident], [PTr])
            op("pe", lambda: PE.transpose(PTr.ap[:, c_(1)], W["qk"].ap, ident.ap), [W["qk"], ident], [PTr])
            op("pe", lambda: PE.transpose(PTr.ap[:, c_(2)], kTn.ap, ident.ap), [kTn, ident], [PTr])
            op("pe", lambda: PE.transpose(PTr.ap[:, c_(3)], vTn.ap, ident.ap), [vTn, ident], [PTr])
            op("act", lambda: A.copy(out=W["Bm"].ap, in_=PTr.ap[:, c_(0)]), [PTr], [W["Bm"]])
            op("act", lambda: A.copy(out=W["qkT"].ap, in_=PTr.ap[:, c_(1)]), [PTr], [W["qkT"]])
            op("act", lambda: A.activation(out=W["kbg"].ap, in_=PTr.ap[:, c_(2)], func=AF.Copy, scale=cols.ap[:, 3:4]), [PTr, cols], [W["kbg"]])
            op("act", lambda: A.activation(out=W["kd"].ap, in_=PTr.ap[:, c_(2)], func=AF.Copy, scale=cols.ap[:, 2:3]), [PTr, cols], [W["kd"]])
            op("act", lambda: A.activation(out=W["vb"].ap, in_=PTr.ap[:, c_(3)], func=AF.Copy, scale=be), [PTr, beta], [W["vb"]])
            op("dve", lambda: V.tensor_tensor(out=W["Ym"].ap, in0=W["Bm"].ap, in1=ident.ap, op=OP.add), [W["Bm"], ident], [W["Ym"]])
            Ac, Bc, An, Bn = W["Am"], W["Bm"], W["A2"], W["B2"]
            for stp in range(6):
                op("pe", lambda: PE.matmul(PD.ap[:, c_(0)], lhsT=Bc.ap, rhs=Ac.ap, start=True, stop=True), [Bc, Ac], [PD])
                if stp < 5:
                    op("pe", lambda: PE.matmul(PD.ap[:, c_(1)], lhsT=Ac.ap, rhs=Bc.ap, start=True, stop=True), [Ac, Bc], [PD])
                op("act", lambda: A.copy(out=An.ap, in_=PD.ap[:, c_(0)]), [PD], [An])
                if stp < 5:
                    op("act", lambda: A.copy(out=Bn.ap, in_=PD.ap[:, c_(1)]), [PD], [Bn])
                op("pe", lambda: PE.matmul(PD.ap[:, c_(2)], lhsT=An.ap, rhs=W["Ym"].ap, start=True, stop=True), [An, W["Ym"]], [PD])
                op("dve", lambda: V.tensor_tensor(out=W["Ym"].ap, in0=W["Ym"].ap, in1=PD.ap[:, c_(2)], op=OP.add), [W["Ym"], PD], [W["Ym"]])
                Ac, Bc, An, Bn = An, Bn, Ac, Bc
            op("pe", lambda: PE.matmul(PW.ap[:, c_(0)], lhsT=W["kbg"].ap, rhs=W["Ym"].ap, start=True, stop=True), [W["kbg"], W["Ym"]], [PW])
            op("pe", lambda: PE.matmul(PW.ap[:, c_(1)], lhsT=W["Ym"].ap, rhs=W["vb"].ap, start=True, stop=True), [W["Ym"], W["vb"]], [PW])
            op("act", lambda: A.copy(out=W["wT"].ap, in_=PW.ap[:, c_(0)]), [PW], [W["wT"]])
            op("act", lambda: A.copy(out=W["u"].ap, in_=PW.ap[:, c_(1)]), [PW], [W["u"]])
            op("dve", lambda: V.tensor_tensor(out=W["qdT"].ap, in0=qTn.ap, in1=W["EG"].ap, op=OP.mult), [qTn, W["EG"]], [W["qdT"]])
            op("pe", lambda: PE.matmul(PS.ap[:, c_(0)], lhsT=W["wT"].ap, rhs=St.ap, start=True, stop=True), [W["wT"], St], [PS])
            op("dve", lambda: V.tensor_tensor(out=W["vnew"].ap, in0=W["u"].ap, in1=PS.ap[:, c_(0)], op=OP.subtract), [W["u"], PS], [W["vnew"]])
            op("pe", lambda: PE.matmul(PS.ap[:, c_(1)], lhsT=W["qdT"].ap, rhs=St.ap, start=True, stop=False), [W["qdT"], St], [PS])
            op("pe", lambda: PE.matmul(PS.ap[:, c_(1)], lhsT=W["qkT"].ap, rhs=W["vnew"].ap, start=False, stop=True), [W["qkT"], W["vnew"]], [PS])
            op("pe", lambda: PE.matmul(PS.ap[:, c_(2)], lhsT=W["kd"].ap, rhs=W["vnew"].ap, start=True, stop=True), [W["kd"], W["vnew"]], [PS])
            op("act", lambda: A.copy(out=W["ot"].ap, in_=PS.ap[:, c_(1)]), [PS], [W["ot"]])
            op("dve", lambda: V.scalar_tensor_tensor(out=St.ap, in0=St.ap, scalar=W["EG"].ap[:, 127:128], in1=PS.ap[:, c_(2)], op0=OP.mult, op1=OP.add), [St, W["EG"], PS], [St])
            P.dma(W["zt"], View(D["z"], z_v[n]), q="act")
            op("act", lambda: A.activation(out=W["o2"].ap, in_=W["ot"].ap, func=AF.Square, accum_out=cols.ap[:, 4:5]), [W["ot"]], [W["o2"], cols])
            op("dve", lambda: V.tensor_scalar(out=cols.ap[:, 5:6], in0=cols.ap[:, 4:5], scalar1=1.0 / 128, scalar2=EPS, op0=OP.mult, op1=OP.add), [cols], [cols])
            op("act", lambda: A.activation(out=cols.ap[:, 5:6], in_=cols.ap[:, 5:6], func=AF.Sqrt), [cols], [cols])
            op("dve", lambda: V.reciprocal(out=cols.ap[:, 5:6], in_=cols.ap[:, 5:6]), [cols], [cols])
            op("act", lambda: A.activation(out=W["zt"].ap, in_=W["zt"].ap, func=AF.Silu), [W["zt"]], [W["zt"]])
            op("dve", lambda: V.scalar_tensor_tensor(out=W["o2"].ap, in0=W["ot"].ap, scalar=cols.ap[:, 5:6], in1=onb.ap, op0=OP.mult, op1=OP.mult), [W["ot"], cols, onb], [W["o2"]])
            op("dve", lambda: V.tensor_tensor(out=W["o2"].ap, in0=W["o2"].ap, in1=W["zt"].ap, op=OP.mult), [W["o2"], W["zt"]], [W["o2"]])
            P.dma(View(D["OA"], oa_v[n]), W["o2"], q="sp")


def build_phase_b_gdn(S):
    nc = bass.Bass("TRN2", target_bir_lowering=False)
    with ExitStack() as st:
        P = Prog(nc, st)
        D = {}
        for nm in ("xq", "xk", "xv"):
            D[nm] = P.dram(nm, [128, S + 3], F32, kind="ExternalInput")
        D["cw"] = P.dram("cw", [128, 12], F32, kind="ExternalInput")
        D["z"] = P.dram("z", [S, 128], F32, kind="ExternalInput")
        D["ba"] = P.dram("ba", [128, S // 128, 2], F32, kind="ExternalInput")
        D["hc"] = P.dram("hc", [128, 2], F32, kind="ExternalInput")
        D["onorm"] = P.dram("onorm", [128, 128], F32, kind="ExternalInput")
        for nm in ("ident", "tri", "maskS", "maskI"):
            D[nm] = P.dram(nm, [128, 128], F32, kind="ExternalInput")
        D["OA"] = P.dram("OA", [S, 128], F32, kind="ExternalOutput")
        with P.scope():
            gdn_body(P, nc, S, D)
        P.barrier()
        P.finish([D["OA"]])
        print("phase B gdn: ninst", P.ninst, "nwait", P.nwait)
    return nc


EPS = 1e-6
NEG = -3.0e38


def bcast_rows(P, nc, ones_row, row, dst, ps, ncols):
    for c0 in range(0, ncols, 512):
        P.op("pe", lambda: nc.tensor.matmul(ps.ap[:, 0:512], lhsT=ones_row.ap[0:1, 0:128], rhs=row.ap[0:1, c0:c0 + 512], start=True, stop=True), [ones_row, row], [ps])
        P.op("act", lambda: nc.scalar.copy(out=dst.ap[:, c0:c0 + 512], in_=ps.ap[:, 0:512]), [ps], [dst])


def build_phase_c(T, final=False, NCH=32):
    nc = bass.Bass("TRN2", target_bir_lowering=False)
    NT = T // 128
    with ExitStack() as st:
        P = Prog(nc, st)
        x = P.dram("x", [T, 1024], F32, kind="ExternalInput")
        oT = P.dram("oT", [1024, T], F32, kind="ExternalInput")
        w_out = P.dram("w_out", [1024, 1024], F32, kind="ExternalInput")
        cT = P.dram("cT", [128, 8], F32, kind="ExternalInput")
        modw = P.dram("modw", [1024, 4096], F32, kind="ExternalInput")
        modb = P.dram("modb", [1, 4096], F32, kind="ExternalInput")
        nrm = P.dram("nrm", [1, 1024], F32, kind="ExternalInput")
        fnrm = P.dram("fnrm", [1, 1024], F32, kind="ExternalInput")
        w_q = P.dram("w_q", [1024, 2048], F32, kind="ExternalInput")
        skT = P.dram("skT", [128, 16, 128], F32, kind="ExternalInput")
        uT = P.dram("uT", [1024, NCH * 512], F32, kind="ExternalInput")
        vv = P.dram("vv", [NCH * 512, 1024], F32, kind="ExternalInput")
        ident_d = P.dram("ident", [128, 128], F32, kind="ExternalInput")
        xo = P.dram("xo", [T, 1024], F32, kind="ExternalOutput")
        sc_s = P.dram("sc_s", [NT, 128, 3, 8, 128], F32)
        sc_x1 = P.dram("sc_x1", [T, 1024], F32)
        sc_h2T = P.dram("sc_h2T", [NT, 128, 8, 128], BF16)
        uT_bf = P.dram("uT_bf", [1024, NCH * 512], BF16)
        vv_bf = P.dram("vv_bf", [NCH * 512, 1024], BF16)

        ident = P.sb([128, 128], F32, "ident_sb")
        identb = P.sb([128, 128], BF16, "identb")
        ones_row = P.sb([1, 128], F32, "ones_row")
        G1b = P.sb([128, 1024], F32, "G1b")
        A2b = P.sb([128, 1024], F32, "A2b")
        B2b = P.sb([128, 1024], F32, "B2b")
        G2b = P.sb([128, 1024], F32, "G2b")
        FNb = P.sb([128, 1024], F32, "FNb")
        P.dma(ident, ident_d)
        P.op("dve", lambda: nc.vector.tensor_copy(out=identb.ap, in_=ident.ap), [ident], [identb])
        P.op("dve", lambda: nc.vector.memset(ones_row.ap, 1.0), [], [ones_row])

        for i in range(8):
            P.dma(View(uT_bf, uT_bf.ap[i * 128:(i + 1) * 128, :]), View(uT, uT.ap[i * 128:(i + 1) * 128, :]), q="pool")
        NV = NCH * 512 // 8
        for i in range(8):
            P.dma(View(vv_bf, vv_bf.ap[i * NV:(i + 1) * NV, :]), View(vv, vv.ap[i * NV:(i + 1) * NV, :]), q="pool")
        with P.scope():
            P1 = P.ps([128, 1024], F32, "P1a")
            cond = P.sb([128, 8], F32, "cond")
            modrow = P.sb([1, 4096], F32, "modrow")
            mb = P.sb([1, 4096], F32, "mb")
            nr = P.sb([1, 1024], F32, "nr")
            fn = P.sb([1, 1024], F32, "fn")
            a2row = P.sb([1, 1024], F32, "a2row")
            mwc = [P.sb([128, 8, 512], F32, f"mwc{i}") for i in range(2)]
            P.dma(cond, cT)
            P.dma(mb, modb, q="act")
            P.dma(nr, nrm, q="act")
            P.dma(fn, fnrm, q="act")
            P.op("act", lambda: nc.scalar.activation(out=cond.ap, in_=cond.ap, func=AF.Silu), [cond], [cond])
            mw_v = modw.ap.rearrange("(kc p) o -> p kc o", p=128)
            for ch in range(8):
                buf = mwc[ch % 2]
                P.dma(buf, View(modw, mw_v[:, :, ch * 512:(ch + 1) * 512]), q="sp" if ch % 2 == 0 else "act")
                for kc in range(8):
                    P.op("pe", lambda: nc.tensor.matmul(P1.ap[0:1, 0:512], lhsT=cond.ap[:, kc:kc + 1], rhs=buf.ap[:, kc, :], start=(kc == 0), stop=(kc == 7)), [cond, buf], [P1])
                P.op("dve", lambda: nc.vector.tensor_tensor(out=modrow.ap[0:1, ch * 512:(ch + 1) * 512], in0=P1.ap[0:1, 0:512], in1=mb.ap[0:1, ch * 512:(ch + 1) * 512], op=OP.add), [P1, mb], [modrow])
            P.op("dve", lambda: nc.vector.scalar_tensor_tensor(out=a2row.ap, in0=modrow.ap[0:1, 2048:3072], scalar=1.0, in1=nr.ap, op0=OP.add, op1=OP.mult), [modrow, nr], [a2row])
            g1row = View(modrow, modrow.ap[0:1, 0:1024])
            b2row = View(modrow, modrow.ap[0:1, 1024:2048])
            g2row = View(modrow, modrow.ap[0:1, 3072:4096])
            for (row, dst) in ((g1row, G1b), (a2row, A2b), (b2row, B2b), (g2row, G2b), (fn, FNb)):
                for c0 in range(0, 1024, 512):
                    rap = _ap(row)[0:1, c0:c0 + 512]
                    P.op("pe", lambda: nc.tensor.matmul(P1.ap[:, 0:512], lhsT=ones_row.ap[0:1, 0:128], rhs=rap, start=True, stop=True), [ones_row, row], [P1])
                    P.op("act", lambda: nc.scalar.copy(out=dst.ap[:, c0:c0 + 512], in_=P1.ap[:, 0:512]), [P1], [dst])

        with P.scope():
            P1 = P.ps([128, 1024], F32, "P1b")
            wo = P.sb([128, 8, 1024], BF16, "wo")
            wq = P.sb([128, 8, 2048], F32, "wq")
            sk = P.sb([128, 16, 128], F32, "sk")
            P.dma(wo, View(w_out, w_out.ap.rearrange("(kc p) o -> p kc o", p=128)), q="pool")
            for i in range(4):
                P.dma(View(wq, wq.ap[:, 2 * i:2 * i + 2, :]), View(w_q, w_q.ap.rearrange("(kc p) o -> p kc o", p=128)[:, 2 * i:2 * i + 2, :]), q="act" if i % 2 else "sp")
            P.dma(sk, skT, q="sp")
            xt = P.sb([128, 1024], F32, "xt")
            ot = P.sb([128, 8, 128], BF16, "ot")
            h2Tb = P.sb([128, 8, 128], BF16, "h2Tb")
            x1 = P.sb([128, 1024], F32, "x1")
            h2 = P.sb([128, 1024], F32, "h2")
            h2Tf = P.sb([128, 8, 128], F32, "h2Tf")
            qT = P.sb([128, 16, 128], F32, "qT")
            S3 = P.sb([128, 3, 8, 128], F32, "S3")
            s_sb = P.sb([128, 16, 128], F32, "s_sb")
            s_r = P.sb([128, 16, 128], F32, "s_r")
            v16 = P.sb([128, 16, 16], F32, "v16")
            cand = P.sb([128, 8, 256], F32, "cand")
            c16 = P.sb([128, 8, 16], F32, "c16")
            ec = P.sb([128, 8, 16], F32, "ec")
            st4 = P.sb([128, 4, 8], F32, "st4")
            ss = P.sb([128, 2], F32, "ss")
            oT_v = oT.ap.rearrange("(kc p) t -> p kc t", p=128)
            for ti in range(NT):
              if True:
                tsl = slice(ti * 128, (ti + 1) * 128)
                P.dma(xt, View(x, x.ap[tsl, :]), q="sp")
                P.dma(ot, View(oT, oT_v[:, :, tsl]), q="pool")
                for hf in range(2):
                    for kc in range(8):
                        P.op("pe", lambda: nc.tensor.matmul(P1.ap[:, hf * 512:(hf + 1) * 512], lhsT=ot.ap[:, kc, :], rhs=wo.ap[:, kc, hf * 512:(hf + 1) * 512], start=(kc == 0), stop=(kc == 7)), [ot, wo], [P1])
                P.op("dve", lambda: nc.vector.tensor_tensor(out=x1.ap, in0=P1.ap, in1=G1b.ap, op=OP.mult), [P1, G1b], [x1])
                P.op("pool", lambda: nc.gpsimd.tensor_tensor(out=x1.ap, in0=x1.ap, in1=xt.ap, op=OP.add), [x1, xt], [x1])
                P.dma(View(sc_x1, sc_x1.ap[tsl, :]), x1, q="sp")
                P.op("act", lambda: nc.scalar.activation(out=h2.ap, in_=x1.ap, func=AF.Square, accum_out=ss.ap[:, 0:1]), [x1], [h2, ss])
                P.op("dve", lambda: nc.vector.tensor_scalar(out=ss.ap[:, 1:2], in0=ss.ap[:, 0:1], scalar1=1.0 / 1024, scalar2=EPS, op0=OP.mult, op1=OP.add), [ss], [ss])
                P.op("act", lambda: nc.scalar.activation(out=ss.ap[:, 1:2], in_=ss.ap[:, 1:2], func=AF.Sqrt), [ss], [ss])
                P.op("dve", lambda: nc.vector.reciprocal(out=ss.ap[:, 1:2], in_=ss.ap[:, 1:2]), [ss], [ss])
                P.op("dve", lambda: nc.vector.scalar_tensor_tensor(out=h2.ap, in0=x1.ap, scalar=ss.ap[:, 1:2], in1=A2b.ap, op0=OP.mult, op1=OP.mult), [x1, ss, A2b], [h2])
                P.op("pool", lambda: nc.gpsimd.tensor_tensor(out=h2.ap, in0=h2.ap, in1=B2b.ap, op=OP.add), [h2, B2b], [h2])
                for kc in range(8):
                    P.op("pe", lambda: nc.tensor.transpose(P1.ap[:, kc * 128:(kc + 1) * 128], h2.ap[:, kc * 128:(kc + 1) * 128], ident.ap), [h2, ident], [P1])
                P.op("act", lambda: nc.scalar.copy(out=h2Tf.ap.rearrange("p a b -> p (a b)"), in_=P1.ap), [P1], [h2Tf])
                P.op("dve", lambda: nc.vector.tensor_copy(out=h2Tb.ap, in_=h2Tf.ap), [h2Tf], [h2Tb])
                P.dma(View(sc_h2T, sc_h2T.ap[ti]), h2Tb, q="sp")
                for rnd in range(2):
                    for j in range(8):
                        hp = rnd * 8 + j
                        for kc in range(8):
                            P.op("pe", lambda: nc.tensor.matmul(P1.ap[:, j * 128:(j + 1) * 128], lhsT=wq.ap[:, kc, hp * 128:(hp + 1) * 128], rhs=h2Tf.ap[:, kc, :], start=(kc == 0), stop=(kc == 7)), [wq, h2Tf], [P1])
                    P.op("act", lambda: nc.scalar.copy(out=qT.ap[:, rnd * 8:(rnd + 1) * 8, :].rearrange("p a b -> p (a b)"), in_=P1.ap), [P1], [qT])
                for rnd in range(2):
                    for j in range(8):
                        hp = rnd * 8 + j
                        P.op("pe", lambda: nc.tensor.matmul(P1.ap[:, j * 128:(j + 1) * 128], lhsT=qT.ap[:, hp, :], rhs=sk.ap[:, hp, :], start=True, stop=True), [qT, sk], [P1])
                    P.op("act", lambda: nc.scalar.copy(out=s_sb.ap[:, rnd * 8:(rnd + 1) * 8, :].rearrange("p a b -> p (a b)"), in_=P1.ap), [P1], [s_sb])
                for hp in range(16):
                    P.op("dve", lambda: nc.vector.max(out=v16.ap[:, hp, 0:8], in_=s_sb.ap[:, hp, :]), [s_sb], [v16])
                    P.op("dve", lambda: nc.vector.match_replace(out=s_r.ap[:, hp, :], in_to_replace=v16.ap[:, hp, 0:8], in_values=s_sb.ap[:, hp, :], imm_value=NEG), [s_sb, v16], [s_r])
                    P.op("dve", lambda: nc.vector.max(out=v16.ap[:, hp, 8:16], in_=s_r.ap[:, hp, :]), [s_r], [v16])
                v16v = v16.ap.rearrange("p (h two) k -> p h two k", two=2)
                P.op("dve", lambda: nc.vector.tensor_tensor(out=cand.ap.rearrange("p h (i j) -> p h i j", i=16), in0=v16v[:, :, 0, :].unsqueeze(3).to_broadcast([128, 8, 16, 16]), in1=v16v[:, :, 1, :].unsqueeze(2).to_broadcast([128, 8, 16, 16]), op=OP.add), [v16], [cand])
                for h in range(8):
                    P.op("dve", lambda: nc.vector.max(out=c16.ap[:, h, 0:8], in_=cand.ap[:, h, :]), [cand], [c16])
                    P.op("dve", lambda: nc.vector.match_replace(out=s_r.ap.rearrange("p a b -> p (a b)").rearrange("p (h c) -> p h c", h=8)[:, h, :], in_to_replace=c16.ap[:, h, 0:8], in_values=cand.ap[:, h, :], imm_value=NEG), [cand, c16], [s_r])
                    P.op("dve", lambda: nc.vector.max(out=c16.ap[:, h, 8:16], in_=s_r.ap.rearrange("p a b -> p (a b)").rearrange("p (h c) -> p h c", h=8)[:, h, :]), [s_r], [c16])
                P.op("dve", lambda: nc.vector.tensor_tensor(out=ec.ap, in0=c16.ap, in1=c16.ap[:, :, 0:1].to_broadcast([128, 8, 16]), op=OP.subtract), [c16], [ec])
                P.op("act", lambda: nc.scalar.activation(out=ec.ap, in_=ec.ap, func=AF.Exp), [ec], [ec])
                P.op("dve", lambda: nc.vector.tensor_reduce(out=st4.ap[:, 1, :], in_=ec.ap, axis=AX.X, op=OP.add), [ec], [st4])
                P.op("act", lambda: nc.scalar.activation(out=st4.ap[:, 2, :], in_=st4.ap[:, 1, :], func=AF.Ln), [st4], [st4])
                P.op("dve", lambda: nc.vector.tensor_tensor(out=st4.ap[:, 2, :], in0=st4.ap[:, 2, :], in1=c16.ap[:, :, 0], op=OP.add), [st4, c16], [st4])
                P.op("dve", lambda: nc.vector.tensor_scalar(out=st4.ap[:, 3, :], in0=c16.ap[:, :, 15], scalar1=-1e-3, scalar2=None, op0=OP.add), [c16], [st4])
                s_v = s_sb.ap.rearrange("p (h two) n -> p h two n", two=2)
                P.op("dve", lambda: nc.vector.tensor_tensor(out=S3.ap[:, 0], in0=s_v[:, :, 0, :], in1=st4.ap[:, 2, :].unsqueeze(2).to_broadcast([128, 8, 128]), op=OP.subtract), [s_sb, st4], [S3])
                P.op("dve", lambda: nc.vector.tensor_tensor(out=st4.ap[:, 0, :], in0=st4.ap[:, 3, :], in1=st4.ap[:, 2, :], op=OP.subtract), [st4], [st4])
                P.op("dve", lambda: nc.vector.memset(S3.ap[:, 1], 0.0), [], [S3])
                P.op("act", lambda: nc.scalar.activation(out=S3.ap[:, 1, :, 0], in_=st4.ap[:, 0, :], func=AF.Exp), [st4], [S3])
                P.op("pool", lambda: nc.gpsimd.tensor_copy(out=S3.ap[:, 2], in_=s_v[:, :, 1, :]), [s_sb], [S3])
                P.dma(View(sc_s, sc_s.ap[ti]), S3, q="act")

        with P.scope():
            uc = [P.sb([128, 8, 512], BF16, f"uc{i}") for i in range(2)]
            vc = [P.sb([128, 4, 1024], BF16, f"vc{i}") for i in range(3)]
            S3s = [P.sb([128, 3, 8, 128], F32, f"S3b{i}") for i in range(4)]
            x1s = [P.sb([128, 1024], F32, f"x1b{i}") for i in range(4)]
            h2Ts = [P.sb([128, 8, 128], BF16, f"h2Tt{i}") for i in range(4)]
            sumE = [P.sb([128, 8, 4, 128], F32, f"sumE{i}") for i in range(2)]
            Gh = [P.sb([128, 8, 512], BF16, f"Gh{i}") for i in range(2)]
            actT = [P.sb([128, 512], F32, f"actT{i}") for i in range(3)]
            gaT = [P.sb([128, 4, 128], BF16, f"gaT{i}") for i in range(2)]
            xo_t = P.sb([128, 1024], F32, "xo_t")
            junk = P.sb([128, 1024], F32, "junk2")
            ss = P.sb([128, 2], F32, "ss2")
            ACCs = [P.ps([128, 1024], F32, f"ACC{i}") for i in range(2)]
            PA = [P.ps([128, 512], F32, f"PA{i}") for i in range(2)]
            GTs = [P.ps([128, 512], F32, "GT0")] * 2
            PT = P.ps([128, 512], BF16, "PT")
            ga = P.sb([128, 512], BF16, "ga")
            uT_v = uT_bf.ap.rearrange("(kc p) e -> p kc e", p=128)
            vv_v = vv_bf.ap.rearrange("(b p) d -> p b d", p=128)
            assert NT % 2 == 0
            items = []
            for tp in range(NT // 2):
                for ci in range(NCH):
                    items.append((2 * tp, ci, 0))
                    items.append((2 * tp + 1, ci, 1))

            def load_tile(ti):
                P.dma(S3s[ti % 4], View(sc_s, sc_s.ap[ti]), q="sp")
                P.dma(x1s[ti % 4], View(sc_x1, sc_x1.ap[ti * 128:(ti + 1) * 128, :]), q="sp")
                P.dma(h2Ts[ti % 4], View(sc_h2T, sc_h2T.ap[ti]), q="sp")

            def stage1(idx):
                ti, ci, sub = items[idx]
                b = idx % 2
                b3 = idx % 3
                k = idx // 2
                S3 = S3s[ti % 4]
                h2T = h2Ts[ti % 4]
                if ci == 2 and sub == 0 and ti + 2 < NT:
                    load_tile(ti + 2)
                    load_tile(ti + 3)
                if sub == 0:
                    P.dma(uc[k % 2], View(uT_bf, uT_v[:, :, ci * 512:(ci + 1) * 512]), q="sp")
                    P.dma(vc[k % 3], View(vv_bf, vv_v[:, ci * 4:(ci + 1) * 4, :]), q="act")
                for kc in range(8):
                    P.op("pe", lambda: nc.tensor.matmul(PA[b].ap, lhsT=h2T.ap[:, kc, :], rhs=uc[k % 2].ap[:, kc, :], start=(kc == 0), stop=(kc == 7)), [h2T, uc[k % 2]], [PA[b]])
                P.op("act", lambda: nc.scalar.activation(out=actT[b3].ap, in_=PA[b].ap, func=AF.Gelu), [PA[b]], [actT[b3]])
                s1e_b = S3.ap[:, 0, :, ci * 4:(ci + 1) * 4].unsqueeze(3).to_broadcast([128, 8, 4, 128])
                s2_b = S3.ap[:, 2].unsqueeze(2).to_broadcast([128, 8, 4, 128])
                P.op("pool", lambda: nc.gpsimd.tensor_tensor(out=sumE[b].ap, in0=s1e_b, in1=s2_b, op=OP.add), [S3], [sumE[b]])
                P.op("act", lambda: nc.scalar.activation(out=sumE[b].ap, in_=sumE[b].ap, func=AF.Exp), [sumE[b]], [sumE[b]])

            def stage2a(idx):
                ti, ci, sub = items[idx]
                b = idx % 2
                S3 = S3s[ti % 4]
                GT = GTs[b]
                Ev = sumE[b].ap.rearrange("p h a n -> p h (a n)")
                for h in range(8):
                    P.op("dve", lambda: nc.vector.scalar_tensor_tensor(out=Gh[b].ap[:, h, :], in0=Ev[:, h, :], scalar=S3.ap[:, 1, h, 0:1], in1=Ev[:, h, :], op0=OP.is_ge, op1=OP.mult), [sumE[b], S3], [Gh[b]], indep=True)
                for h in range(8):
                    P.op("pe", lambda: nc.tensor.matmul(GT.ap, lhsT=identb.ap, rhs=Gh[b].ap[:, h, :], start=(h == 0), stop=(h == 7)), [Gh[b], identb], [GT])

            def stage2b(idx):
                ti, ci, sub = items[idx]
                b = idx % 2
                b3 = idx % 3
                k = idx // 2
                GT = GTs[b]
                ACC = ACCs[sub]
                P.op("dve", lambda: nc.vector.tensor_tensor(out=ga.ap, in0=GT.ap, in1=actT[b3].ap, op=OP.mult), [GT, actT[b3]], [ga])
                for bb in range(4):
                    P.op("pe", lambda: nc.tensor.transpose(PT.ap[:, bb * 128:(bb + 1) * 128], ga.ap[:, bb * 128:(bb + 1) * 128], identb.ap), [ga, identb], [PT])
                P.op("act", lambda: nc.scalar.copy(out=gaT[b].ap.rearrange("p a b -> p (a b)"), in_=PT.ap), [PT], [gaT[b]])
                for bb in range(4):
                    for hf in range(2):
                        P.op("pe", lambda: nc.tensor.matmul(ACC.ap[:, hf * 512:(hf + 1) * 512], lhsT=gaT[b].ap[:, bb, :], rhs=vc[k % 3].ap[:, bb, hf * 512:(hf + 1) * 512], start=(ci == 0 and bb == 0), stop=(ci == NCH - 1 and bb == 3)), [gaT[b], vc[k % 3]], [ACC])

            def epilogue(ti, sub):
                x1 = x1s[ti % 4]
                ACC = ACCs[sub]
                P.op("dve", lambda: nc.vector.tensor_tensor(out=xo_t.ap, in0=ACC.ap, in1=G2b.ap, op=OP.mult), [ACC, G2b], [xo_t])
                P.op("pool", lambda: nc.gpsimd.tensor_tensor(out=xo_t.ap, in0=xo_t.ap, in1=x1.ap, op=OP.add), [xo_t, x1], [xo_t])
                if final:
                    P.op("act", lambda: nc.scalar.activation(out=junk.ap, in_=xo_t.ap, func=AF.Square, accum_out=ss.ap[:, 0:1]), [xo_t], [junk, ss])
                    P.op("dve", lambda: nc.vector.tensor_scalar(out=ss.ap[:, 1:2], in0=ss.ap[:, 0:1], scalar1=1.0 / 1024, scalar2=EPS, op0=OP.mult, op1=OP.add), [ss], [ss])
                    P.op("act", lambda: nc.scalar.activation(out=ss.ap[:, 1:2], in_=ss.ap[:, 1:2], func=AF.Sqrt), [ss], [ss])
                    P.op("dve", lambda: nc.vector.reciprocal(out=ss.ap[:, 1:2], in_=ss.ap[:, 1:2]), [ss], [ss])
                    P.op("dve", lambda: nc.vector.scalar_tensor_tensor(out=xo_t.ap, in0=xo_t.ap, scalar=ss.ap[:, 1:2], in1=FNb.ap, op0=OP.mult, op1=OP.mult), [xo_t, ss, FNb], [xo_t])
                P.dma(View(xo, xo.ap[ti * 128:(ti + 1) * 128, :]), xo_t, q="sp")

            load_tile(0)
            load_tile(1)
            NI = len(items)
            stage1(0)
            stage1(1)
            stage2a(0)
            for idx in range(NI):
                if idx + 2 < NI:
                    stage1(idx + 2)
                stage2b(idx)
                if items[idx][1] == NCH - 1:
                    epilogue(items[idx][0], items[idx][2])
                if idx + 1 < NI:
                    stage2a(idx + 1)
        P.barrier()
        P.finish([xo])
        print("phase C: ninst", P.ninst, "nwait", P.nwait)
    return nc


SEQ = 16384
NCORE = 8
TOK = SEQ // NCORE
_PROGS = {}
f32 = np.float32


def build_phase_b_even(S):
    nc = bass.Bass("TRN2", target_bir_lowering=False)
    NBLK = S // 256
    with ExitStack() as st:
        P = Prog(nc, st)
        D = {}
        for nm in ("xq", "xk", "xv"):
            D[nm] = P.dram(nm, [128, S + 3], F32, kind="ExternalInput")
        D["cw"] = P.dram("cw", [128, 12], F32, kind="ExternalInput")
        D["z"] = P.dram("z", [S, 128], F32, kind="ExternalInput")
        D["ba"] = P.dram("ba", [128, S // 128, 2], F32, kind="ExternalInput")
        D["hc"] = P.dram("hc", [128, 2], F32, kind="ExternalInput")
        D["onorm"] = P.dram("onorm", [128, 128], F32, kind="ExternalInput")
        for nm in ("ident", "tri", "maskS", "maskI"):
            D[nm] = P.dram(nm, [128, 128], F32, kind="ExternalInput")
        D["OA"] = P.dram("OA", [S, 128], F32, kind="ExternalOutput")
        for nm in ("qT", "qTs", "kT", "kTs"):
            D[nm] = P.dram(nm, [128, S], F32, kind="ExternalInput")
        D["v"] = P.dram("v", [S, 128], F32, kind="ExternalInput")
        D["pos"] = P.dram("pos", [1, S], I32, kind="ExternalInput")
        D["cst"] = P.dram("cst", [128, 2], F32, kind="ExternalInput")
        D["maskT"] = P.dram("maskT", [128, 128], F32, kind="ExternalInput")
        D["selT"] = P.dram("selT", [NBLK, NBLK * 128], F32, kind="ExternalInput")
        D["OT"] = P.dram("OT", [128, S], F32, kind="ExternalOutput")
        with P.scope():
            gdn_body(P, nc, S, D)
        with P.scope():
            moba_body(P, nc, S, D)
        P.barrier()
        P.finish([D["OA"], D["OT"]])
    return nc


def _prog(key):
    if key not in _PROGS:
        if key == "A_even":
            _PROGS[key] = build_phase_a(TOK, 3592)
        elif key == "A_odd":
            _PROGS[key] = build_phase_a(TOK, 832)
        elif key == "B_even":
            _PROGS[key] = build_phase_b_even(SEQ)
        elif key == "B_odd":
            _PROGS[key] = build_phase_b_mla(SEQ)
        elif key == "C":
            _PROGS[key] = build_phase_c(TOK, final=False)
        elif key == "C_final":
            _PROGS[key] = build_phase_c(TOK, final=True)
    return _PROGS[key]


def _run(key, in_maps):
    res = run_bass_kernel_spmd(_prog(key), in_maps, core_ids=list(range(NCORE)))
    return res.results


def _c(a):
    return np.ascontiguousarray(a)


def kernel(x, c, positions, mod_w, mod_b, norm_mix, norm_ffn, hy_w_in, gdn_conv, gdn_a_log,
           gdn_dt_bias, gdn_o_norm, hy_w_out, mla_w_in, mla_q_norm, mla_kv_norm, mla_w_uq,
           mla_w_ukv, mla_w_out, peer_w_q, peer_sub_keys, peer_u, peer_v, final_norm):
    A_ = lambda a: np.asarray(a)
    x, c, positions, mod_w, mod_b = A_(x), A_(c), A_(positions), A_(mod_w), A_(mod_b)
    norm_mix, norm_ffn, hy_w_in, gdn_conv = A_(norm_mix), A_(norm_ffn), A_(hy_w_in), A_(gdn_conv)
    gdn_a_log, gdn_dt_bias, gdn_o_norm, hy_w_out = A_(gdn_a_log), A_(gdn_dt_bias), A_(gdn_o_norm), A_(hy_w_out)
    mla_w_in, mla_q_norm, mla_kv_norm, mla_w_uq = A_(mla_w_in), A_(mla_q_norm), A_(mla_kv_norm), A_(mla_w_uq)
    mla_w_ukv, mla_w_out, peer_w_q, peer_sub_keys = A_(mla_w_ukv), A_(mla_w_out), A_(peer_w_q), A_(peer_sub_keys)
    peer_u, peer_v, final_norm = A_(peer_u), A_(peer_v), A_(final_norm)
    S = SEQ
    xcur = _c(x[0].astype(f32, copy=False))
    cT = _c(c.reshape(8, 128).T)
    pos = _c(positions.reshape(1, S).astype(np.int32, copy=False))
    I = np.eye(128, dtype=f32)
    tri = np.triu(np.ones((128, 128), f32))
    maskS = np.tril(np.ones((128, 128), f32), -1)
    maskI = np.tril(np.ones((128, 128), f32))
    maskT = np.triu(np.ones((128, 128), f32))
    NBLK = S // 256
    selT = np.kron(np.eye(NBLK, dtype=f32), np.ones((1, 128), f32))
    invf_b = (10000.0 ** (-np.arange(0, 128, 2, dtype=np.float32) / 128)).astype(f32)
    cst_b = np.zeros((128, 2), f32); cst_b[:, 0] = np.concatenate([invf_b, invf_b]); cst_b[:64, 1] = -1; cst_b[64:, 1] = 1
    invf_c = (10000.0 ** (-np.arange(0, 64, 2, dtype=np.float32) / 64)).astype(f32)
    cst_c = np.zeros((128, 2), f32); cst_c[:64, 0] = np.concatenate([invf_c, invf_c]); cst_c[:32, 1] = -1; cst_c[32:64, 1] = 1
    sw = lambda a: _c(np.concatenate([a[a.shape[0] // 2:], a[:a.shape[0] // 2]], 0))

    for l in range(4):
        i = l // 2
        even = (l % 2 == 0)
        W = hy_w_in[i] if even else mla_w_in[i]
        modw_a = _c(mod_w[l][:, 0:2048]); modb_a = _c(mod_b[l][None, 0:2048]); nrm_a = _c(norm_mix[l][None])
        in_maps = [dict(x=xcur[k * TOK:(k + 1) * TOK], cT=cT, modw=modw_a, modb=modb_a, nrm=nrm_a, W=_c(W), ident=I) for k in range(NCORE)]
        res = _run("A_even" if even else "A_odd", in_maps)
        Y = np.concatenate([r["Y"] for r in res], 0)
        if even:
            in_maps = []
            for k in range(NCORE):
                h = k % 4
                def xT(off):
                    a = Y[:, off + h * 128: off + (h + 1) * 128].T
                    return _c(np.concatenate([np.zeros((128, 3), f32), a], 1))
                cw = _c(np.concatenate([gdn_conv[i][:, off + h * 128: off + (h + 1) * 128].T for off in (0, 512, 1024)], 1))
                ba = _c(np.stack([Y[:, 2048 + h], Y[:, 2052 + h]], -1).reshape(S // 128, 128, 2).transpose(1, 0, 2))
                hc = _c(np.tile(np.array([[gdn_a_log[i][h], gdn_dt_bias[i][h]]], f32), (128, 1)))
                qT = _c(Y[:, 2056 + h * 128: 2056 + (h + 1) * 128].T)
                kT = _c(Y[:, 2568 + h * 128: 2568 + (h + 1) * 128].T)
                in_maps.append(dict(xq=xT(0), xk=xT(512), xv=xT(1024), cw=cw, z=_c(Y[:, 1536 + h * 128: 1536 + (h + 1) * 128]),
                                    ba=ba, hc=hc, onorm=_c(np.tile(gdn_o_norm[i][None], (128, 1))), ident=I, tri=tri, maskS=maskS, maskI=maskI,
                                    qT=qT, qTs=sw(qT), kT=kT, kTs=sw(kT), v=_c(Y[:, 3080 + h * 128: 3080 + (h + 1) * 128]),
                                    pos=pos, cst=cst_b, maskT=maskT, selT=selT))
            res = _run("B_even", in_maps)
            oT = np.concatenate([res[h]["OA"].T for h in range(4)] + [res[h]["OT"] for h in range(4)], 0)
            w_out = hy_w_out[i]
        else:
            YT = _c(Y.T)
            qn = _c(mla_q_norm[i].reshape(4, 128).T); kvn = _c(mla_kv_norm[i].reshape(2, 128).T)
            krT = _c(YT[768:832]); krTs = sw(krT)
            in_maps = []
            for h in range(NCORE):
                wq = mla_w_uq[i][:, h * 192:(h + 1) * 192]; wkv = mla_w_ukv[i][:, h * 256:(h + 1) * 256]
                wr = wq[:, 128:]
                in_maps.append(dict(cqT=YT[:512], ckvT=YT[512:768], krT=krT, krTs=krTs, pos=pos, qn=qn, kvn=kvn,
                                    wuq_n=_c(wq[:, :128]), wuq_r=_c(wr), wuq_rs=_c(np.concatenate([wr[:, 32:], wr[:, :32]], 1)),
                                    wukv_k=_c(wkv[:, :128]), wukv_v=_c(wkv[:, 128:]), cst=cst_c, maskT=maskT))
            res = _run("B_odd", in_maps)
            oT = np.concatenate([res[h]["OT"] for h in range(NCORE)], 0)
            w_out = mla_w_out[i]
        modw_c = _c(mod_w[l][:, 2048:6144]); modb_c = _c(mod_b[l][None, 2048:6144])
        skT = _c(peer_sub_keys[l].reshape(16, 128, 128).transpose(2, 0, 1))
        uT = _c(peer_u[l].T)
        vv = _c(peer_v[l])
        in_maps = [dict(x=xcur[k * TOK:(k + 1) * TOK], oT=_c(oT[:, k * TOK:(k + 1) * TOK]), w_out=_c(w_out), cT=cT, modw=modw_c, modb=modb_c,
                        nrm=_c(norm_ffn[l][None]), fnrm=_c(final_norm[None]), w_q=_c(peer_w_q[l]), skT=skT, uT=uT, vv=vv, ident=I) for k in range(NCORE)]
        res = _run("C_final" if l == 3 else "C", in_maps)
        xcur = np.concatenate([r["xo"] for r in res], 0)
    return xcur.reshape(1, S, 1024).astype(f32, copy=False)
```

```python
import math
import numpy as np
from contextlib import ExitStack
import concourse.bass as bass
import concourse.mybir as mybir
from concourse.bass_utils import run_bass_kernel_spmd


F32 = mybir.dt.float32
BF16 = mybir.dt.bfloat16
I32 = mybir.dt.int32
AF = mybir.ActivationFunctionType
OP = mybir.AluOpType
AX = mybir.AxisListType


SAME_ENGINE_SYNC = True


class Buf:
    __slots__ = ("ap", "w", "r", "dsem", "dcnt", "name", "is_dram", "is_psum")

    def __init__(self, ap, name=""):
        self.ap = ap
        self.w = None
        self.r = {}
        self.dsem = None
        self.dcnt = 0
        self.name = name
        self.is_dram = False
        self.is_psum = False

    def __getitem__(self, idx):
        return View(self, self.ap[idx])


class View:
    __slots__ = ("buf", "ap")

    def __init__(self, buf, ap):
        self.buf = buf
        self.ap = ap

    def __getitem__(self, idx):
        return View(self.buf, self.ap[idx])


def _b(x):
    return x.buf if isinstance(x, View) else x


def _ap(x):
    if isinstance(x, (Buf, View)):
        return x.ap
    return x


class Prog:
    ENG = ("pe", "act", "dve", "pool", "sp")

    def __init__(self, nc, stack, same_engine_sync=None):
        self.nc = nc
        self.stack = stack
        self.e = {"pe": nc.tensor, "act": nc.scalar, "dve": nc.vector, "pool": nc.gpsimd, "sp": nc.sync}
        self.sem = {k: stack.enter_context(nc.semaphore("s_" + k)) for k in self.ENG}
        self.cnt = {k: 0 for k in self.ENG}
        self.seen = {k: {} for k in self.ENG}
        self.same = SAME_ENGINE_SYNC if same_engine_sync is None else same_engine_sync
        self.ninst = 0
        self.nwait = 0
        self._dsems = []
        self._dbufs = []
        self._free_dsems = []
        self._scopes = []
        self.top = stack

    def sb(self, shape, dt=F32, name=None):
        t = self.stack.enter_context(self.nc.sbuf_tensor(name or f"sb{self.ninst}_{len(self._dsems)}_{np.random.randint(1<<30)}", list(shape), dt))
        return Buf(t.ap() if hasattr(t, "ap") and callable(getattr(t, "ap")) else t[:], name or "")

    def ps(self, shape, dt=F32, name=None):
        t = self.stack.enter_context(self.nc.psum_tensor(name or f"ps{np.random.randint(1<<30)}", list(shape), dt))
        b = Buf(t.ap() if hasattr(t, "ap") and callable(getattr(t, "ap")) else t[:], name or "")
        b.is_psum = True
        return b

    def dram(self, name, shape, dt=F32, kind="Internal"):
        t = self.nc.dram_tensor(name, list(shape), dt, kind=kind)
        b = Buf(t.ap(), name)
        b.is_dram = True
        return b

    def _need(self, eng, dep):
        if dep is None:
            return
        kind, key, count = dep
        if kind == "eng":
            if key == eng and (eng == "pe" or not self.same):
                return
            sem = self.sem[key]
            skey = key
        else:
            sem = key
            skey = ("d", id(key))
        if self.seen[eng].get(skey, 0) >= count:
            return
        self.e[eng].wait_ge(sem, count)
        self.seen[eng][skey] = count
        self.nwait += 1

    def _deps(self, eng, reads, writes):
        for x in reads:
            b = _b(x)
            self._need(eng, b.w)
            if b.is_psum:
                for k, d in b.r.items():
                    if k != eng:
                        self._need(eng, d)
        for x in writes:
            b = _b(x)
            self._need(eng, b.w)
            for d in b.r.values():
                self._need(eng, d)

    def op(self, eng, fn, reads=(), writes=(), indep=False):
        if indep:
            sv = self.same
            self.same = False
            self._deps(eng, reads, writes)
            self.same = sv
        else:
            self._deps(eng, reads, writes)
        inst = fn()
        self.cnt[eng] += 1
        c = self.cnt[eng]
        inst.then_inc(self.sem[eng], 1)
        tag = ("eng", eng, c)
        for x in reads:
            _b(x).r[eng] = tag
        for x in writes:
            b = _b(x)
            b.w = tag
            b.r = {}
        self.ninst += 1
        return inst

    def dma(self, out, in_, q="sp", **kw):
        ob, ib = _b(out), _b(in_)
        self._deps(q, [ib], [ob])
        owner = ib if (ob.is_dram and not ib.is_dram) else ob
        if owner.dsem is None:
            if False:
                pass
            else:
                owner.dsem = self.top.enter_context(self.nc.semaphore(f"d{len(self._dsems)}"))
                owner.dcnt = 0
                self._dsems.append(owner.dsem)
            self._dbufs.append(owner)
            if self._scopes:
                self._scopes[-1].append(owner)
        inst = self.e[q].dma_start(out=_ap(out), in_=_ap(in_), **kw)
        owner.dcnt += 16
        inst.then_inc(owner.dsem, 16)
        tag = ("dma", owner.dsem, owner.dcnt)
        ob.w = tag
        ob.r = {}
        ib.r[("dma", id(owner.dsem))] = tag
        self.ninst += 1
        return inst

    def barrier(self):
        for e in self.ENG:
            for k in self.ENG:
                if k != e and self.cnt[k] > 0:
                    self._need(e, ("eng", k, self.cnt[k]))
        for b in self._dbufs:
            for e in self.ENG:
                if b.dcnt > 0:
                    self._need(e, ("dma", b.dsem, b.dcnt))

    def scope(self):
        return _Scope(self)

    def finish(self, bufs, eng="sp"):
        for b in bufs:
            self._need(eng, b.w)


class _Scope:
    def __init__(self, P):
        self.P = P

    def __enter__(self):
        self.es = ExitStack()
        self.es.__enter__()
        self.prev = self.P.stack
        self.P.stack = self.es
        self.P._scopes.append([])
        return self

    def __exit__(self, *a):
        P = self.P
        P.barrier()
        owners = P._scopes.pop()
        for b in owners:
            P._free_dsems.append((b.dsem, b.dcnt))
            P._dbufs.remove(b)
            b.dsem = None
        P.stack = self.prev
        return self.es.__exit__(*a)


EPS = 1e-6


def build_phase_a(T, O):
    nc = bass.Bass("TRN2", target_bir_lowering=False)
    NT = T // 128
    with ExitStack() as st:
        P = Prog(nc, st)
        x = P.dram("x", [T, 1024], F32, kind="ExternalInput")
        cT = P.dram("cT", [128, 8], F32, kind="ExternalInput")
        modw = P.dram("modw", [1024, 2048], F32, kind="ExternalInput")
        modb = P.dram("modb", [1, 2048], F32, kind="ExternalInput")
        nrm = P.dram("nrm", [1, 1024], F32, kind="ExternalInput")
        W = P.dram("W", [1024, O], F32, kind="ExternalInput")
        ident_d = P.dram("ident", [128, 128], F32, kind="ExternalInput")
        Y = P.dram("Y", [T, O], F32, kind="ExternalOutput")

        ident = P.sb([128, 128], F32, "ident_sb")
        ones_row = P.sb([1, 128], F32, "ones_row")
        A1b = P.sb([128, 1024], F32, "A1b")
        B1b = P.sb([128, 1024], F32, "B1b")
        P1 = P.ps([128, 1024], F32, "P1")
        P2 = P.ps([128, 1024], F32, "P2")
        P.dma(ident, ident_d)
        P.op("dve", lambda: nc.vector.memset(ones_row.ap, 1.0), [], [ones_row])
        with P.scope():
            cond = P.sb([128, 8], F32, "cond")
            modrow = P.sb([1, 2048], F32, "modrow")
            mb = P.sb([1, 2048], F32, "mb")
            nr = P.sb([1, 1024], F32, "nr")
            a1row = P.sb([1, 1024], F32, "a1row")
            mwc = [P.sb([128, 8, 512], F32, f"mwc{i}") for i in range(2)]
            P.dma(cond, cT)
            P.dma(mb, modb, q="act")
            P.dma(nr, nrm, q="act")
            P.op("act", lambda: nc.scalar.activation(out=cond.ap, in_=cond.ap, func=AF.Silu), [cond], [cond])
            mw_v = modw.ap.rearrange("(kc p) o -> p kc o", p=128)
            for ch in range(4):
                buf = mwc[ch % 2]
                P.dma(buf, View(modw, mw_v[:, :, ch * 512:(ch + 1) * 512]), q="sp" if ch % 2 == 0 else "act")
                for kc in range(8):
                    P.op("pe", lambda: nc.tensor.matmul(P1.ap[0:1, 0:512], lhsT=cond.ap[:, kc:kc + 1], rhs=buf.ap[:, kc, :], start=(kc == 0), stop=(kc == 7)), [cond, buf], [P1])
                P.op("dve", lambda: nc.vector.tensor_tensor(out=modrow.ap[0:1, ch * 512:(ch + 1) * 512], in0=P1.ap[0:1, 0:512], in1=mb.ap[0:1, ch * 512:(ch + 1) * 512], op=OP.add), [P1, mb], [modrow])
            P.op("dve", lambda: nc.vector.scalar_tensor_tensor(out=a1row.ap, in0=modrow.ap[0:1, 1024:2048], scalar=1.0, in1=nr.ap, op0=OP.add, op1=OP.mult), [modrow, nr], [a1row])
            b1row = View(modrow, modrow.ap[0:1, 0:1024])
            for (row, dst) in ((a1row, A1b), (b1row, B1b)):
                for c0 in range(0, 1024, 512):
                    rap = _ap(row)[0:1, c0:c0 + 512]
                    P.op("pe", lambda: nc.tensor.matmul(P1.ap[:, 0:512], lhsT=ones_row.ap[0:1, 0:128], rhs=rap, start=True, stop=True), [ones_row, row], [P1])
                    P.op("act", lambda: nc.scalar.copy(out=dst.ap[:, c0:c0 + 512], in_=P1.ap[:, 0:512]), [P1], [dst])
        with P.scope():
            Wsb = P.sb([128, 8, O], BF16, "Wsb")
            W_v = W.ap.rearrange("(kc p) o -> p kc o", p=128)
            for kc in range(8):
                P.dma(View(Wsb, Wsb.ap[:, kc, :]), View(W, W_v[:, kc, :]), q="pool")
            xt = [P.sb([128, 1024], F32, f"xt{i}") for i in range(2)]
            h = P.sb([128, 1024], F32, "h")
            hT = [P.sb([128, 8, 128], BF16, f"hT{i}") for i in range(2)]
            yt = [P.sb([128, 1024], F32, f"yt{i}") for i in range(2)]
            ss = P.sb([128, 2], F32, "ss")
            nyc = 0
            for ti in range(NT):
                tsl = slice(ti * 128, (ti + 1) * 128)
                xb = xt[ti % 2]
                hb = hT[ti % 2]
                P.dma(xb, View(x, x.ap[tsl, :]), q="sp")
                P.op("act", lambda: nc.scalar.activation(out=h.ap, in_=xb.ap, func=AF.Square, accum_out=ss.ap[:, 0:1]), [xb], [h, ss])
                P.op("dve", lambda: nc.vector.tensor_scalar(out=ss.ap[:, 1:2], in0=ss.ap[:, 0:1], scalar1=1.0 / 1024, scalar2=EPS, op0=OP.mult, op1=OP.add), [ss], [ss])
                P.op("act", lambda: nc.scalar.activation(out=ss.ap[:, 1:2], in_=ss.ap[:, 1:2], func=AF.Sqrt), [ss], [ss])
                P.op("dve", lambda: nc.vector.reciprocal(out=ss.ap[:, 1:2], in_=ss.ap[:, 1:2]), [ss], [ss])
                P.op("dve", lambda: nc.vector.scalar_tensor_tensor(out=h.ap, in0=xb.ap, scalar=ss.ap[:, 1:2], in1=A1b.ap, op0=OP.mult, op1=OP.mult), [xb, ss, A1b], [h])
                P.op("pool", lambda: nc.gpsimd.tensor_tensor(out=h.ap, in0=h.ap, in1=B1b.ap, op=OP.add), [h, B1b], [h])
                for kc in range(8):
                    P.op("pe", lambda: nc.tensor.transpose(P1.ap[:, kc * 128:(kc + 1) * 128], h.ap[:, kc * 128:(kc + 1) * 128], ident.ap), [h, ident], [P1])
                P.op("act", lambda: nc.scalar.copy(out=hb.ap.rearrange("p a b -> p (a b)"), in_=P1.ap), [P1], [hb])
                for o0 in range(0, O, 1024):
                    ow = min(1024, O - o0)
                    yb = yt[nyc % 2]
                    nyc += 1
                    for c0 in range(0, ow, 512):
                        cw = min(512, ow - c0)
                        for kc in range(8):
                            P.op("pe", lambda: nc.tensor.matmul(P2.ap[:, c0:c0 + cw], lhsT=hb.ap[:, kc, :], rhs=Wsb.ap[:, kc, o0 + c0:o0 + c0 + cw], start=(kc == 0), stop=(kc == 7)), [hb, Wsb], [P2])
                    if (nyc % 2) == 0:
                        P.op("act", lambda: nc.scalar.copy(out=yb.ap[:, 0:ow], in_=P2.ap[:, 0:ow]), [P2], [yb])
                    else:
                        P.op("dve", lambda: nc.vector.tensor_copy(out=yb.ap[:, 0:ow], in_=P2.ap[:, 0:ow]), [P2], [yb])
                    P.dma(View(Y, Y.ap[tsl, o0:o0 + ow]), View(yb, yb.ap[:, 0:ow]), q="act")
        P.barrier()
        P.finish([Y])
        print("phase A: ninst", P.ninst, "nwait", P.nwait)
    return nc


EPS = 1e-6
TWO_PI = 2.0 * math.pi
C1 = 6.28125
C2 = TWO_PI - C1


def rope_tables(P, nc, pos_i, posf, tmp, tmi, CS, SN, invf, sgn, HP):
    P.op("dve", lambda: nc.vector.tensor_copy(out=posf.ap, in_=pos_i.ap), [pos_i], [posf])
    P.op("dve", lambda: nc.vector.tensor_scalar(out=posf.ap, in0=posf.ap, scalar1=invf.ap[0:HP, 0:1], scalar2=None, op0=OP.mult), [posf, invf], [posf])
    P.op("dve", lambda: nc.vector.tensor_scalar(out=tmi.ap, in0=posf.ap, scalar1=1.0 / TWO_PI, scalar2=None, op0=OP.mult), [posf], [tmi])
    P.op("dve", lambda: nc.vector.tensor_copy(out=tmp.ap, in_=tmi.ap), [tmi], [tmp])
    P.op("dve", lambda: nc.vector.scalar_tensor_tensor(out=posf.ap, in0=tmp.ap, scalar=-C1, in1=posf.ap, op0=OP.mult, op1=OP.add), [tmp, posf], [posf])
    P.op("dve", lambda: nc.vector.scalar_tensor_tensor(out=posf.ap, in0=tmp.ap, scalar=-C2, in1=posf.ap, op0=OP.mult, op1=OP.add), [tmp, posf], [posf])
    P.op("dve", lambda: nc.vector.tensor_scalar(out=tmp.ap, in0=posf.ap, scalar1=math.pi, scalar2=-TWO_PI, op0=OP.is_gt, op1=OP.mult), [posf], [tmp])
    P.op("dve", lambda: nc.vector.tensor_tensor(out=posf.ap, in0=posf.ap, in1=tmp.ap, op=OP.add), [posf, tmp], [posf])
    P.op("dve", lambda: nc.vector.tensor_scalar(out=tmp.ap, in0=posf.ap, scalar1=-math.pi, scalar2=TWO_PI, op0=OP.is_lt, op1=OP.mult), [posf], [tmp])
    P.op("dve", lambda: nc.vector.tensor_tensor(out=posf.ap, in0=posf.ap, in1=tmp.ap, op=OP.add), [posf, tmp], [posf])
    P.op("dve", lambda: nc.vector.tensor_scalar(out=posf.ap, in0=posf.ap, scalar1=math.pi, scalar2=-math.pi, op0=OP.min, op1=OP.max), [posf], [posf])
    P.op("act", lambda: nc.scalar.activation(out=SN.ap, in_=posf.ap, func=AF.Sin), [posf], [SN])
    P.op("dve", lambda: nc.vector.tensor_scalar(out=SN.ap, in0=SN.ap, scalar1=sgn.ap[0:HP, 0:1], scalar2=None, op0=OP.mult), [SN, sgn], [SN])
    P.op("act", lambda: nc.scalar.activation(out=tmp.ap, in_=posf.ap, func=AF.Abs), [posf], [tmp])
    P.op("dve", lambda: nc.vector.tensor_scalar(out=tmp.ap, in0=tmp.ap, scalar1=-1.0, scalar2=math.pi / 2, op0=OP.mult, op1=OP.add), [tmp], [tmp])
    P.op("act", lambda: nc.scalar.activation(out=CS.ap, in_=tmp.ap, func=AF.Sin), [tmp], [CS])


def build_phase_b_mla(S):
    nc = bass.Bass("TRN2", target_bir_lowering=False)
    NG = S // 512
    NB = S // 128
    SCALE = (128 + 64) ** -0.5
    with ExitStack() as st:
        P = Prog(nc, st)
        cqT = P.dram("cqT", [512, S], F32, kind="ExternalInput")
        ckvT = P.dram("ckvT", [256, S], F32, kind="ExternalInput")
        krT = P.dram("krT", [64, S], F32, kind="ExternalInput")
        krTs = P.dram("krTs", [64, S], F32, kind="ExternalInput")
        pos = P.dram("pos", [1, S], I32, kind="ExternalInput")
        qn = P.dram("qn", [128, 4], F32, kind="ExternalInput")
        kvn = P.dram("kvn", [128, 2], F32, kind="ExternalInput")
        wuq_n = P.dram("wuq_n", [512, 128], F32, kind="ExternalInput")
        wuq_r = P.dram("wuq_r", [512, 64], F32, kind="ExternalInput")
        wuq_rs = P.dram("wuq_rs", [512, 64], F32, kind="ExternalInput")
        wukv_k = P.dram("wukv_k", [256, 128], F32, kind="ExternalInput")
        wukv_v = P.dram("wukv_v", [256, 128], F32, kind="ExternalInput")
        cst = P.dram("cst", [128, 2], F32, kind="ExternalInput")
        maskT_d = P.dram("maskT", [128, 128], F32, kind="ExternalInput")
        OT = P.dram("OT", [128, S], F32, kind="ExternalOutput")

        KTn = P.sb([128, S], BF16, "KTn")
        KTr = P.sb([64, S], BF16, "KTr")
        Vsb = P.sb([128, NB, 128], BF16, "Vsb")
        ones_f = P.sb([128, 128], F32, "ones_f")
        ones_b = P.sb([128, 128], BF16, "ones_b")
        maskT = P.sb([128, 128], BF16, "maskT_sb")
        cs = P.sb([128, 2], F32, "cst_sb")
        qn_sb = P.sb([128, 4], F32, "qn_sb")
        kvn_sb = P.sb([128, 2], F32, "kvn_sb")
        Wqn = P.sb([128, 4, 128], BF16, "Wqn")
        Wqr = P.sb([128, 4, 64], BF16, "Wqr")
        Wqrs = P.sb([128, 4, 64], BF16, "Wqrs")
        Wkk = P.sb([128, 2, 128], BF16, "Wkk")
        Wkv = P.sb([128, 2, 128], BF16, "Wkv")
        P.op("dve", lambda: nc.vector.memset(ones_f.ap, 1.0), [], [ones_f])
        P.op("dve", lambda: nc.vector.memset(ones_b.ap, 1.0), [], [ones_b])
        P.dma(maskT, maskT_d, q="pool")
        P.dma(cs, cst); P.dma(qn_sb, qn); P.dma(kvn_sb, kvn)
        P.dma(Wqn, View(wuq_n, wuq_n.ap.rearrange("(kc p) o -> p kc o", p=128)), q="pool")
        P.dma(Wqr, View(wuq_r, wuq_r.ap.rearrange("(kc p) o -> p kc o", p=128)), q="pool")
        P.dma(Wqrs, View(wuq_rs, wuq_rs.ap.rearrange("(kc p) o -> p kc o", p=128)), q="pool")
        P.dma(Wkk, View(wukv_k, wukv_k.ap.rearrange("(kc p) o -> p kc o", p=128)), q="pool")
        P.dma(Wkv, View(wukv_v, wukv_v.ap.rearrange("(kc p) o -> p kc o", p=128)), q="pool")
        invf = View(cs, cs.ap[:, 0:1]); sgn = View(cs, cs.ap[:, 1:2])

        cq_t = P.sb([128, 4, 512], F32, "cq_t")
        ckv_t = P.sb([128, 2, 512], F32, "ckv_t")
        sq = P.sb([128, 4, 512], F32, "sq")
        rs_q = P.sb([128, 512], F32, "rs_q")
        rs_k = P.sb([128, 512], F32, "rs_k")
        cqn = P.sb([128, 4, 512], BF16, "cqn")
        ckvn = P.sb([128, 2, 512], BF16, "ckvn")
        pos_i = P.sb([64, 512], I32, "pos_i")
        posf = P.sb([64, 512], F32, "posf")
        tmp = P.sb([64, 512], F32, "tmp")
        tmi = P.sb([64, 512], I32, "tmi")
        CS = P.sb([64, 512], F32, "CS")
        SN = P.sb([64, 512], F32, "SN")
        kr_t = P.sb([64, 512], F32, "kr_t")
        krs_t = P.sb([64, 512], F32, "krs_t")
        r1 = P.sb([64, 512], F32, "r1")
        r2 = P.sb([64, 512], F32, "r2")
        QTn = P.sb([128, 512], BF16, "QTn")
        QTr = P.sb([64, 512], BF16, "QTr")
        PT = [P.sb([128, 512], BF16, f"PT{i}") for i in range(2)]
        rec = P.sb([128, 512], F32, "rec")
        o_sb = P.sb([128, 512], F32, "o_sb")
        P1 = P.ps([128, 1024], F32, "P1")
        ST = [P.ps([128, 512], F32, f"ST{i}") for i in range(2)]
        OTp = P.ps([128, 512], F32, "OTp")
        DEN = P.ps([128, 512], F32, "DEN")

        cqT_v = cqT.ap.rearrange("(kc p) t -> p kc t", p=128)
        ckvT_v = ckvT.ap.rearrange("(kc p) t -> p kc t", p=128)
        it = 0
        for g in range(NG):
            gs = slice(g * 512, (g + 1) * 512)
            P.dma(cq_t, View(cqT, cqT_v[:, :, gs]), q="sp")
            P.dma(ckv_t, View(ckvT, ckvT_v[:, :, gs]), q="act")
            P.dma(kr_t, View(krT, krT.ap[:, gs]), q="sp")
            P.dma(krs_t, View(krTs, krTs.ap[:, gs]), q="act")
            P.dma(pos_i, View(pos, pos.ap[0:1, gs].to_broadcast([64, 512])), q="sp")
            rope_tables(P, nc, pos_i, posf, tmp, tmi, CS, SN, invf, sgn, 64)
            P.op("act", lambda: nc.scalar.activation(out=sq.ap, in_=cq_t.ap, func=AF.Square), [cq_t], [sq])
            for kc in range(4):
                P.op("pe", lambda: nc.tensor.matmul(P1.ap[:, 0:512], lhsT=ones_f.ap, rhs=sq.ap[:, kc, :], start=(kc == 0), stop=(kc == 3)), [ones_f, sq], [P1])
            P.op("dve", lambda: nc.vector.tensor_scalar(out=rs_q.ap, in0=P1.ap[:, 0:512], scalar1=1.0 / 512, scalar2=EPS, op0=OP.mult, op1=OP.add), [P1], [rs_q])
            P.op("act", lambda: nc.scalar.activation(out=rs_q.ap, in_=rs_q.ap, func=AF.Sqrt), [rs_q], [rs_q])
            P.op("dve", lambda: nc.vector.reciprocal(out=rs_q.ap, in_=rs_q.ap), [rs_q], [rs_q])
            for kc in range(4):
                P.op("dve", lambda: nc.vector.scalar_tensor_tensor(out=cqn.ap[:, kc, :], in0=cq_t.ap[:, kc, :], scalar=qn_sb.ap[:, kc:kc + 1], in1=rs_q.ap, op0=OP.mult, op1=OP.mult), [cq_t, qn_sb, rs_q], [cqn])
            P.op("act", lambda: nc.scalar.activation(out=sq.ap[:, 0:2, :], in_=ckv_t.ap, func=AF.Square), [ckv_t], [sq])
            for kc in range(2):
                P.op("pe", lambda: nc.tensor.matmul(P1.ap[:, 512:1024], lhsT=ones_f.ap, rhs=sq.ap[:, kc, :], start=(kc == 0), stop=(kc == 1)), [ones_f, sq], [P1])
            P.op("dve", lambda: nc.vector.tensor_scalar(out=rs_k.ap, in0=P1.ap[:, 512:1024], scalar1=1.0 / 256, scalar2=EPS, op0=OP.mult, op1=OP.add), [P1], [rs_k])
            P.op("act", lambda: nc.scalar.activation(out=rs_k.ap, in_=rs_k.ap, func=AF.Sqrt), [rs_k], [rs_k])
            P.op("dve", lambda: nc.vector.reciprocal(out=rs_k.ap, in_=rs_k.ap), [rs_k], [rs_k])
            for kc in range(2):
                P.op("dve", lambda: nc.vector.scalar_tensor_tensor(out=ckvn.ap[:, kc, :], in0=ckv_t.ap[:, kc, :], scalar=kvn_sb.ap[:, kc:kc + 1], in1=rs_k.ap, op0=OP.mult, op1=OP.mult), [ckv_t, kvn_sb, rs_k], [ckvn])
            for kc in range(4):
                P.op("pe", lambda: nc.tensor.matmul(P1.ap[:, 0:512], lhsT=Wqn.ap[:, kc, :], rhs=cqn.ap[:, kc, :], start=(kc == 0), stop=(kc == 3)), [Wqn, cqn], [P1])
            P.op("act", lambda: nc.scalar.activation(out=QTn.ap, in_=P1.ap[:, 0:512], func=AF.Copy, scale=SCALE), [P1], [QTn])
            for kc in range(4):
                P.op("pe", lambda: nc.tensor.matmul(P1.ap[0:64, 512:1024], lhsT=Wqr.ap[:, kc, :], rhs=cqn.ap[:, kc, :], start=(kc == 0), stop=(kc == 3)), [Wqr, cqn], [P1])
            P.op("dve", lambda: nc.vector.tensor_tensor(out=r1.ap, in0=P1.ap[0:64, 512:1024], in1=CS.ap, op=OP.mult), [P1, CS], [r1])
            for kc in range(4):
                P.op("pe", lambda: nc.tensor.matmul(P1.ap[0:64, 0:512], lhsT=Wqrs.ap[:, kc, :], rhs=cqn.ap[:, kc, :], start=(kc == 0), stop=(kc == 3)), [Wqrs, cqn], [P1])
            P.op("dve", lambda: nc.vector.tensor_tensor(out=r2.ap, in0=P1.ap[0:64, 0:512], in1=SN.ap, op=OP.mult), [P1, SN], [r2])
            P.op("dve", lambda: nc.vector.tensor_tensor(out=r1.ap, in0=r1.ap, in1=r2.ap, op=OP.add), [r1, r2], [r1])
            P.op("act", lambda: nc.scalar.activation(out=QTr.ap, in_=r1.ap, func=AF.Copy, scale=SCALE), [r1], [QTr])
            for kc in range(2):
                P.op("pe", lambda: nc.tensor.matmul(P1.ap[:, 512:1024], lhsT=Wkk.ap[:, kc, :], rhs=ckvn.ap[:, kc, :], start=(kc == 0), stop=(kc == 1)), [Wkk, ckvn], [P1])
            P.op("act", lambda: nc.scalar.copy(out=KTn.ap[:, gs], in_=P1.ap[:, 512:1024]), [P1], [KTn])
            P.op("dve", lambda: nc.vector.tensor_tensor(out=r1.ap, in0=kr_t.ap, in1=CS.ap, op=OP.mult), [kr_t, CS], [r1])
            P.op("dve", lambda: nc.vector.tensor_tensor(out=r2.ap, in0=krs_t.ap, in1=SN.ap, op=OP.mult), [krs_t, SN], [r2])
            P.op("dve", lambda: nc.vector.tensor_tensor(out=KTr.ap[:, gs], in0=r1.ap, in1=r2.ap, op=OP.add), [r1, r2], [KTr])
            for tt in range(4):
                for kc in range(2):
                    P.op("pe", lambda: nc.tensor.matmul(P1.ap[:, tt * 128:(tt + 1) * 128], lhsT=ckvn.ap[:, kc, tt * 128:(tt + 1) * 128], rhs=Wkv.ap[:, kc, :], start=(kc == 0), stop=(kc == 1)), [ckvn, Wkv], [P1])
            P.op("act", lambda: nc.scalar.copy(out=Vsb.ap[:, g * 4:(g + 1) * 4, :].rearrange("p a b -> p (a b)"), in_=P1.ap[:, 0:512]), [P1], [Vsb])
            nj = 4 * g + 4

            def S_(j):
                b = (it + j) % 2
                c0 = max(0, j - 4 * g) * 128
                ks = slice(j * 128, (j + 1) * 128)
                P.op("pe", lambda: nc.tensor.matmul(ST[b].ap[:, c0:512], lhsT=KTn.ap[:, ks], rhs=QTn.ap[:, c0:512], start=True, stop=False), [KTn, QTn], [ST[b]])
                P.op("pe", lambda: nc.tensor.matmul(ST[b].ap[:, c0:512], lhsT=KTr.ap[:, ks], rhs=QTr.ap[:, c0:512], start=False, stop=True), [KTr, QTr], [ST[b]])

            def EPV_(j):
                b = (it + j) % 2
                c0 = max(0, j - 4 * g) * 128
                P.op("act", lambda: nc.scalar.activation(out=PT[b].ap[:, c0:512], in_=ST[b].ap[:, c0:512], func=AF.Exp), [ST[b]], [PT[b]])
                if j >= 4 * g:
                    P.op("pool", lambda: nc.gpsimd.tensor_tensor(out=PT[b].ap[:, c0:c0 + 128], in0=PT[b].ap[:, c0:c0 + 128], in1=maskT.ap, op=OP.mult), [PT[b], maskT], [PT[b]])
                P.op("pe", lambda: nc.tensor.matmul(OTp.ap[:, c0:512], lhsT=Vsb.ap[:, j, :], rhs=PT[b].ap[:, c0:512], start=(j == 0), stop=(j == nj - 1)), [Vsb, PT[b]], [OTp])
                P.op("pe", lambda: nc.tensor.matmul(DEN.ap[:, c0:512], lhsT=ones_b.ap, rhs=PT[b].ap[:, c0:512], start=(j == 0), stop=(j == nj - 1)), [ones_b, PT[b]], [DEN])

            S_(0)
            for j in range(nj):
                if j + 1 < nj:
                    S_(j + 1)
                EPV_(j)
            it += nj
            P.op("dve", lambda: nc.vector.reciprocal(out=rec.ap, in_=DEN.ap), [DEN], [rec])
            P.op("dve", lambda: nc.vector.tensor_tensor(out=o_sb.ap, in0=OTp.ap, in1=rec.ap, op=OP.mult), [OTp, rec], [o_sb])
            P.dma(View(OT, OT.ap[:, gs]), o_sb, q="sp")
        P.barrier()
        P.finish([OT])
        print("phase B mla: ninst", P.ninst, "nwait", P.nwait)
    return nc


BIG = 30000.0
NEG = -3.0e38


def moba_body(P, nc, S, D):
    NG = S // 512
    NB = S // 128
    NBLK = S // 256
    SCALE = 128 ** -0.5
    qT, qTs, kT, kTs, v, pos = D["qT"], D["qTs"], D["kT"], D["kTs"], D["v"], D["pos"]
    OT = D["OT"]
    KT = P.sb([128, S], BF16, "m_KT")
    Vsb = P.sb([128, NB, 128], BF16, "m_Vsb")
    KMT = P.sb([128, NBLK], F32, "m_KMT")
    SelT = P.sb([NBLK, NBLK, 128], BF16, "m_SelT")
    ones_b = P.sb([128, 128], BF16, "m_ones_b")
    maskT = P.sb([128, 128], BF16, "m_maskT")
    ident = P.sb([128, 128], F32, "m_ident")
    cs = P.sb([128, 2], F32, "m_cst")
    P.op("dve", lambda: nc.vector.memset(ones_b.ap, 1.0), [], [ones_b])
    P.dma(maskT, D["maskT"], q="pool")
    P.dma(SelT, View(D["selT"], D["selT"].ap.rearrange("n (m k) -> n m k", k=128)), q="pool")
    P.dma(ident, D["ident"])
    P.dma(cs, D["cst"])
    v_v = v.ap.rearrange("(b p) d -> p b d", p=128)
    for b0 in range(0, NB, 16):
        b1 = min(NB, b0 + 16)
        P.dma(View(Vsb, Vsb.ap[:, b0:b1, :]), View(v, v_v[:, b0:b1, :]), q="pool")
    invf = View(cs, cs.ap[:, 0:1]); sgn = View(cs, cs.ap[:, 1:2])
    q_t = P.sb([128, 512], F32, "m_q_t"); qs_t = P.sb([128, 512], F32, "m_qs_t")
    k_t = P.sb([128, 512], F32, "m_k_t"); ks_t = P.sb([128, 512], F32, "m_ks_t")
    pos_i = P.sb([128, 512], I32, "m_pos_i"); posf = P.sb([128, 512], F32, "m_posf")
    tmp = P.sb([128, 512], F32, "m_tmp"); tmi = P.sb([128, 512], I32, "m_tmi")
    CS = P.sb([128, 512], F32, "m_CS"); SN = P.sb([128, 512], F32, "m_SN")
    r1 = P.sb([128, 512], F32, "m_r1"); r2 = P.sb([128, 512], F32, "m_r2")
    QTf = P.sb([128, 512], F32, "m_QTf")
    QT = P.sb([128, 512], BF16, "m_QT")
    gate = P.sb([128, NBLK], F32, "m_gate")
    m8 = P.sb([128, 8], F32, "m_m8")
    pen = P.sb([128, NBLK], F32, "m_pen")
    PenT = P.sb([NBLK, 512], BF16, "m_PenT")
    PT = [P.sb([128, 512], BF16, f"m_PT{i}") for i in range(2)]
    rec = P.sb([128, 512], F32, "m_rec")
    o_sb = P.sb([128, 512], F32, "m_o_sb")
    P1 = P.ps([128, 512], F32, "m_P1")
    ST = [P.ps([128, 512], F32, f"m_ST{i}") for i in range(2)]
    OTp = P.ps([128, 512], F32, "m_OTp")
    DEN = P.ps([128, 512], F32, "m_DEN")
    it = 0
    for g in range(NG):
        gs = slice(g * 512, (g + 1) * 512)
        P.dma(q_t, View(qT, qT.ap[:, gs]), q="sp")
        P.dma(qs_t, View(qTs, qTs.ap[:, gs]), q="act")
        P.dma(k_t, View(kT, kT.ap[:, gs]), q="sp")
        P.dma(ks_t, View(kTs, kTs.ap[:, gs]), q="act")
        P.dma(pos_i, View(pos, pos.ap[0:1, gs].to_broadcast([128, 512])), q="sp")
        rope_tables(P, nc, pos_i, posf, tmp, tmi, CS, SN, invf, sgn, 128)
        P.op("dve", lambda: nc.vector.tensor_tensor(out=r1.ap, in0=q_t.ap, in1=CS.ap, op=OP.mult), [q_t, CS], [r1])
        P.op("pool", lambda: nc.gpsimd.tensor_tensor(out=r2.ap, in0=qs_t.ap, in1=SN.ap, op=OP.mult), [qs_t, SN], [r2])
        P.op("dve", lambda: nc.vector.tensor_tensor(out=QTf.ap, in0=r1.ap, in1=r2.ap, op=OP.add), [r1, r2], [QTf])
        P.op("act", lambda: nc.scalar.activation(out=QT.ap, in_=QTf.ap, func=AF.Copy, scale=SCALE), [QTf], [QT])
        P.op("dve", lambda: nc.vector.tensor_tensor(out=r1.ap, in0=k_t.ap, in1=CS.ap, op=OP.mult), [k_t, CS], [r1])
        P.op("pool", lambda: nc.gpsimd.tensor_tensor(out=r2.ap, in0=ks_t.ap, in1=SN.ap, op=OP.mult), [ks_t, SN], [r2])
        P.op("dve", lambda: nc.vector.tensor_tensor(out=r1.ap, in0=r1.ap, in1=r2.ap, op=OP.add), [r1, r2], [r1])
        P.op("act", lambda: nc.scalar.copy(out=KT.ap[:, gs], in_=r1.ap), [r1], [KT])
        P.op("dve", lambda: nc.vector.tensor_reduce(out=KMT.ap[:, 2 * g:2 * g + 2], in_=r1.ap.rearrange("p (n k) -> p n k", k=256), axis=AX.X, op=OP.add), [r1], [KMT])
        P.op("dve", lambda: nc.vector.tensor_scalar(out=KMT.ap[:, 2 * g:2 * g + 2], in0=KMT.ap[:, 2 * g:2 * g + 2], scalar1=1.0 / 256, scalar2=None, op0=OP.mult), [KMT], [KMT])
        for qb in range(4):
            own = 2 * g + qb // 2
            P.op("dve", lambda: nc.vector.memset(gate.ap, NEG), [], [gate])
            if own > 0:
                P.op("pe", lambda: nc.tensor.matmul(P1.ap[:, 0:own], lhsT=QTf.ap[:, qb * 128:(qb + 1) * 128], rhs=KMT.ap[:, 0:own], start=True, stop=True), [QTf, KMT], [P1])
                P.op("dve", lambda: nc.vector.tensor_copy(out=gate.ap[:, 0:own], in_=P1.ap[:, 0:own]), [P1], [gate])
            P.op("dve", lambda: nc.vector.max(out=m8.ap, in_=gate.ap), [gate], [m8])
            P.op("dve", lambda: nc.vector.tensor_scalar(out=m8.ap[:, 2:3], in0=m8.ap[:, 2:3], scalar1=-1.0e30, scalar2=None, op0=OP.max), [m8], [m8])
            P.op("dve", lambda: nc.vector.tensor_scalar(out=pen.ap, in0=gate.ap, scalar1=m8.ap[:, 2:3], scalar2=BIG, op0=OP.is_ge, op1=OP.mult), [gate, m8], [pen])
            P.op("dve", lambda: nc.vector.tensor_scalar(out=pen.ap, in0=pen.ap, scalar1=-BIG, scalar2=None, op0=OP.add), [pen], [pen])
            P.op("dve", lambda: nc.vector.memset(pen.ap[:, own:own + 1], 0.0), [], [pen])
            P.op("pe", lambda: nc.tensor.transpose(P1.ap[0:NBLK, 128:256], pen.ap, ident.ap), [pen, ident], [P1])
            P.op("act", lambda: nc.scalar.copy(out=PenT.ap[:, qb * 128:(qb + 1) * 128], in_=P1.ap[0:NBLK, 128:256]), [P1], [PenT])
        nj = 4 * g + 4

        def S_(j):
            b = (it + j) % 2
            c0 = max(0, j - 4 * g) * 128
            ks = slice(j * 128, (j + 1) * 128)
            n = j // 2
            P.op("pe", lambda: nc.tensor.matmul(ST[b].ap[:, c0:512], lhsT=KT.ap[:, ks], rhs=QT.ap[:, c0:512], start=True, stop=False), [KT, QT], [ST[b]])
            P.op("pe", lambda: nc.tensor.matmul(ST[b].ap[:, c0:512], lhsT=SelT.ap[:, n, :], rhs=PenT.ap[:, c0:512], start=False, stop=True), [SelT, PenT], [ST[b]])

        def EPV_(j):
            b = (it + j) % 2
            c0 = max(0, j - 4 * g) * 128
            P.op("act", lambda: nc.scalar.activation(out=PT[b].ap[:, c0:512], in_=ST[b].ap[:, c0:512], func=AF.Exp), [ST[b]], [PT[b]])
            if j >= 4 * g:
                P.op("pool", lambda: nc.gpsimd.tensor_tensor(out=PT[b].ap[:, c0:c0 + 128], in0=PT[b].ap[:, c0:c0 + 128], in1=maskT.ap, op=OP.mult), [PT[b], maskT], [PT[b]])
            P.op("pe", lambda: nc.tensor.matmul(OTp.ap[:, c0:512], lhsT=Vsb.ap[:, j, :], rhs=PT[b].ap[:, c0:512], start=(j == 0), stop=(j == nj - 1)), [Vsb, PT[b]], [OTp])
            P.op("pe", lambda: nc.tensor.matmul(DEN.ap[:, c0:512], lhsT=ones_b.ap, rhs=PT[b].ap[:, c0:512], start=(j == 0), stop=(j == nj - 1)), [ones_b, PT[b]], [DEN])

        S_(0)
        for j in range(nj):
            if j + 1 < nj:
                S_(j + 1)
            EPV_(j)
        it += nj
        P.op("dve", lambda: nc.vector.reciprocal(out=rec.ap, in_=DEN.ap), [DEN], [rec])
        P.op("dve", lambda: nc.vector.tensor_tensor(out=o_sb.ap, in0=OTp.ap, in1=rec.ap, op=OP.mult), [OTp, rec], [o_sb])
        P.dma(View(OT, OT.ap[:, gs]), o_sb, q="sp")


def build_phase_b_moba(S):
    nc = bass.Bass("TRN2", target_bir_lowering=False)
    NBLK = S // 256
    with ExitStack() as st:
        P = Prog(nc, st)
        D = {}
        for nm in ("qT", "qTs", "kT", "kTs"):
            D[nm] = P.dram(nm, [128, S], F32, kind="ExternalInput")
        D["v"] = P.dram("v", [S, 128], F32, kind="ExternalInput")
        D["pos"] = P.dram("pos", [1, S], I32, kind="ExternalInput")
        D["cst"] = P.dram("cst", [128, 2], F32, kind="ExternalInput")
        D["maskT"] = P.dram("maskT", [128, 128], F32, kind="ExternalInput")
        D["selT"] = P.dram("selT", [NBLK, NBLK * 128], F32, kind="ExternalInput")
        D["ident"] = P.dram("ident", [128, 128], F32, kind="ExternalInput")
        D["OT"] = P.dram("OT", [128, S], F32, kind="ExternalOutput")
        with P.scope():
            moba_body(P, nc, S, D)
        P.barrier()
        P.finish([D["OT"]])
        print("phase B moba: ninst", P.ninst, "nwait", P.nwait)
    return nc


EPS = 1e-6


def gdn_body(P, nc, S, D):
    NG = S // 512
    NCH = S // 128
    op = P.op
    V = nc.vector
    A = nc.scalar
    PE = nc.tensor
    ident = P.sb([128, 128], F32, "g_ident"); tri = P.sb([128, 128], F32, "g_tri")
    maskS = P.sb([128, 128], F32, "g_maskS"); maskI = P.sb([128, 128], F32, "g_maskI")
    ones_f = P.sb([128, 128], F32, "g_ones")
    cw = P.sb([128, 12], F32, "g_cw"); hc = P.sb([128, 2], F32, "g_hc"); onb = P.sb([128, 128], F32, "g_onb")
    ba = P.sb([128, NCH, 2], F32, "g_ba")
    beta = P.sb([128, NCH], F32, "g_beta"); nbeta = P.sb([128, NCH], F32, "g_nbeta"); graw = P.sb([128, NCH], F32, "g_graw")
    tmpc = P.sb([128, NCH], F32, "g_tmpc")
    for b_, d_ in ((ident, "ident"), (tri, "tri"), (maskS, "maskS"), (maskI, "maskI"), (cw, "cw"), (hc, "hc"), (onb, "onorm")):
        P.dma(b_, D[d_], q="sp")
    P.dma(ba, D["ba"], q="act")
    op("dve", lambda: V.memset(ones_f.ap, 1.0), [], [ones_f])
    op("act", lambda: A.activation(out=beta.ap, in_=ba.ap[:, :, 0], func=AF.Sigmoid), [ba], [beta])
    op("dve", lambda: V.tensor_scalar(out=nbeta.ap, in0=beta.ap, scalar1=-1.0, scalar2=None, op0=OP.mult), [beta], [nbeta])
    op("act", lambda: A.activation(out=tmpc.ap, in_=ba.ap[:, :, 1], func=AF.Exp, bias=hc.ap[:, 1:2]), [ba, hc], [tmpc])
    op("act", lambda: A.activation(out=tmpc.ap, in_=tmpc.ap, func=AF.Ln, bias=ones_f.ap[:, 0:1]), [tmpc, ones_f], [tmpc])
    op("act", lambda: A.activation(out=hc.ap[:, 0:1], in_=hc.ap[:, 0:1], func=AF.Exp), [hc], [hc])
    op("dve", lambda: V.tensor_scalar(out=graw.ap, in0=tmpc.ap, scalar1=hc.ap[:, 0:1], scalar2=-1.0, op0=OP.mult, op1=OP.mult), [tmpc, hc], [graw])

    St = P.sb([128, 128], F32, "g_S")
    op("dve", lambda: V.memset(St.ap, 0.0), [], [St])
    xin = [P.sb([128, 515], F32, f"g_xin{i}") for i in range(3)]
    yc = [P.sb([128, 512], F32, f"g_yc{i}") for i in range(3)]
    sq = P.sb([128, 512], F32, "g_sq")
    rs = P.sb([128, 512], F32, "g_rs")
    names = ["GrB", "Gm_sb", "t1", "Dm", "EG", "Am", "Bm", "A2", "B2", "Ym", "qk", "qkT", "kbg", "kd", "vb", "wT", "u", "qdT", "vnew", "zt", "ot", "o2"]
    W = {n: P.sb([128, 128], F32, "g_" + n) for n in names}
    cols = P.sb([128, 8], F32, "g_cols")
    PG = P.ps([128, 512], F32, "g_PG"); PTr = P.ps([128, 512], F32, "g_PTr"); PD = P.ps([128, 512], F32, "g_PD")
    PW = P.ps([128, 512], F32, "g_PW"); PS = P.ps([128, 512], F32, "g_PS"); PC = P.ps([128, 512], F32, "g_PC")
    c_ = lambda i: slice(i * 128, (i + 1) * 128)
    xs = (D["xq"], D["xk"], D["xv"])
    z_v = D["z"].ap.rearrange("(n p) d -> n p d", p=128)
    oa_v = D["OA"].ap.rearrange("(n p) d -> n p d", p=128)
    for g in range(NG):
        for i in range(3):
            P.dma(xin[i], View(xs[i], xs[i].ap[:, g * 512:g * 512 + 515]), q="sp" if i != 1 else "act")
            op("dve", lambda: V.tensor_scalar(out=yc[i].ap, in0=xin[i].ap[:, 0:512], scalar1=cw.ap[:, 4 * i:4 * i + 1], scalar2=None, op0=OP.mult), [xin[i], cw], [yc[i]])
            for j in range(1, 4):
                op("dve", lambda: V.scalar_tensor_tensor(out=yc[i].ap, in0=xin[i].ap[:, j:j + 512], scalar=cw.ap[:, 4 * i + j:4 * i + j + 1], in1=yc[i].ap, op0=OP.mult, op1=OP.add), [xin[i], cw, yc[i]], [yc[i]])
            op("act", lambda: A.activation(out=yc[i].ap, in_=yc[i].ap, func=AF.Silu), [yc[i]], [yc[i]])
            if i < 2:
                op("act", lambda: A.activation(out=sq.ap, in_=yc[i].ap, func=AF.Square), [yc[i]], [sq])
                op("pe", lambda: PE.matmul(PC.ap, lhsT=ones_f.ap, rhs=sq.ap, start=True, stop=True), [ones_f, sq], [PC])
                op("dve", lambda: V.tensor_scalar(out=rs.ap, in0=PC.ap, scalar1=EPS, scalar2=None, op0=OP.add), [PC], [rs])
                op("act", lambda: A.activation(out=rs.ap, in_=rs.ap, func=AF.Sqrt), [rs], [rs])
                op("dve", lambda: V.reciprocal(out=rs.ap, in_=rs.ap), [rs], [rs])
                sc_ = (128 ** -0.5) if i == 0 else 1.0
                op("dve", lambda: V.scalar_tensor_tensor(out=yc[i].ap, in0=yc[i].ap, scalar=sc_, in1=rs.ap, op0=OP.mult, op1=OP.mult), [yc[i], rs], [yc[i]])
        for cc in range(4):
            n = g * 4 + cc
            qTn = View(yc[0], yc[0].ap[:, c_(cc)]); kTn = View(yc[1], yc[1].ap[:, c_(cc)]); vTn = View(yc[2], yc[2].ap[:, c_(cc)])
            gr = graw.ap[:, n:n + 1]; be = beta.ap[:, n:n + 1]; nbe = nbeta.ap[:, n:n + 1]
            op("dve", lambda: V.tensor_scalar(out=W["GrB"].ap, in0=ones_f.ap, scalar1=gr, scalar2=None, op0=OP.mult), [ones_f, graw], [W["GrB"]])
            op("pe", lambda: PE.matmul(PG.ap[:, c_(0)], lhsT=W["GrB"].ap, rhs=tri.ap, start=True, stop=True), [W["GrB"], tri], [PG])
            op("pe", lambda: PE.matmul(PG.ap[:, 128:129], lhsT=tri.ap, rhs=gr, start=True, stop=True), [tri, graw], [PG])
            op("pe", lambda: PE.matmul(PG.ap[:, c_(2)], lhsT=kTn.ap, rhs=kTn.ap, start=True, stop=True), [kTn], [PG])
            op("pe", lambda: PE.matmul(PG.ap[:, c_(3)], lhsT=qTn.ap, rhs=kTn.ap, start=True, stop=True), [qTn, kTn], [PG])
            op("act", lambda: A.copy(out=W["Gm_sb"].ap, in_=PG.ap[:, c_(0)]), [PG], [W["Gm_sb"]])
            op("act", lambda: A.copy(out=cols.ap[:, 0:1], in_=PG.ap[:, 128:129]), [PG], [cols])
            op("dve", lambda: V.tensor_scalar(out=W["t1"].ap, in0=W["Gm_sb"].ap, scalar1=cols.ap[:, 0:1], scalar2=0.0, op0=OP.subtract, op1=OP.max), [W["Gm_sb"], cols], [W["t1"]])
            op("act", lambda: A.activation(out=W["Dm"].ap, in_=W["t1"].ap, func=AF.Exp, scale=-1.0), [W["t1"]], [W["Dm"]])
            op("act", lambda: A.activation(out=W["EG"].ap, in_=W["Gm_sb"].ap, func=AF.Exp), [W["Gm_sb"]], [W["EG"]])
            op("act", lambda: A.activation(out=cols.ap[:, 1:2], in_=cols.ap[:, 0:1], func=AF.Exp), [cols], [cols])
            op("act", lambda: A.activation(out=cols.ap[:, 2:3], in_=cols.ap[:, 0:1], func=AF.Exp, scale=-1.0, bias=W["Gm_sb"].ap[:, 127:128]), [cols, W["Gm_sb"]], [cols])
            op("dve", lambda: V.tensor_tensor(out=cols.ap[:, 3:4], in0=cols.ap[:, 1:2], in1=be, op=OP.mult), [cols, beta], [cols])
            op("dve", lambda: V.tensor_tensor(out=W["t1"].ap, in0=PG.ap[:, c_(2)], in1=W["Dm"].ap, op=OP.mult), [PG, W["Dm"]], [W["t1"]])
            op("dve", lambda: V.scalar_tensor_tensor(out=W["Am"].ap, in0=W["t1"].ap, scalar=nbe, in1=maskS.ap, op0=OP.mult, op1=OP.mult), [W["t1"], nbeta, maskS], [W["Am"]])
            op("dve", lambda: V.tensor_tensor(out=W["t1"].ap, in0=PG.ap[:, c_(3)], in1=W["Dm"].ap, op=OP.mult), [PG, W["Dm"]], [W["t1"]])
            op("dve", lambda: V.tensor_tensor(out=W["qk"].ap, in0=W["t1"].ap, in1=maskI.ap, op=OP.mult), [W["t1"], maskI], [W["qk"]])
            op("pe", lambda: PE.transpose(PTr.ap[:, c_(0)], W["Am"].ap, ident.ap), [W["Am"], ident], [PTr])
            op("pe", lambda: PE.transpose(PTr.ap[:, c_(1)], W["qk"].ap, ident.ap), [W["qk"], ident], [PTr])
            op("pe", lambda: PE.transpose(PTr.ap[:, c_(2)], kTn.ap, ident.ap), [kTn, ident], [PTr])
            op("pe", lambda: PE.transpose(PTr.ap[:, c_(3)], vTn.ap, ident.ap), [vTn, ident], [PTr])
            op("act", lambda: A.copy(out=W["Bm"].ap, in_=PTr.ap[:, c_(0)]), [PTr], [W["Bm"]])
            op("act", lambda: A.copy(out=W["qkT"].ap, in_=PTr.ap[:, c_(1)]), [PTr], [W["qkT"]])
            op("act", lambda: A.activation(out=W["kbg"].ap, in_=PTr.ap[:, c_(2)], func=AF.Copy, scale=cols.ap[:, 3:4]), [PTr, cols], [W["kbg"]])
            op("act", lambda: A.activation(out=W["kd"].ap, in_=PTr.ap[:, c_(2)], func=AF.Copy, scale=cols.ap[:, 2:3]), [PTr, cols], [W["kd"]])
            op("act", lambda: A.activation(out=W["vb"].ap, in_=PTr.ap[:, c_(3)], func=AF.Copy, scale=be), [PTr, beta], [W["vb"]])
            op("dve", lambda: V.tensor_tensor(out=W["Ym"].ap, in0=W["Bm"].ap, in1=ident.ap, op=OP.add), [W["Bm"], ident], [W["Ym"]])
            Ac, Bc, An, Bn = W["Am"], W["Bm"], W["A2"], W["B2"]
            for stp in range(6):
                op("pe", lambda: PE.matmul(PD.ap[:, c_(0)], lhsT=Bc.ap, rhs=Ac.ap, start=True, stop=True), [Bc, Ac], [PD])
                if stp < 5:
                    op("pe", lambda: PE.matmul(PD.ap[:, c_(1)], lhsT=Ac.ap, rhs=Bc.ap, start=True, stop=True), [Ac, Bc], [PD])
                op("act", lambda: A.copy(out=An.ap, in_=PD.ap[:, c_(0)]), [PD], [An])
                if stp < 5:
                    op("act", lambda: A.copy(out=Bn.ap, in_=PD.ap[:, c_(1)]), [PD], [Bn])
                op("pe", lambda: PE.matmul(PD.ap[:, c_(2)], lhsT=An.ap, rhs=W["Ym"].ap, start=True, stop=True), [An, W["Ym"]], [PD])
                op("dve", lambda: V.tensor_tensor(out=W["Ym"].ap, in0=W["Ym"].ap, in1=PD.ap[:, c_(2)], op=OP.add), [W["Ym"], PD], [W["Ym"]])
                Ac, Bc, An, Bn = An, Bn, Ac, Bc
            op("pe", lambda: PE.matmul(PW.ap[:, c_(0)], lhsT=W["kbg"].ap, rhs=W["Ym"].ap, start=True, stop=True), [W["kbg"], W["Ym"]], [PW])
            op("pe", lambda: PE.matmul(PW.ap[:, c_(1)], lhsT=W["Ym"].ap, rhs=W["vb"].ap, start=True, stop=True), [W["Ym"], W["vb"]], [PW])
            op("act", lambda: A.copy(out=W["wT"].ap, in_=PW.ap[:, c_(0)]), [PW], [W["wT"]])
            op("act", lambda: A.copy(out=W["u"].ap, in_=PW.ap[:, c_(1)]), [PW], [W["u"]])
            op("dve", lambda: V.tensor_tensor(out=W["qdT"].ap, in0=qTn.ap, in1=W["EG"].ap, op=OP.mult), [qTn, W["EG"]], [W["qdT"]])
            op("pe", lambda: PE.matmul(PS.ap[:, c_(0)], lhsT=W["wT"].ap, rhs=St.ap, start=True, stop=True), [W["wT"], St], [PS])
            op("dve", lambda: V.tensor_tensor(out=W["vnew"].ap, in0=W["u"].ap, in1=PS.ap[:, c_(0)], op=OP.subtract), [W["u"], PS], [W["vnew"]])
            op("pe", lambda: PE.matmul(PS.ap[:, c_(1)], lhsT=W["qdT"].ap, rhs=St.ap, start=True, stop=False), [W["qdT"], St], [PS])
            op("pe", lambda: PE.matmul(PS.ap[:, c_(1)], lhsT=W["qkT"].ap, rhs=W["vnew"].ap, start=False, stop=True), [W["qkT"], W["vnew"]], [PS])
            op("pe", lambda: PE.matmul(PS.ap[:, c_(2)], lhsT=W["kd"].ap, rhs=W["vnew"].ap, start=True, stop=True), [W["kd"], W["vnew"]], [PS])
            op("act", lambda: A.copy(out=W["ot"].ap, in_=PS.ap[:, c_(1)]), [PS], [W["ot"]])
            op("dve", lambda: V.scalar_tensor_tensor(out=St.ap, in0=St.ap, scalar=W["EG"].ap[:, 127:128], in1=PS.ap[:, c_(2)], op0=OP.mult, op1=OP.add), [St, W["EG"], PS], [St])
            P.dma(W["zt"], View(D["z"], z_v[n]), q="act")
            op("act", lambda: A.activation(out=W["o2"].ap, in_=W["ot"].ap, func=AF.Square, accum_out=cols.ap[:, 4:5]), [W["ot"]], [W["o2"], cols])
            op("dve", lambda: V.tensor_scalar(out=cols.ap[:, 5:6], in0=cols.ap[:, 4:5], scalar1=1.0 / 128, scalar2=EPS, op0=OP.mult, op1=OP.add), [cols], [cols])
            op("act", lambda: A.activation(out=cols.ap[:, 5:6], in_=cols.ap[:, 5:6], func=AF.Sqrt), [cols], [cols])
            op("dve", lambda: V.reciprocal(out=cols.ap[:, 5:6], in_=cols.ap[:, 5:6]), [cols], [cols])
            op("act", lambda: A.activation(out=W["zt"].ap, in_=W["zt"].ap, func=AF.Silu), [W["zt"]], [W["zt"]])
            op("dve", lambda: V.scalar_tensor_tensor(out=W["o2"].ap, in0=W["ot"].ap, scalar=cols.ap[:, 5:6], in1=onb.ap, op0=OP.mult, op1=OP.mult), [W["ot"], cols, onb], [W["o2"]])
            op("dve", lambda: V.tensor_tensor(out=W["o2"].ap, in0=W["o2"].ap, in1=W["zt"].ap, op=OP.mult), [W["o2"], W["zt"]], [W["o2"]])
            P.dma(View(D["OA"], oa_v[n]), W["o2"], q="sp")


def build_phase_b_gdn(S):
    nc = bass.Bass("TRN2", target_bir_lowering=False)
    with ExitStack() as st:
        P = Prog(nc, st)
        D = {}
        for nm in ("xq", "xk", "xv"):
            D[nm] = P.dram(nm, [128, S + 3], F32, kind="ExternalInput")
        D["cw"] = P.dram("cw", [128, 12], F32, kind="ExternalInput")
        D["z"] = P.dram("z", [S, 128], F32, kind="ExternalInput")
        D["ba"] = P.dram("ba", [128, S // 128, 2], F32, kind="ExternalInput")
        D["hc"] = P.dram("hc", [128, 2], F32, kind="ExternalInput")
        D["onorm"] = P.dram("onorm", [128, 128], F32, kind="ExternalInput")
        for nm in ("ident", "tri", "maskS", "maskI"):
            D[nm] = P.dram(nm, [128, 128], F32, kind="ExternalInput")
        D["OA"] = P.dram("OA", [S, 128], F32, kind="ExternalOutput")
        with P.scope():
            gdn_body(P, nc, S, D)
        P.barrier()
        P.finish([D["OA"]])
        print("phase B gdn: ninst", P.ninst, "nwait", P.nwait)
    return nc


EPS = 1e-6
NEG = -3.0e38


def bcast_rows(P, nc, ones_row, row, dst, ps, ncols):
    for c0 in range(0, ncols, 512):
        P.op("pe", lambda: nc.tensor.matmul(ps.ap[:, 0:512], lhsT=ones_row.ap[0:1, 0:128], rhs=row.ap[0:1, c0:c0 + 512], start=True, stop=True), [ones_row, row], [ps])
        P.op("act", lambda: nc.scalar.copy(out=dst.ap[:, c0:c0 + 512], in_=ps.ap[:, 0:512]), [ps], [dst])


def build_phase_c(T, final=False, NCH=32):
    nc = bass.Bass("TRN2", target_bir_lowering=False)
    NT = T // 128
    with ExitStack() as st:
        P = Prog(nc, st)
        x = P.dram("x", [T, 1024], F32, kind="ExternalInput")
        oT = P.dram("oT", [1024, T], F32, kind="ExternalInput")
        w_out = P.dram("w_out", [1024, 1024], F32, kind="ExternalInput")
        cT = P.dram("cT", [128, 8], F32, kind="ExternalInput")
        modw = P.dram("modw", [1024, 4096], F32, kind="ExternalInput")
        modb = P.dram("modb", [1, 4096], F32, kind="ExternalInput")
        nrm = P.dram("nrm", [1, 1024], F32, kind="ExternalInput")
        fnrm = P.dram("fnrm", [1, 1024], F32, kind="ExternalInput")
        w_q = P.dram("w_q", [1024, 2048], F32, kind="ExternalInput")
        skT = P.dram("skT", [128, 16, 128], F32, kind="ExternalInput")
        uT = P.dram("uT", [1024, NCH * 512], F32, kind="ExternalInput")
        vv = P.dram("vv", [NCH * 512, 1024], F32, kind="ExternalInput")
        ident_d = P.dram("ident", [128, 128], F32, kind="ExternalInput")
        xo = P.dram("xo", [T, 1024], F32, kind="ExternalOutput")
        sc_s = P.dram("sc_s", [NT, 128, 3, 8, 128], F32)
        sc_x1 = P.dram("sc_x1", [T, 1024], F32)
        sc_h2T = P.dram("sc_h2T", [NT, 128, 8, 128], BF16)
        uT_bf = P.dram("uT_bf", [1024, NCH * 512], BF16)
        vv_bf = P.dram("vv_bf", [NCH * 512, 1024], BF16)

        ident = P.sb([128, 128], F32, "ident_sb")
        identb = P.sb([128, 128], BF16, "identb")
        ones_row = P.sb([1, 128], F32, "ones_row")
        G1b = P.sb([128, 1024], F32, "G1b")
        A2b = P.sb([128, 1024], F32, "A2b")
        B2b = P.sb([128, 1024], F32, "B2b")
        G2b = P.sb([128, 1024], F32, "G2b")
        FNb = P.sb([128, 1024], F32, "FNb")
        P.dma(ident, ident_d)
        P.op("dve", lambda: nc.vector.tensor_copy(out=identb.ap, in_=ident.ap), [ident], [identb])
        P.op("dve", lambda: nc.vector.memset(ones_row.ap, 1.0), [], [ones_row])

        for i in range(8):
            P.dma(View(uT_bf, uT_bf.ap[i * 128:(i + 1) * 128, :]), View(uT, uT.ap[i * 128:(i + 1) * 128, :]), q="pool")
        NV = NCH * 512 // 8
        for i in range(8):
            P.dma(View(vv_bf, vv_bf.ap[i * NV:(i + 1) * NV, :]), View(vv, vv.ap[i * NV:(i + 1) * NV, :]), q="pool")
        with P.scope():
            P1 = P.ps([128, 1024], F32, "P1a")
            cond = P.sb([128, 8], F32, "cond")
            modrow = P.sb([1, 4096], F32, "modrow")
            mb = P.sb([1, 4096], F32, "mb")
            nr = P.sb([1, 1024], F32, "nr")
            fn = P.sb([1, 1024], F32, "fn")
            a2row = P.sb([1, 1024], F32, "a2row")
            mwc = [P.sb([128, 8, 512], F32, f"mwc{i}") for i in range(2)]
            P.dma(cond, cT)
            P.dma(mb, modb, q="act")
            P.dma(nr, nrm, q="act")
            P.dma(fn, fnrm, q="act")
            P.op("act", lambda: nc.scalar.activation(out=cond.ap, in_=cond.ap, func=AF.Silu), [cond], [cond])
            mw_v = modw.ap.rearrange("(kc p) o -> p kc o", p=128)
            for ch in range(8):
                buf = mwc[ch % 2]
                P.dma(buf, View(modw, mw_v[:, :, ch * 512:(ch + 1) * 512]), q="sp" if ch % 2 == 0 else "act")
                for kc in range(8):
                    P.op("pe", lambda: nc.tensor.matmul(P1.ap[0:1, 0:512], lhsT=cond.ap[:, kc:kc + 1], rhs=buf.ap[:, kc, :], start=(kc == 0), stop=(kc == 7)), [cond, buf], [P1])
                P.op("dve", lambda: nc.vector.tensor_tensor(out=modrow.ap[0:1, ch * 512:(ch + 1) * 512], in0=P1.ap[0:1, 0:512], in1=mb.ap[0:1, ch * 512:(ch + 1) * 512], op=OP.add), [P1, mb], [modrow])
            P.op("dve", lambda: nc.vector.scalar_tensor_tensor(out=a2row.ap, in0=modrow.ap[0:1, 2048:3072], scalar=1.0, in1=nr.ap, op0=OP.add, op1=OP.mult), [modrow, nr], [a2row])
            g1row = View(modrow, modrow.ap[0:1, 0:1024])
            b2row = View(modrow, modrow.ap[0:1, 1024:2048])
            g2row = View(modrow, modrow.ap[0:1, 3072:4096])
            for (row, dst) in ((g1row, G1b), (a2row, A2b), (b2row, B2b), (g2row, G2b), (fn, FNb)):
                for c0 in range(0, 1024, 512):
                    rap = _ap(row)[0:1, c0:c0 + 512]
                    P.op("pe", lambda: nc.tensor.matmul(P1.ap[:, 0:512], lhsT=ones_row.ap[0:1, 0:128], rhs=rap, start=True, stop=True), [ones_row, row], [P1])
                    P.op("act", lambda: nc.scalar.copy(out=dst.ap[:, c0:c0 + 512], in_=P1.ap[:, 0:512]), [P1], [dst])

        with P.scope():
            P1 = P.ps([128, 1024], F32, "P1b")
            wo = P.sb([128, 8, 1024], BF16, "wo")
            wq = P.sb([128, 8, 2048], F32, "wq")
            sk = P.sb([128, 16, 128], F32, "sk")
            P.dma(wo, View(w_out, w_out.ap.rearrange("(kc p) o -> p kc o", p=128)), q="pool")
            for i in range(4):
                P.dma(View(wq, wq.ap[:, 2 * i:2 * i + 2, :]), View(w_q, w_q.ap.rearrange("(kc p) o -> p kc o", p=128)[:, 2 * i:2 * i + 2, :]), q="act" if i % 2 else "sp")
            P.dma(sk, skT, q="sp")
            xt = P.sb([128, 1024], F32, "xt")
            ot = P.sb([128, 8, 128], BF16, "ot")
            h2Tb = P.sb([128, 8, 128], BF16, "h2Tb")
            x1 = P.sb([128, 1024], F32, "x1")
            h2 = P.sb([128, 1024], F32, "h2")
            h2Tf = P.sb([128, 8, 128], F32, "h2Tf")
            qT = P.sb([128, 16, 128], F32, "qT")
            S3 = P.sb([128, 3, 8, 128], F32, "S3")
            s_sb = P.sb([128, 16, 128], F32, "s_sb")
            s_r = P.sb([128, 16, 128], F32, "s_r")
            v16 = P.sb([128, 16, 16], F32, "v16")
            cand = P.sb([128, 8, 256], F32, "cand")
            c16 = P.sb([128, 8, 16], F32, "c16")
            ec = P.sb([128, 8, 16], F32, "ec")
            st4 = P.sb([128, 4, 8], F32, "st4")
            ss = P.sb([128, 2], F32, "ss")
            oT_v = oT.ap.rearrange("(kc p) t -> p kc t", p=128)
            for ti in range(NT):
              if True:
                tsl = slice(ti * 128, (ti + 1) * 128)
                P.dma(xt, View(x, x.ap[tsl, :]), q="sp")
                P.dma(ot, View(oT, oT_v[:, :, tsl]), q="pool")
                for hf in range(2):
                    for kc in range(8):
                        P.op("pe", lambda: nc.tensor.matmul(P1.ap[:, hf * 512:(hf + 1) * 512], lhsT=ot.ap[:, kc, :], rhs=wo.ap[:, kc, hf * 512:(hf + 1) * 512], start=(kc == 0), stop=(kc == 7)), [ot, wo], [P1])
                P.op("dve", lambda: nc.vector.tensor_tensor(out=x1.ap, in0=P1.ap, in1=G1b.ap, op=OP.mult), [P1, G1b], [x1])
                P.op("pool", lambda: nc.gpsimd.tensor_tensor(out=x1.ap, in0=x1.ap, in1=xt.ap, op=OP.add), [x1, xt], [x1])
                P.dma(View(sc_x1, sc_x1.ap[tsl, :]), x1, q="sp")
                P.op("act", lambda: nc.scalar.activation(out=h2.ap, in_=x1.ap, func=AF.Square, accum_out=ss.ap[:, 0:1]), [x1], [h2, ss])
                P.op("dve", lambda: nc.vector.tensor_scalar(out=ss.ap[:, 1:2], in0=ss.ap[:, 0:1], scalar1=1.0 / 1024, scalar2=EPS, op0=OP.mult, op1=OP.add), [ss], [ss])
                P.op("act", lambda: nc.scalar.activation(out=ss.ap[:, 1:2], in_=ss.ap[:, 1:2], func=AF.Sqrt), [ss], [ss])
                P.op("dve", lambda: nc.vector.reciprocal(out=ss.ap[:, 1:2], in_=ss.ap[:, 1:2]), [ss], [ss])
                P.op("dve", lambda: nc.vector.scalar_tensor_tensor(out=h2.ap, in0=x1.ap, scalar=ss.ap[:, 1:2], in1=A2b.ap, op0=OP.mult, op1=OP.mult), [x1, ss, A2b], [h2])
                P.op("pool", lambda: nc.gpsimd.tensor_tensor(out=h2.ap, in0=h2.ap, in1=B2b.ap, op=OP.add), [h2, B2b], [h2])
                for kc in range(8):
                    P.op("pe", lambda: nc.tensor.transpose(P1.ap[:, kc * 128:(kc + 1) * 128], h2.ap[:, kc * 128:(kc + 1) * 128], ident.ap), [h2, ident], [P1])
                P.op("act", lambda: nc.scalar.copy(out=h2Tf.ap.rearrange("p a b -> p (a b)"), in_=P1.ap), [P1], [h2Tf])
                P.op("dve", lambda: nc.vector.tensor_copy(out=h2Tb.ap, in_=h2Tf.ap), [h2Tf], [h2Tb])
                P.dma(View(sc_h2T, sc_h2T.ap[ti]), h2Tb, q="sp")
                for rnd in range(2):
                    for j in range(8):
                        hp = rnd * 8 + j
                        for kc in range(8):
                            P.op("pe", lambda: nc.tensor.matmul(P1.ap[:, j * 128:(j + 1) * 128], lhsT=wq.ap[:, kc, hp * 128:(hp + 1) * 128], rhs=h2Tf.ap[:, kc, :], start=(kc == 0), stop=(kc == 7)), [wq, h2Tf], [P1])
                    P.op("act", lambda: nc.scalar.copy(out=qT.ap[:, rnd * 8:(rnd + 1) * 8, :].rearrange("p a b -> p (a b)"), in_=P1.ap), [P1], [qT])
                for rnd in range(2):
                    for j in range(8):
                        hp = rnd * 8 + j
                        P.op("pe", lambda: nc.tensor.matmul(P1.ap[:, j * 128:(j + 1) * 128], lhsT=qT.ap[:, hp, :], rhs=sk.ap[:, hp, :], start=True, stop=True), [qT, sk], [P1])
                    P.op("act", lambda: nc.scalar.copy(out=s_sb.ap[:, rnd * 8:(rnd + 1) * 8, :].rearrange("p a b -> p (a b)"), in_=P1.ap), [P1], [s_sb])
                for hp in range(16):
                    P.op("dve", lambda: nc.vector.max(out=v16.ap[:, hp, 0:8], in_=s_sb.ap[:, hp, :]), [s_sb], [v16])
                    P.op("dve", lambda: nc.vector.match_replace(out=s_r.ap[:, hp, :], in_to_replace=v16.ap[:, hp, 0:8], in_values=s_sb.ap[:, hp, :], imm_value=NEG), [s_sb, v16], [s_r])
                    P.op("dve", lambda: nc.vector.max(out=v16.ap[:, hp, 8:16], in_=s_r.ap[:, hp, :]), [s_r], [v16])
                v16v = v16.ap.rearrange("p (h two) k -> p h two k", two=2)
                P.op("dve", lambda: nc.vector.tensor_tensor(out=cand.ap.rearrange("p h (i j) -> p h i j", i=16), in0=v16v[:, :, 0, :].unsqueeze(3).to_broadcast([128, 8, 16, 16]), in1=v16v[:, :, 1, :].unsqueeze(2).to_broadcast([128, 8, 16, 16]), op=OP.add), [v16], [cand])
                for h in range(8):
                    P.op("dve", lambda: nc.vector.max(out=c16.ap[:, h, 0:8], in_=cand.ap[:, h, :]), [cand], [c16])
                    P.op("dve", lambda: nc.vector.match_replace(out=s_r.ap.rearrange("p a b -> p (a b)").rearrange("p (h c) -> p h c", h=8)[:, h, :], in_to_replace=c16.ap[:, h, 0:8], in_values=cand.ap[:, h, :], imm_value=NEG), [cand, c16], [s_r])
                    P.op("dve", lambda: nc.vector.max(out=c16.ap[:, h, 8:16], in_=s_r.ap.rearrange("p a b -> p (a b)").rearrange("p (h c) -> p h c", h=8)[:, h, :]), [s_r], [c16])
                P.op("dve", lambda: nc.vector.tensor_tensor(out=ec.ap, in0=c16.ap, in1=c16.ap[:, :, 0:1].to_broadcast([128, 8, 16]), op=OP.subtract), [c16], [ec])
                P.op("act", lambda: nc.scalar.activation(out=ec.ap, in_=ec.ap, func=AF.Exp), [ec], [ec])
                P.op("dve", lambda: nc.vector.tensor_reduce(out=st4.ap[:, 1, :], in_=ec.ap, axis=AX.X, op=OP.add), [ec], [st4])
                P.op("act", lambda: nc.scalar.activation(out=st4.ap[:, 2, :], in_=st4.ap[:, 1, :], func=AF.Ln), [st4], [st4])
                P.op("dve", lambda: nc.vector.tensor_tensor(out=st4.ap[:, 2, :], in0=st4.ap[:, 2, :], in1=c16.ap[:, :, 0], op=OP.add), [st4, c16], [st4])
                P.op("dve", lambda: nc.vector.tensor_scalar(out=st4.ap[:, 3, :], in0=c16.ap[:, :, 15], scalar1=-1e-3, scalar2=None, op0=OP.add), [c16], [st4])
                s_v = s_sb.ap.rearrange("p (h two) n -> p h two n", two=2)
                P.op("dve", lambda: nc.vector.tensor_tensor(out=S3.ap[:, 0], in0=s_v[:, :, 0, :], in1=st4.ap[:, 2, :].unsqueeze(2).to_broadcast([128, 8, 128]), op=OP.subtract), [s_sb, st4], [S3])
                P.op("dve", lambda: nc.vector.tensor_tensor(out=st4.ap[:, 0, :], in0=st4.ap[:, 3, :], in1=st4.ap[:, 2, :], op=OP.subtract), [st4], [st4])
                P.op("dve", lambda: nc.vector.memset(S3.ap[:, 1], 0.0), [], [S3])
                P.op("act", lambda: nc.scalar.activation(out=S3.ap[:, 1, :, 0], in_=st4.ap[:, 0, :], func=AF.Exp), [st4], [S3])
                P.op("pool", lambda: nc.gpsimd.tensor_copy(out=S3.ap[:, 2], in_=s_v[:, :, 1, :]), [s_sb], [S3])
                P.dma(View(sc_s, sc_s.ap[ti]), S3, q="act")

        with P.scope():
            uc = [P.sb([128, 8, 512], BF16, f"uc{i}") for i in range(2)]
            vc = [P.sb([128, 4, 1024], BF16, f"vc{i}") for i in range(3)]
            S3s = [P.sb([128, 3, 8, 128], F32, f"S3b{i}") for i in range(4)]
            x1s = [P.sb([128, 1024], F32, f"x1b{i}") for i in range(4)]
            h2Ts = [P.sb([128, 8, 128], BF16, f"h2Tt{i}") for i in range(4)]
            sumE = [P.sb([128, 8, 4, 128], F32, f"sumE{i}") for i in range(3)]
            Gh = [P.sb([128, 8, 512], BF16, f"Gh{i}") for i in range(2)]
            actT = [P.sb([128, 512], BF16, f"actT{i}") for i in range(3)]
            gaT = [P.sb([128, 4, 128], BF16, f"gaT{i}") for i in range(2)]
            xo_t = P.sb([128, 1024], F32, "xo_t")
            ss = P.sb([128, 2], F32, "ss2")
            ACCs = [P.ps([128, 1024], F32, f"ACC{i}") for i in range(2)]
            PA = [P.ps([128, 512], F32, f"PA{i}") for i in range(2)]
            GTs = [P.ps([128, 512], F32, "GT0")] * 2
            PT = P.ps([128, 512], BF16, "PT")
            ga = P.sb([128, 512], BF16, "ga")
            uT_v = uT_bf.ap.rearrange("(kc p) e -> p kc e", p=128)
            vv_v = vv_bf.ap.rearrange("(b p) d -> p b d", p=128)
            assert NT % 2 == 0
            items = []
            for tp in range(NT // 2):
                for ci in range(NCH):
                    items.append((2 * tp, ci, 0))
                    items.append((2 * tp + 1, ci, 1))

            def load_tile(ti):
                P.dma(S3s[ti % 4], View(sc_s, sc_s.ap[ti]), q="sp")
                P.dma(x1s[ti % 4], View(sc_x1, sc_x1.ap[ti * 128:(ti + 1) * 128, :]), q="sp")
                P.dma(h2Ts[ti % 4], View(sc_h2T, sc_h2T.ap[ti]), q="sp")

            def stage1(idx):
                ti, ci, sub = items[idx]
                b = idx % 2
                b3 = idx % 3
                k = idx // 2
                S3 = S3s[ti % 4]
                h2T = h2Ts[ti % 4]
                if ci == 2 and sub == 0 and ti + 2 < NT:
                    load_tile(ti + 2)
                    load_tile(ti + 3)
                if sub == 0:
                    P.dma(uc[k % 2], View(uT_bf, uT_v[:, :, ci * 512:(ci + 1) * 512]), q="sp")
                    P.dma(vc[k % 3], View(vv_bf, vv_v[:, ci * 4:(ci + 1) * 4, :]), q="act")
                for kc in range(8):
                    P.op("pe", lambda: nc.tensor.matmul(PA[b].ap, lhsT=h2T.ap[:, kc, :], rhs=uc[k % 2].ap[:, kc, :], start=(kc == 0), stop=(kc == 7)), [h2T, uc[k % 2]], [PA[b]])
                P.op("act", lambda: nc.scalar.activation(out=actT[b3].ap, in_=PA[b].ap, func=AF.Gelu), [PA[b]], [actT[b3]])
                s1e_b = S3.ap[:, 0, :, ci * 4:(ci + 1) * 4].unsqueeze(3).to_broadcast([128, 8, 4, 128])
                s2_b = S3.ap[:, 2].unsqueeze(2).to_broadcast([128, 8, 4, 128])
                P.op("pool", lambda: nc.gpsimd.tensor_tensor(out=sumE[b3].ap, in0=s1e_b, in1=s2_b, op=OP.add), [S3], [sumE[b3]])
                P.op("act", lambda: nc.scalar.activation(out=sumE[b3].ap, in_=sumE[b3].ap, func=AF.Exp), [sumE[b3]], [sumE[b3]])

            def stage2a(idx):
                ti, ci, sub = items[idx]
                b = idx % 2
                S3 = S3s[ti % 4]
                GT = GTs[b]
                Ev = sumE[idx % 3].ap.rearrange("p h a n -> p h (a n)")
                for h in range(8):
                    P.op("dve", lambda: nc.vector.scalar_tensor_tensor(out=Gh[b].ap[:, h, :], in0=Ev[:, h, :], scalar=S3.ap[:, 1, h, 0:1], in1=Ev[:, h, :], op0=OP.is_ge, op1=OP.mult), [sumE[idx % 3], S3], [Gh[b]], indep=True)
                for h in range(8):
                    P.op("pe", lambda: nc.tensor.matmul(GT.ap, lhsT=identb.ap, rhs=Gh[b].ap[:, h, :], start=(h == 0), stop=(h == 7)), [Gh[b], identb], [GT])

            def stage2b(idx):
                ti, ci, sub = items[idx]
                b = idx % 2
                b3 = idx % 3
                k = idx // 2
                GT = GTs[b]
                ACC = ACCs[sub]
                P.op("dve", lambda: nc.vector.tensor_tensor(out=ga.ap, in0=GT.ap, in1=actT[b3].ap, op=OP.mult), [GT, actT[b3]], [ga])
                for bb in range(4):
                    P.op("pe", lambda: nc.tensor.transpose(PT.ap[:, bb * 128:(bb + 1) * 128], ga.ap[:, bb * 128:(bb + 1) * 128], identb.ap), [ga, identb], [PT])
                P.op("dve", lambda: nc.vector.tensor_copy(out=gaT[b].ap.rearrange("p a b -> p (a b)"), in_=PT.ap), [PT], [gaT[b]])
                for bb in range(4):
                    for hf in range(2):
                        P.op("pe", lambda: nc.tensor.matmul(ACC.ap[:, hf * 512:(hf + 1) * 512], lhsT=gaT[b].ap[:, bb, :], rhs=vc[k % 3].ap[:, bb, hf * 512:(hf + 1) * 512], start=(ci == 0 and bb == 0), stop=(ci == NCH - 1 and bb == 3)), [gaT[b], vc[k % 3]], [ACC])

            def epilogue(ti, sub, idx):
                x1 = x1s[ti % 4]
                jb = sumE[idx % 3]
                ACC = ACCs[sub]
                P.op("dve", lambda: nc.vector.tensor_tensor(out=xo_t.ap, in0=ACC.ap, in1=G2b.ap, op=OP.mult), [ACC, G2b], [xo_t])
                P.op("pool", lambda: nc.gpsimd.tensor_tensor(out=xo_t.ap, in0=xo_t.ap, in1=x1.ap, op=OP.add), [xo_t, x1], [xo_t])
                if final:
                    P.op("act", lambda: nc.scalar.activation(out=jb.ap.rearrange("p h a n -> p (h a n)")[:, 0:1024], in_=xo_t.ap, func=AF.Square, accum_out=ss.ap[:, 0:1]), [xo_t], [jb, ss])
                    P.op("dve", lambda: nc.vector.tensor_scalar(out=ss.ap[:, 1:2], in0=ss.ap[:, 0:1], scalar1=1.0 / 1024, scalar2=EPS, op0=OP.mult, op1=OP.add), [ss], [ss])
                    P.op("act", lambda: nc.scalar.activation(out=ss.ap[:, 1:2], in_=ss.ap[:, 1:2], func=AF.Sqrt), [ss], [ss])
                    P.op("dve", lambda: nc.vector.reciprocal(out=ss.ap[:, 1:2], in_=ss.ap[:, 1:2]), [ss], [ss])
                    P.op("dve", lambda: nc.vector.scalar_tensor_tensor(out=xo_t.ap, in0=xo_t.ap, scalar=ss.ap[:, 1:2], in1=FNb.ap, op0=OP.mult, op1=OP.mult), [xo_t, ss, FNb], [xo_t])
                P.dma(View(xo, xo.ap[ti * 128:(ti + 1) * 128, :]), xo_t, q="sp")

            load_tile(0)
            load_tile(1)
            NI = len(items)
            stage1(0)
            stage1(1)
            stage2a(0)
            for idx in range(NI):
                if idx + 2 < NI:
                    stage1(idx + 2)
                stage2b(idx)
                if items[idx][1] == NCH - 1:
                    epilogue(items[idx][0], items[idx][2], idx)
                if idx + 1 < NI:
                    stage2a(idx + 1)
        P.barrier()
        P.finish([xo])
        print("phase C: ninst", P.ninst, "nwait", P.nwait)
    return nc


SEQ = 16384
NCORE = 8
TOK = SEQ // NCORE
_PROGS = {}
f32 = np.float32


def build_phase_b_even(S):
    nc = bass.Bass("TRN2", target_bir_lowering=False)
    NBLK = S // 256
    with ExitStack() as st:
        P = Prog(nc, st)
        D = {}
        for nm in ("xq", "xk", "xv"):
            D[nm] = P.dram(nm, [128, S + 3], F32, kind="ExternalInput")
        D["cw"] = P.dram("cw", [128, 12], F32, kind="ExternalInput")
        D["z"] = P.dram("z", [S, 128], F32, kind="ExternalInput")
        D["ba"] = P.dram("ba", [128, S // 128, 2], F32, kind="ExternalInput")
        D["hc"] = P.dram("hc", [128, 2], F32, kind="ExternalInput")
        D["onorm"] = P.dram("onorm", [128, 128], F32, kind="ExternalInput")
        for nm in ("ident", "tri", "maskS", "maskI"):
            D[nm] = P.dram(nm, [128, 128], F32, kind="ExternalInput")
        D["OA"] = P.dram("OA", [S, 128], F32, kind="ExternalOutput")
        for nm in ("qT", "qTs", "kT", "kTs"):
            D[nm] = P.dram(nm, [128, S], F32, kind="ExternalInput")
        D["v"] = P.dram("v", [S, 128], F32, kind="ExternalInput")
        D["pos"] = P.dram("pos", [1, S], I32, kind="ExternalInput")
        D["cst"] = P.dram("cst", [128, 2], F32, kind="ExternalInput")
        D["maskT"] = P.dram("maskT", [128, 128], F32, kind="ExternalInput")
        D["selT"] = P.dram("selT", [NBLK, NBLK * 128], F32, kind="ExternalInput")
        D["OT"] = P.dram("OT", [128, S], F32, kind="ExternalOutput")
        with P.scope():
            gdn_body(P, nc, S, D)
        with P.scope():
            moba_body(P, nc, S, D)
        P.barrier()
        P.finish([D["OA"], D["OT"]])
    return nc


def _prog(key):
    if key not in _PROGS:
        if key == "A_even":
            _PROGS[key] = build_phase_a(TOK, 3592)
        elif key == "A_odd":
            _PROGS[key] = build_phase_a(TOK, 832)
        elif key == "B_even":
            _PROGS[key] = build_phase_b_even(SEQ)
        elif key == "B_odd":
            _PROGS[key] = build_phase_b_mla(SEQ)
        elif key == "C":
            _PROGS[key] = build_phase_c(TOK, final=False)
        elif key == "C_final":
            _PROGS[key] = build_phase_c(TOK, final=True)
    return _PROGS[key]


def _run(key, in_maps):
    res = run_bass_kernel_spmd(_prog(key), in_maps, core_ids=list(range(NCORE)))
    return res.results


def _c(a):
    return np.ascontiguousarray(a)


def kernel(x, c, positions, mod_w, mod_b, norm_mix, norm_ffn, hy_w_in, gdn_conv, gdn_a_log,
           gdn_dt_bias, gdn_o_norm, hy_w_out, mla_w_in, mla_q_norm, mla_kv_norm, mla_w_uq,
           mla_w_ukv, mla_w_out, peer_w_q, peer_sub_keys, peer_u, peer_v, final_norm):
    A_ = lambda a: np.asarray(a)
    x, c, positions, mod_w, mod_b = A_(x), A_(c), A_(positions), A_(mod_w), A_(mod_b)
    norm_mix, norm_ffn, hy_w_in, gdn_conv = A_(norm_mix), A_(norm_ffn), A_(hy_w_in), A_(gdn_conv)
    gdn_a_log, gdn_dt_bias, gdn_o_norm, hy_w_out = A_(gdn_a_log), A_(gdn_dt_bias), A_(gdn_o_norm), A_(hy_w_out)
    mla_w_in, mla_q_norm, mla_kv_norm, mla_w_uq = A_(mla_w_in), A_(mla_q_norm), A_(mla_kv_norm), A_(mla_w_uq)
    mla_w_ukv, mla_w_out, peer_w_q, peer_sub_keys = A_(mla_w_ukv), A_(mla_w_out), A_(peer_w_q), A_(peer_sub_keys)
    peer_u, peer_v, final_norm = A_(peer_u), A_(peer_v), A_(final_norm)
    S = SEQ
    xcur = _c(x[0].astype(f32, copy=False))
    cT = _c(c.reshape(8, 128).T)
    pos = _c(positions.reshape(1, S).astype(np.int32, copy=False))
    I = np.eye(128, dtype=f32)
    tri = np.triu(np.ones((128, 128), f32))
    maskS = np.tril(np.ones((128, 128), f32), -1)
    maskI = np.tril(np.ones((128, 128), f32))
    maskT = np.triu(np.ones((128, 128), f32))
    NBLK = S // 256
    selT = np.kron(np.eye(NBLK, dtype=f32), np.ones((1, 128), f32))
    invf_b = (10000.0 ** (-np.arange(0, 128, 2, dtype=np.float32) / 128)).astype(f32)
    cst_b = np.zeros((128, 2), f32); cst_b[:, 0] = np.concatenate([invf_b, invf_b]); cst_b[:64, 1] = -1; cst_b[64:, 1] = 1
    invf_c = (10000.0 ** (-np.arange(0, 64, 2, dtype=np.float32) / 64)).astype(f32)
    cst_c = np.zeros((128, 2), f32); cst_c[:64, 0] = np.concatenate([invf_c, invf_c]); cst_c[:32, 1] = -1; cst_c[32:64, 1] = 1
    sw = lambda a: _c(np.concatenate([a[a.shape[0] // 2:], a[:a.shape[0] // 2]], 0))

    for l in range(4):
        i = l // 2
        even = (l % 2 == 0)
        W = hy_w_in[i] if even else mla_w_in[i]
        modw_a = _c(mod_w[l][:, 0:2048]); modb_a = _c(mod_b[l][None, 0:2048]); nrm_a = _c(norm_mix[l][None])
        in_maps = [dict(x=xcur[k * TOK:(k + 1) * TOK], cT=cT, modw=modw_a, modb=modb_a, nrm=nrm_a, W=_c(W), ident=I) for k in range(NCORE)]
        res = _run("A_even" if even else "A_odd", in_maps)
        Y = np.concatenate([r["Y"] for r in res], 0)
        if even:
            in_maps = []
            for k in range(NCORE):
                h = k % 4
                def xT(off):
                    a = Y[:, off + h * 128: off + (h + 1) * 128].T
                    return _c(np.concatenate([np.zeros((128, 3), f32), a], 1))
                cw = _c(np.concatenate([gdn_conv[i][:, off + h * 128: off + (h + 1) * 128].T for off in (0, 512, 1024)], 1))
                ba = _c(np.stack([Y[:, 2048 + h], Y[:, 2052 + h]], -1).reshape(S // 128, 128, 2).transpose(1, 0, 2))
                hc = _c(np.tile(np.array([[gdn_a_log[i][h], gdn_dt_bias[i][h]]], f32), (128, 1)))
                qT = _c(Y[:, 2056 + h * 128: 2056 + (h + 1) * 128].T)
                kT = _c(Y[:, 2568 + h * 128: 2568 + (h + 1) * 128].T)
                in_maps.append(dict(xq=xT(0), xk=xT(512), xv=xT(1024), cw=cw, z=_c(Y[:, 1536 + h * 128: 1536 + (h + 1) * 128]),
                                    ba=ba, hc=hc, onorm=_c(np.tile(gdn_o_norm[i][None], (128, 1))), ident=I, tri=tri, maskS=maskS, maskI=maskI,
                                    qT=qT, qTs=sw(qT), kT=kT, kTs=sw(kT), v=_c(Y[:, 3080 + h * 128: 3080 + (h + 1) * 128]),
                                    pos=pos, cst=cst_b, maskT=maskT, selT=selT))
            res = _run("B_even", in_maps)
            oT = np.concatenate([res[h]["OA"].T for h in range(4)] + [res[h]["OT"] for h in range(4)], 0)
            w_out = hy_w_out[i]
        else:
            YT = _c(Y.T)
            qn = _c(mla_q_norm[i].reshape(4, 128).T); kvn = _c(mla_kv_norm[i].reshape(2, 128).T)
            krT = _c(YT[768:832]); krTs = sw(krT)
            in_maps = []
            for h in range(NCORE):
                wq = mla_w_uq[i][:, h * 192:(h + 1) * 192]; wkv = mla_w_ukv[i][:, h * 256:(h + 1) * 256]
                wr = wq[:, 128:]
                in_maps.append(dict(cqT=YT[:512], ckvT=YT[512:768], krT=krT, krTs=krTs, pos=pos, qn=qn, kvn=kvn,
                                    wuq_n=_c(wq[:, :128]), wuq_r=_c(wr), wuq_rs=_c(np.concatenate([wr[:, 32:], wr[:, :32]], 1)),
                                    wukv_k=_c(wkv[:, :128]), wukv_v=_c(wkv[:, 128:]), cst=cst_c, maskT=maskT))
            res = _run("B_odd", in_maps)
            oT = np.concatenate([res[h]["OT"] for h in range(NCORE)], 0)
            w_out = mla_w_out[i]
        modw_c = _c(mod_w[l][:, 2048:6144]); modb_c = _c(mod_b[l][None, 2048:6144])
        skT = _c(peer_sub_keys[l].reshape(16, 128, 128).transpose(2, 0, 1))
        uT = _c(peer_u[l].T)
        vv = _c(peer_v[l])
        in_maps = [dict(x=xcur[k * TOK:(k + 1) * TOK], oT=_c(oT[:, k * TOK:(k + 1) * TOK]), w_out=_c(w_out), cT=cT, modw=modw_c, modb=modb_c,
                        nrm=_c(norm_ffn[l][None]), fnrm=_c(final_norm[None]), w_q=_c(peer_w_q[l]), skT=skT, uT=uT, vv=vv, ident=I) for k in range(NCORE)]
        res = _run("C_final" if l == 3 else "C", in_maps)
        xcur = np.concatenate([r["xo"] for r in res], 0)
    return xcur.reshape(1, S, 1024).astype(f32, copy=False)
```

```python
import math
import numpy as np
from contextlib import ExitStack
import concourse.bass as bass
import concourse.mybir as mybir
from concourse.bass_utils import run_bass_kernel_spmd


F32 = mybir.dt.float32
BF16 = mybir.dt.bfloat16
I32 = mybir.dt.int32
AF = mybir.ActivationFunctionType
OP = mybir.AluOpType
AX = mybir.AxisListType


SAME_ENGINE_SYNC = True


class Buf:
    __slots__ = ("ap", "w", "r", "dsem", "dcnt", "name", "is_dram", "is_psum")

    def __init__(self, ap, name=""):
        self.ap = ap
        self.w = None
        self.r = {}
        self.dsem = None
        self.dcnt = 0
        self.name = name
        self.is_dram = False
        self.is_psum = False

    def __getitem__(self, idx):
        return View(self, self.ap[idx])


class View:
    __slots__ = ("buf", "ap")

    def __init__(self, buf, ap):
        self.buf = buf
        self.ap = ap

    def __getitem__(self, idx):
        return View(self.buf, self.ap[idx])


def _b(x):
    return x.buf if isinstance(x, View) else x


def _ap(x):
    if isinstance(x, (Buf, View)):
        return x.ap
    return x


class Prog:
    ENG = ("pe", "act", "dve", "pool", "sp")

    def __init__(self, nc, stack, same_engine_sync=None):
        self.nc = nc
        self.stack = stack
        self.e = {"pe": nc.tensor, "act": nc.scalar, "dve": nc.vector, "pool": nc.gpsimd, "sp": nc.sync}
        self.sem = {k: stack.enter_context(nc.semaphore("s_" + k)) for k in self.ENG}
        self.cnt = {k: 0 for k in self.ENG}
        self.seen = {k: {} for k in self.ENG}
        self.same = SAME_ENGINE_SYNC if same_engine_sync is None else same_engine_sync
        self.ninst = 0
        self.nwait = 0
        self._dsems = []
        self._dbufs = []
        self._free_dsems = []
        self._scopes = []
        self.top = stack

    def sb(self, shape, dt=F32, name=None):
        t = self.stack.enter_context(self.nc.sbuf_tensor(name or f"sb{self.ninst}_{len(self._dsems)}_{np.random.randint(1<<30)}", list(shape), dt))
        return Buf(t.ap() if hasattr(t, "ap") and callable(getattr(t, "ap")) else t[:], name or "")

    def ps(self, shape, dt=F32, name=None):
        t = self.stack.enter_context(self.nc.psum_tensor(name or f"ps{np.random.randint(1<<30)}", list(shape), dt))
        b = Buf(t.ap() if hasattr(t, "ap") and callable(getattr(t, "ap")) else t[:], name or "")
        b.is_psum = True
        return b

    def dram(self, name, shape, dt=F32, kind="Internal"):
        t = self.nc.dram_tensor(name, list(shape), dt, kind=kind)
        b = Buf(t.ap(), name)
        b.is_dram = True
        return b

    def _need(self, eng, dep):
        if dep is None:
            return
        kind, key, count = dep
        if kind == "eng":
            if key == eng and (eng == "pe" or not self.same):
                return
            sem = self.sem[key]
            skey = key
        else:
            sem = key
            skey = ("d", id(key))
        if self.seen[eng].get(skey, 0) >= count:
            return
        self.e[eng].wait_ge(sem, count)
        self.seen[eng][skey] = count
        self.nwait += 1

    def _deps(self, eng, reads, writes):
        for x in reads:
            b = _b(x)
            self._need(eng, b.w)
            if b.is_psum:
                for k, d in b.r.items():
                    if k != eng:
                        self._need(eng, d)
        for x in writes:
            b = _b(x)
            self._need(eng, b.w)
            for d in b.r.values():
                self._need(eng, d)

    def op(self, eng, fn, reads=(), writes=(), indep=False):
        if indep:
            sv = self.same
            self.same = False
            self._deps(eng, reads, writes)
            self.same = sv
        else:
            self._deps(eng, reads, writes)
        inst = fn()
        self.cnt[eng] += 1
        c = self.cnt[eng]
        inst.then_inc(self.sem[eng], 1)
        tag = ("eng", eng, c)
        for x in reads:
            _b(x).r[eng] = tag
        for x in writes:
            b = _b(x)
            b.w = tag
            b.r = {}
        self.ninst += 1
        return inst

    def dma(self, out, in_, q="sp", **kw):
        ob, ib = _b(out), _b(in_)
        self._deps(q, [ib], [ob])
        owner = ib if (ob.is_dram and not ib.is_dram) else ob
        if owner.dsem is None:
            if False:
                pass
            else:
                owner.dsem = self.top.enter_context(self.nc.semaphore(f"d{len(self._dsems)}"))
                owner.dcnt = 0
                self._dsems.append(owner.dsem)
            self._dbufs.append(owner)
            if self._scopes:
                self._scopes[-1].append(owner)
        inst = self.e[q].dma_start(out=_ap(out), in_=_ap(in_), **kw)
        owner.dcnt += 16
        inst.then_inc(owner.dsem, 16)
        tag = ("dma", owner.dsem, owner.dcnt)
        ob.w = tag
        ob.r = {}
        ib.r[("dma", id(owner.dsem))] = tag
        self.ninst += 1
        return inst

    def barrier(self):
        for e in self.ENG:
            for k in self.ENG:
                if k != e and self.cnt[k] > 0:
                    self._need(e, ("eng", k, self.cnt[k]))
        for b in self._dbufs:
            for e in self.ENG:
                if b.dcnt > 0:
                    self._need(e, ("dma", b.dsem, b.dcnt))

    def scope(self):
        return _Scope(self)

    def finish(self, bufs, eng="sp"):
        for b in bufs:
            self._need(eng, b.w)


class _Scope:
    def __init__(self, P):
        self.P = P

    def __enter__(self):
        self.es = ExitStack()
        self.es.__enter__()
        self.prev = self.P.stack
        self.P.stack = self.es
        self.P._scopes.append([])
        return self

    def __exit__(self, *a):
        P = self.P
        P.barrier()
        owners = P._scopes.pop()
        for b in owners:
            P._free_dsems.append((b.dsem, b.dcnt))
            P._dbufs.remove(b)
            b.dsem = None
        P.stack = self.prev
        return self.es.__exit__(*a)


EPS = 1e-6


def build_phase_a(T, O):
    nc = bass.Bass("TRN2", target_bir_lowering=False)
    NT = T // 128
    with ExitStack() as st:
        P = Prog(nc, st)
        x = P.dram("x", [T, 1024], F32, kind="ExternalInput")
        cT = P.dram("cT", [128, 8], F32, kind="ExternalInput")
        modw = P.dram("modw", [1024, 2048], F32, kind="ExternalInput")
        modb = P.dram("modb", [1, 2048], F32, kind="ExternalInput")
        nrm = P.dram("nrm", [1, 1024], F32, kind="ExternalInput")
        W = P.dram("W", [1024, O], F32, kind="ExternalInput")
        ident_d = P.dram("ident", [128, 128], F32, kind="ExternalInput")
        Y = P.dram("Y", [T, O], F32, kind="ExternalOutput")

        ident = P.sb([128, 128], F32, "ident_sb")
        ones_row = P.sb([1, 128], F32, "ones_row")
        A1b = P.sb([128, 1024], F32, "A1b")
        B1b = P.sb([128, 1024], F32, "B1b")
        P1 = P.ps([128, 1024], F32, "P1")
        P2 = P.ps([128, 1024], F32, "P2")
        P.dma(ident, ident_d)
        P.op("dve", lambda: nc.vector.memset(ones_row.ap, 1.0), [], [ones_row])
        with P.scope():
            cond = P.sb([128, 8], F32, "cond")
            modrow = P.sb([1, 2048], F32, "modrow")
            mb = P.sb([1, 2048], F32, "mb")
            nr = P.sb([1, 1024], F32, "nr")
            a1row = P.sb([1, 1024], F32, "a1row")
            mwc = [P.sb([128, 8, 512], F32, f"mwc{i}") for i in range(2)]
            P.dma(cond, cT)
            P.dma(mb, modb, q="act")
            P.dma(nr, nrm, q="act")
            P.op("act", lambda: nc.scalar.activation(out=cond.ap, in_=cond.ap, func=AF.Silu), [cond], [cond])
            mw_v = modw.ap.rearrange("(kc p) o -> p kc o", p=128)
            for ch in range(4):
                buf = mwc[ch % 2]
                P.dma(buf, View(modw, mw_v[:, :, ch * 512:(ch + 1) * 512]), q="sp" if ch % 2 == 0 else "act")
                for kc in range(8):
                    P.op("pe", lambda: nc.tensor.matmul(P1.ap[0:1, 0:512], lhsT=cond.ap[:, kc:kc + 1], rhs=buf.ap[:, kc, :], start=(kc == 0), stop=(kc == 7)), [cond, buf], [P1])
                P.op("dve", lambda: nc.vector.tensor_tensor(out=modrow.ap[0:1, ch * 512:(ch + 1) * 512], in0=P1.ap[0:1, 0:512], in1=mb.ap[0:1, ch * 512:(ch + 1) * 512], op=OP.add), [P1, mb], [modrow])
            P.op("dve", lambda: nc.vector.scalar_tensor_tensor(out=a1row.ap, in0=modrow.ap[0:1, 1024:2048], scalar=1.0, in1=nr.ap, op0=OP.add, op1=OP.mult), [modrow, nr], [a1row])
            b1row = View(modrow, modrow.ap[0:1, 0:1024])
            for (row, dst) in ((a1row, A1b), (b1row, B1b)):
                for c0 in range(0, 1024, 512):
                    rap = _ap(row)[0:1, c0:c0 + 512]
                    P.op("pe", lambda: nc.tensor.matmul(P1.ap[:, 0:512], lhsT=ones_row.ap[0:1, 0:128], rhs=rap, start=True, stop=True), [ones_row, row], [P1])
                    P.op("act", lambda: nc.scalar.copy(out=dst.ap[:, c0:c0 + 512], in_=P1.ap[:, 0:512]), [P1], [dst])
        with P.scope():
            Wsb = P.sb([128, 8, O], BF16, "Wsb")
            W_v = W.ap.rearrange("(kc p) o -> p kc o", p=128)
            for kc in range(8):
                P.dma(View(Wsb, Wsb.ap[:, kc, :]), View(W, W_v[:, kc, :]), q="pool")
            xt = [P.sb([128, 1024], F32, f"xt{i}") for i in range(2)]
            h = P.sb([128, 1024], F32, "h")
            hT = [P.sb([128, 8, 128], BF16, f"hT{i}") for i in range(2)]
            yt = [P.sb([128, 1024], F32, f"yt{i}") for i in range(2)]
            ss = P.sb([128, 2], F32, "ss")
            nyc = 0
            for ti in range(NT):
                tsl = slice(ti * 128, (ti + 1) * 128)
                xb = xt[ti % 2]
                hb = hT[ti % 2]
                P.dma(xb, View(x, x.ap[tsl, :]), q="sp")
                P.op("act", lambda: nc.scalar.activation(out=h.ap, in_=xb.ap, func=AF.Square, accum_out=ss.ap[:, 0:1]), [xb], [h, ss])
                P.op("dve", lambda: nc.vector.tensor_scalar(out=ss.ap[:, 1:2], in0=ss.ap[:, 0:1], scalar1=1.0 / 1024, scalar2=EPS, op0=OP.mult, op1=OP.add), [ss], [ss])
                P.op("act", lambda: nc.scalar.activation(out=ss.ap[:, 1:2], in_=ss.ap[:, 1:2], func=AF.Sqrt), [ss], [ss])
                P.op("dve", lambda: nc.vector.reciprocal(out=ss.ap[:, 1:2], in_=ss.ap[:, 1:2]), [ss], [ss])
                P.op("dve", lambda: nc.vector.scalar_tensor_tensor(out=h.ap, in0=xb.ap, scalar=ss.ap[:, 1:2], in1=A1b.ap, op0=OP.mult, op1=OP.mult), [xb, ss, A1b], [h])
                P.op("pool", lambda: nc.gpsimd.tensor_tensor(out=h.ap, in0=h.ap, in1=B1b.ap, op=OP.add), [h, B1b], [h])
                for kc in range(8):
                    P.op("pe", lambda: nc.tensor.transpose(P1.ap[:, kc * 128:(kc + 1) * 128], h.ap[:, kc * 128:(kc + 1) * 128], ident.ap), [h, ident], [P1])
                P.op("act", lambda: nc.scalar.copy(out=hb.ap.rearrange("p a b -> p (a b)"), in_=P1.ap), [P1], [hb])
                for o0 in range(0, O, 1024):
                    ow = min(1024, O - o0)
                    yb = yt[nyc % 2]
                    nyc += 1
                    for c0 in range(0, ow, 512):
                        cw = min(512, ow - c0)
                        for kc in range(8):
                            P.op("pe", lambda: nc.tensor.matmul(P2.ap[:, c0:c0 + cw], lhsT=hb.ap[:, kc, :], rhs=Wsb.ap[:, kc, o0 + c0:o0 + c0 + cw], start=(kc == 0), stop=(kc == 7)), [hb, Wsb], [P2])
                    if (nyc % 2) == 0:
                        P.op("act", lambda: nc.scalar.copy(out=yb.ap[:, 0:ow], in_=P2.ap[:, 0:ow]), [P2], [yb])
                    else:
                        P.op("dve", lambda: nc.vector.tensor_copy(out=yb.ap[:, 0:ow], in_=P2.ap[:, 0:ow]), [P2], [yb])
                    P.dma(View(Y, Y.ap[tsl, o0:o0 + ow]), View(yb, yb.ap[:, 0:ow]), q="act")
        P.barrier()
        P.finish([Y])
        print("phase A: ninst", P.ninst, "nwait", P.nwait)
    return nc


EPS = 1e-6
TWO_PI = 2.0 * math.pi
C1 = 6.28125
C2 = TWO_PI - C1


def rope_tables(P, nc, pos_i, posf, tmp, tmi, CS, SN, invf, sgn, HP):
    P.op("dve", lambda: nc.vector.tensor_copy(out=posf.ap, in_=pos_i.ap), [pos_i], [posf])
    P.op("dve", lambda: nc.vector.tensor_scalar(out=posf.ap, in0=posf.ap, scalar1=invf.ap[0:HP, 0:1], scalar2=None, op0=OP.mult), [posf, invf], [posf])
    P.op("dve", lambda: nc.vector.tensor_scalar(out=tmi.ap, in0=posf.ap, scalar1=1.0 / TWO_PI, scalar2=None, op0=OP.mult), [posf], [tmi])
    P.op("dve", lambda: nc.vector.tensor_copy(out=tmp.ap, in_=tmi.ap), [tmi], [tmp])
    P.op("dve", lambda: nc.vector.scalar_tensor_tensor(out=posf.ap, in0=tmp.ap, scalar=-C1, in1=posf.ap, op0=OP.mult, op1=OP.add), [tmp, posf], [posf])
    P.op("dve", lambda: nc.vector.scalar_tensor_tensor(out=posf.ap, in0=tmp.ap, scalar=-C2, in1=posf.ap, op0=OP.mult, op1=OP.add), [tmp, posf], [posf])
    P.op("dve", lambda: nc.vector.tensor_scalar(out=tmp.ap, in0=posf.ap, scalar1=math.pi, scalar2=-TWO_PI, op0=OP.is_gt, op1=OP.mult), [posf], [tmp])
    P.op("dve", lambda: nc.vector.tensor_tensor(out=posf.ap, in0=posf.ap, in1=tmp.ap, op=OP.add), [posf, tmp], [posf])
    P.op("dve", lambda: nc.vector.tensor_scalar(out=tmp.ap, in0=posf.ap, scalar1=-math.pi, scalar2=TWO_PI, op0=OP.is_lt, op1=OP.mult), [posf], [tmp])
    P.op("dve", lambda: nc.vector.tensor_tensor(out=posf.ap, in0=posf.ap, in1=tmp.ap, op=OP.add), [posf, tmp], [posf])
    P.op("dve", lambda: nc.vector.tensor_scalar(out=posf.ap, in0=posf.ap, scalar1=math.pi, scalar2=-math.pi, op0=OP.min, op1=OP.max), [posf], [posf])
    P.op("act", lambda: nc.scalar.activation(out=SN.ap, in_=posf.ap, func=AF.Sin), [posf], [SN])
    P.op("dve", lambda: nc.vector.tensor_scalar(out=SN.ap, in0=SN.ap, scalar1=sgn.ap[0:HP, 0:1], scalar2=None, op0=OP.mult), [SN, sgn], [SN])
    P.op("act", lambda: nc.scalar.activation(out=tmp.ap, in_=posf.ap, func=AF.Abs), [posf], [tmp])
    P.op("dve", lambda: nc.vector.tensor_scalar(out=tmp.ap, in0=tmp.ap, scalar1=-1.0, scalar2=math.pi / 2, op0=OP.mult, op1=OP.add), [tmp], [tmp])
    P.op("act", lambda: nc.scalar.activation(out=CS.ap, in_=tmp.ap, func=AF.Sin), [tmp], [CS])


def build_phase_b_mla(S):
    nc = bass.Bass("TRN2", target_bir_lowering=False)
    NG = S // 512
    NB = S // 128
    SCALE = (128 + 64) ** -0.5
    with ExitStack() as st:
        P = Prog(nc, st)
        cqT = P.dram("cqT", [512, S], F32, kind="ExternalInput")
        ckvT = P.dram("ckvT", [256, S], F32, kind="ExternalInput")
        krT = P.dram("krT", [64, S], F32, kind="ExternalInput")
        krTs = P.dram("krTs", [64, S], F32, kind="ExternalInput")
        pos = P.dram("pos", [1, S], I32, kind="ExternalInput")
        qn = P.dram("qn", [128, 4], F32, kind="ExternalInput")
        kvn = P.dram("kvn", [128, 2], F32, kind="ExternalInput")
        wuq_n = P.dram("wuq_n", [512, 128], F32, kind="ExternalInput")
        wuq_r = P.dram("wuq_r", [512, 64], F32, kind="ExternalInput")
        wuq_rs = P.dram("wuq_rs", [512, 64], F32, kind="ExternalInput")
        wukv_k = P.dram("wukv_k", [256, 128], F32, kind="ExternalInput")
        wukv_v = P.dram("wukv_v", [256, 128], F32, kind="ExternalInput")
        cst = P.dram("cst", [128, 2], F32, kind="ExternalInput")
        maskT_d = P.dram("maskT", [128, 128], F32, kind="ExternalInput")
        OT = P.dram("OT", [128, S], F32, kind="ExternalOutput")

        KTn = P.sb([128, S], BF16, "KTn")
        KTr = P.sb([64, S], BF16, "KTr")
        Vsb = P.sb([128, NB, 128], BF16, "Vsb")
        ones_f = P.sb([128, 128], F32, "ones_f")
        ones_b = P.sb([128, 128], BF16, "ones_b")
        maskT = P.sb([128, 128], BF16, "maskT_sb")
        cs = P.sb([128, 2], F32, "cst_sb")
        qn_sb = P.sb([128, 4], F32, "qn_sb")
        kvn_sb = P.sb([128, 2], F32, "kvn_sb")
        Wqn = P.sb([128, 4, 128], BF16, "Wqn")
        Wqr = P.sb([128, 4, 64], BF16, "Wqr")
        Wqrs = P.sb([128, 4, 64], BF16, "Wqrs")
        Wkk = P.sb([128, 2, 128], BF16, "Wkk")
        Wkv = P.sb([128, 2, 128], BF16, "Wkv")
        P.op("dve", lambda: nc.vector.memset(ones_f.ap, 1.0), [], [ones_f])
        P.op("dve", lambda: nc.vector.memset(ones_b.ap, 1.0), [], [ones_b])
        P.dma(maskT, maskT_d, q="pool")
        P.dma(cs, cst); P.dma(qn_sb, qn); P.dma(kvn_sb, kvn)
        P.dma(Wqn, View(wuq_n, wuq_n.ap.rearrange("(kc p) o -> p kc o", p=128)), q="pool")
        P.dma(Wqr, View(wuq_r, wuq_r.ap.rearrange("(kc p) o -> p kc o", p=128)), q="pool")
        P.dma(Wqrs, View(wuq_rs, wuq_rs.ap.rearrange("(kc p) o -> p kc o", p=128)), q="pool")
        P.dma(Wkk, View(wukv_k, wukv_k.ap.rearrange("(kc p) o -> p kc o", p=128)), q="pool")
        P.dma(Wkv, View(wukv_v, wukv_v.ap.rearrange("(kc p) o -> p kc o", p=128)), q="pool")
        invf = View(cs, cs.ap[:, 0:1]); sgn = View(cs, cs.ap[:, 1:2])

        cq_t = P.sb([128, 4, 512], F32, "cq_t")
        ckv_t = P.sb([128, 2, 512], F32, "ckv_t")
        sq = P.sb([128, 4, 512], F32, "sq")
        rs_q = P.sb([128, 512], F32, "rs_q")
        rs_k = P.sb([128, 512], F32, "rs_k")
        cqn = P.sb([128, 4, 512], BF16, "cqn")
        ckvn = P.sb([128, 2, 512], BF16, "ckvn")
        pos_i = P.sb([64, 512], I32, "pos_i")
        posf = P.sb([64, 512], F32, "posf")
        tmp = P.sb([64, 512], F32, "tmp")
        tmi = P.sb([64, 512], I32, "tmi")
        CS = P.sb([64, 512], F32, "CS")
        SN = P.sb([64, 512], F32, "SN")
        kr_t = P.sb([64, 512], F32, "kr_t")
        krs_t = P.sb([64, 512], F32, "krs_t")
        r1 = P.sb([64, 512], F32, "r1")
        r2 = P.sb([64, 512], F32, "r2")
        QTn = P.sb([128, 512], BF16, "QTn")
        QTr = P.sb([64, 512], BF16, "QTr")
        PT = [P.sb([128, 512], BF16, f"PT{i}") for i in range(2)]
        rec = P.sb([128, 512], F32, "rec")
        o_sb = P.sb([128, 512], F32, "o_sb")
        P1 = P.ps([128, 1024], F32, "P1")
        ST = [P.ps([128, 512], F32, f"ST{i}") for i in range(2)]
        OTp = P.ps([128, 512], F32, "OTp")
        DEN = P.ps([128, 512], F32, "DEN")

        cqT_v = cqT.ap.rearrange("(kc p) t -> p kc t", p=128)
        ckvT_v = ckvT.ap.rearrange("(kc p) t -> p kc t", p=128)
        it = 0
        for g in range(NG):
            gs = slice(g * 512, (g + 1) * 512)
            P.dma(cq_t, View(cqT, cqT_v[:, :, gs]), q="sp")
            P.dma(ckv_t, View(ckvT, ckvT_v[:, :, gs]), q="act")
            P.dma(kr_t, View(krT, krT.ap[:, gs]), q="sp")
            P.dma(krs_t, View(krTs, krTs.ap[:, gs]), q="act")
            P.dma(pos_i, View(pos, pos.ap[0:1, gs].to_broadcast([64, 512])), q="sp")
            rope_tables(P, nc, pos_i, posf, tmp, tmi, CS, SN, invf, sgn, 64)
            P.op("act", lambda: nc.scalar.activation(out=sq.ap, in_=cq_t.ap, func=AF.Square), [cq_t], [sq])
            for kc in range(4):
                P.op("pe", lambda: nc.tensor.matmul(P1.ap[:, 0:512], lhsT=ones_f.ap, rhs=sq.ap[:, kc, :], start=(kc == 0), stop=(kc == 3)), [ones_f, sq], [P1])
            P.op("dve", lambda: nc.vector.tensor_scalar(out=rs_q.ap, in0=P1.ap[:, 0:512], scalar1=1.0 / 512, scalar2=EPS, op0=OP.mult, op1=OP.add), [P1], [rs_q])
            P.op("act", lambda: nc.scalar.activation(out=rs_q.ap, in_=rs_q.ap, func=AF.Sqrt), [rs_q], [rs_q])
            P.op("dve", lambda: nc.vector.reciprocal(out=rs_q.ap, in_=rs_q.ap), [rs_q], [rs_q])
            for kc in range(4):
                P.op("dve", lambda: nc.vector.scalar_tensor_tensor(out=cqn.ap[:, kc, :], in0=cq_t.ap[:, kc, :], scalar=qn_sb.ap[:, kc:kc + 1], in1=rs_q.ap, op0=OP.mult, op1=OP.mult), [cq_t, qn_sb, rs_q], [cqn])
            P.op("act", lambda: nc.scalar.activation(out=sq.ap[:, 0:2, :], in_=ckv_t.ap, func=AF.Square), [ckv_t], [sq])
            for kc in range(2):
                P.op("pe", lambda: nc.tensor.matmul(P1.ap[:, 512:1024], lhsT=ones_f.ap, rhs=sq.ap[:, kc, :], start=(kc == 0), stop=(kc == 1)), [ones_f, sq], [P1])
            P.op("dve", lambda: nc.vector.tensor_scalar(out=rs_k.ap, in0=P1.ap[:, 512:1024], scalar1=1.0 / 256, scalar2=EPS, op0=OP.mult, op1=OP.add), [P1], [rs_k])
            P.op("act", lambda: nc.scalar.activation(out=rs_k.ap, in_=rs_k.ap, func=AF.Sqrt), [rs_k], [rs_k])
            P.op("dve", lambda: nc.vector.reciprocal(out=rs_k.ap, in_=rs_k.ap), [rs_k], [rs_k])
            for kc in range(2):
                P.op("dve", lambda: nc.vector.scalar_tensor_tensor(out=ckvn.ap[:, kc, :], in0=ckv_t.ap[:, kc, :], scalar=kvn_sb.ap[:, kc:kc + 1], in1=rs_k.ap, op0=OP.mult, op1=OP.mult), [ckv_t, kvn_sb, rs_k], [ckvn])
            for kc in range(4):
                P.op("pe", lambda: nc.tensor.matmul(P1.ap[:, 0:512], lhsT=Wqn.ap[:, kc, :], rhs=cqn.ap[:, kc, :], start=(kc == 0), stop=(kc == 3)), [Wqn, cqn], [P1])
            P.op("act", lambda: nc.scalar.activation(out=QTn.ap, in_=P1.ap[:, 0:512], func=AF.Copy, scale=SCALE), [P1], [QTn])
            for kc in range(4):
                P.op("pe", lambda: nc.tensor.matmul(P1.ap[0:64, 512:1024], lhsT=Wqr.ap[:, kc, :], rhs=cqn.ap[:, kc, :], start=(kc == 0), stop=(kc == 3)), [Wqr, cqn], [P1])
            P.op("dve", lambda: nc.vector.tensor_tensor(out=r1.ap, in0=P1.ap[0:64, 512:1024], in1=CS.ap, op=OP.mult), [P1, CS], [r1])
            for kc in range(4):
                P.op("pe", lambda: nc.tensor.matmul(P1.ap[0:64, 0:512], lhsT=Wqrs.ap[:, kc, :], rhs=cqn.ap[:, kc, :], start=(kc == 0), stop=(kc == 3)), [Wqrs, cqn], [P1])
            P.op("dve", lambda: nc.vector.tensor_tensor(out=r2.ap, in0=P1.ap[0:64, 0:512], in1=SN.ap, op=OP.mult), [P1, SN], [r2])
            P.op("dve", lambda: nc.vector.tensor_tensor(out=r1.ap, in0=r1.ap, in1=r2.ap, op=OP.add), [r1, r2], [r1])
            P.op("act", lambda: nc.scalar.activation(out=QTr.ap, in_=r1.ap, func=AF.Copy, scale=SCALE), [r1], [QTr])
            for kc in range(2):
                P.op("pe", lambda: nc.tensor.matmul(P1.ap[:, 512:1024], lhsT=Wkk.ap[:, kc, :], rhs=ckvn.ap[:, kc, :], start=(kc == 0), stop=(kc == 1)), [Wkk, ckvn], [P1])
            P.op("act", lambda: nc.scalar.copy(out=KTn.ap[:, gs], in_=P1.ap[:, 512:1024]), [P1], [KTn])
            P.op("dve", lambda: nc.vector.tensor_tensor(out=r1.ap, in0=kr_t.ap, in1=CS.ap, op=OP.mult), [kr_t, CS], [r1])
            P.op("dve", lambda: nc.vector.tensor_tensor(out=r2.ap, in0=krs_t.ap, in1=SN.ap, op=OP.mult), [krs_t, SN], [r2])
            P.op("dve", lambda: nc.vector.tensor_tensor(out=KTr.ap[:, gs], in0=r1.ap, in1=r2.ap, op=OP.add), [r1, r2], [KTr])
            for tt in range(4):
                for kc in range(2):
                    P.op("pe", lambda: nc.tensor.matmul(P1.ap[:, tt * 128:(tt + 1) * 128], lhsT=ckvn.ap[:, kc, tt * 128:(tt + 1) * 128], rhs=Wkv.ap[:, kc, :], start=(kc == 0), stop=(kc == 1)), [ckvn, Wkv], [P1])
            P.op("act", lambda: nc.scalar.copy(out=Vsb.ap[:, g * 4:(g + 1) * 4, :].rearrange("p a b -> p (a b)"), in_=P1.ap[:, 0:512]), [P1], [Vsb])
            nj = 4 * g + 4

            def S_(j):
                b = (it + j) % 2
                c0 = max(0, j - 4 * g) * 128
                ks = slice(j * 128, (j + 1) * 128)
                P.op("pe", lambda: nc.tensor.matmul(ST[b].ap[:, c0:512], lhsT=KTn.ap[:, ks], rhs=QTn.ap[:, c0:512], start=True, stop=False), [KTn, QTn], [ST[b]])
                P.op("pe", lambda: nc.tensor.matmul(ST[b].ap[:, c0:512], lhsT=KTr.ap[:, ks], rhs=QTr.ap[:, c0:512], start=False, stop=True), [KTr, QTr], [ST[b]])

            def EPV_(j):
                b = (it + j) % 2
                c0 = max(0, j - 4 * g) * 128
                P.op("act", lambda: nc.scalar.activation(out=PT[b].ap[:, c0:512], in_=ST[b].ap[:, c0:512], func=AF.Exp), [ST[b]], [PT[b]])
                if j >= 4 * g:
                    P.op("pool", lambda: nc.gpsimd.tensor_tensor(out=PT[b].ap[:, c0:c0 + 128], in0=PT[b].ap[:, c0:c0 + 128], in1=maskT.ap, op=OP.mult), [PT[b], maskT], [PT[b]])
                P.op("pe", lambda: nc.tensor.matmul(OTp.ap[:, c0:512], lhsT=Vsb.ap[:, j, :], rhs=PT[b].ap[:, c0:512], start=(j == 0), stop=(j == nj - 1)), [Vsb, PT[b]], [OTp])
                P.op("pe", lambda: nc.tensor.matmul(DEN.ap[:, c0:512], lhsT=ones_b.ap, rhs=PT[b].ap[:, c0:512], start=(j == 0), stop=(j == nj - 1)), [ones_b, PT[b]], [DEN])

            S_(0)
            for j in range(nj):
                if j + 1 < nj:
                    S_(j + 1)
                EPV_(j)
            it += nj
            P.op("dve", lambda: nc.vector.reciprocal(out=rec.ap, in_=DEN.ap), [DEN], [rec])
            P.op("dve", lambda: nc.vector.tensor_tensor(out=o_sb.ap, in0=OTp.ap, in1=rec.ap, op=OP.mult), [OTp, rec], [o_sb])
            P.dma(View(OT, OT.ap[:, gs]), o_sb, q="sp")
        P.barrier()
        P.finish([OT])
        print("phase B mla: ninst", P.ninst, "nwait", P.nwait)
    return nc


BIG = 30000.0
NEG = -3.0e38


def moba_body(P, nc, S, D):
    NG = S // 512
    NB = S // 128
    NBLK = S // 256
    SCALE = 128 ** -0.5
    qT, qTs, kT, kTs, v, pos = D["qT"], D["qTs"], D["kT"], D["kTs"], D["v"], D["pos"]
    OT = D["OT"]
    KT = P.sb([128, S], BF16, "m_KT")
    Vsb = P.sb([128, NB, 128], BF16, "m_Vsb")
    KMT = P.sb([128, NBLK], F32, "m_KMT")
    SelT = P.sb([NBLK, NBLK, 128], BF16, "m_SelT")
    ones_b = P.sb([128, 128], BF16, "m_ones_b")
    maskT = P.sb([128, 128], BF16, "m_maskT")
    ident = P.sb([128, 128], F32, "m_ident")
    cs = P.sb([128, 2], F32, "m_cst")
    P.op("dve", lambda: nc.vector.memset(ones_b.ap, 1.0), [], [ones_b])
    P.dma(maskT, D["maskT"], q="pool")
    P.dma(SelT, View(D["selT"], D["selT"].ap.rearrange("n (m k) -> n m k", k=128)), q="pool")
    P.dma(ident, D["ident"])
    P.dma(cs, D["cst"])
    v_v = v.ap.rearrange("(b p) d -> p b d", p=128)
    for b0 in range(0, NB, 16):
        b1 = min(NB, b0 + 16)
        P.dma(View(Vsb, Vsb.ap[:, b0:b1, :]), View(v, v_v[:, b0:b1, :]), q="pool")
    invf = View(cs, cs.ap[:, 0:1]); sgn = View(cs, cs.ap[:, 1:2])
    q_t = P.sb([128, 512], F32, "m_q_t"); qs_t = P.sb([128, 512], F32, "m_qs_t")
    k_t = P.sb([128, 512], F32, "m_k_t"); ks_t = P.sb([128, 512], F32, "m_ks_t")
    pos_i = P.sb([128, 512], I32, "m_pos_i"); posf = P.sb([128, 512], F32, "m_posf")
    tmp = P.sb([128, 512], F32, "m_tmp"); tmi = P.sb([128, 512], I32, "m_tmi")
    CS = P.sb([128, 512], F32, "m_CS"); SN = P.sb([128, 512], F32, "m_SN")
    r1 = P.sb([128, 512], F32, "m_r1"); r2 = P.sb([128, 512], F32, "m_r2")
    QTf = P.sb([128, 512], F32, "m_QTf")
    QT = P.sb([128, 512], BF16, "m_QT")
    gate = P.sb([128, NBLK], F32, "m_gate")
    m8 = P.sb([128, 8], F32, "m_m8")
    pen = P.sb([128, NBLK], F32, "m_pen")
    PenT = P.sb([NBLK, 512], BF16, "m_PenT")
    PT = [P.sb([128, 512], BF16, f"m_PT{i}") for i in range(2)]
    rec = P.sb([128, 512], F32, "m_rec")
    o_sb = P.sb([128, 512], F32, "m_o_sb")
    P1 = P.ps([128, 512], F32, "m_P1")
    ST = [P.ps([128, 512], F32, f"m_ST{i}") for i in range(2)]
    OTp = P.ps([128, 512], F32, "m_OTp")
    DEN = P.ps([128, 512], F32, "m_DEN")
    it = 0
    for g in range(NG):
        gs = slice(g * 512, (g + 1) * 512)
        P.dma(q_t, View(qT, qT.ap[:, gs]), q="sp")
        P.dma(qs_t, View(qTs, qTs.ap[:, gs]), q="act")
        P.dma(k_t, View(kT, kT.ap[:, gs]), q="sp")
        P.dma(ks_t, View(kTs, kTs.ap[:, gs]), q="act")
        P.dma(pos_i, View(pos, pos.ap[0:1, gs].to_broadcast([128, 512])), q="sp")
        rope_tables(P, nc, pos_i, posf, tmp, tmi, CS, SN, invf, sgn, 128)
        P.op("dve", lambda: nc.vector.tensor_tensor(out=r1.ap, in0=q_t.ap, in1=CS.ap, op=OP.mult), [q_t, CS], [r1])
        P.op("pool", lambda: nc.gpsimd.tensor_tensor(out=r2.ap, in0=qs_t.ap, in1=SN.ap, op=OP.mult), [qs_t, SN], [r2])
        P.op("dve", lambda: nc.vector.tensor_tensor(out=QTf.ap, in0=r1.ap, in1=r2.ap, op=OP.add), [r1, r2], [QTf])
        P.op("act", lambda: nc.scalar.activation(out=QT.ap, in_=QTf.ap, func=AF.Copy, scale=SCALE), [QTf], [QT])
        P.op("dve", lambda: nc.vector.tensor_tensor(out=r1.ap, in0=k_t.ap, in1=CS.ap, op=OP.mult), [k_t, CS], [r1])
        P.op("pool", lambda: nc.gpsimd.tensor_tensor(out=r2.ap, in0=ks_t.ap, in1=SN.ap, op=OP.mult), [ks_t, SN], [r2])
        P.op("dve", lambda: nc.vector.tensor_tensor(out=r1.ap, in0=r1.ap, in1=r2.ap, op=OP.add), [r1, r2], [r1])
        P.op("act", lambda: nc.scalar.copy(out=KT.ap[:, gs], in_=r1.ap), [r1], [KT])
        P.op("dve", lambda: nc.vector.tensor_reduce(out=KMT.ap[:, 2 * g:2 * g + 2], in_=r1.ap.rearrange("p (n k) -> p n k", k=256), axis=AX.X, op=OP.add), [r1], [KMT])
        P.op("dve", lambda: nc.vector.tensor_scalar(out=KMT.ap[:, 2 * g:2 * g + 2], in0=KMT.ap[:, 2 * g:2 * g + 2], scalar1=1.0 / 256, scalar2=None, op0=OP.mult), [KMT], [KMT])
        for qb in range(4):
            own = 2 * g + qb // 2
            P.op("dve", lambda: nc.vector.memset(gate.ap, NEG), [], [gate])
            if own > 0:
                P.op("pe", lambda: nc.tensor.matmul(P1.ap[:, 0:own], lhsT=QTf.ap[:, qb * 128:(qb + 1) * 128], rhs=KMT.ap[:, 0:own], start=True, stop=True), [QTf, KMT], [P1])
                P.op("dve", lambda: nc.vector.tensor_copy(out=gate.ap[:, 0:own], in_=P1.ap[:, 0:own]), [P1], [gate])
            P.op("dve", lambda: nc.vector.max(out=m8.ap, in_=gate.ap), [gate], [m8])
            P.op("dve", lambda: nc.vector.tensor_scalar(out=m8.ap[:, 2:3], in0=m8.ap[:, 2:3], scalar1=-1.0e30, scalar2=None, op0=OP.max), [m8], [m8])
            P.op("dve", lambda: nc.vector.tensor_scalar(out=pen.ap, in0=gate.ap, scalar1=m8.ap[:, 2:3], scalar2=BIG, op0=OP.is_ge, op1=OP.mult), [gate, m8], [pen])
            P.op("dve", lambda: nc.vector.tensor_scalar(out=pen.ap, in0=pen.ap, scalar1=-BIG, scalar2=None, op0=OP.add), [pen], [pen])
            P.op("dve", lambda: nc.vector.memset(pen.ap[:, own:own + 1], 0.0), [], [pen])
            P.op("pe", lambda: nc.tensor.transpose(P1.ap[0:NBLK, 128:256], pen.ap, ident.ap), [pen, ident], [P1])
            P.op("act", lambda: nc.scalar.copy(out=PenT.ap[:, qb * 128:(qb + 1) * 128], in_=P1.ap[0:NBLK, 128:256]), [P1], [PenT])
        nj = 4 * g + 4

        def S_(j):
            b = (it + j) % 2
            c0 = max(0, j - 4 * g) * 128
            ks = slice(j * 128, (j + 1) * 128)
            n = j // 2
            P.op("pe", lambda: nc.tensor.matmul(ST[b].ap[:, c0:512], lhsT=KT.ap[:, ks], rhs=QT.ap[:, c0:512], start=True, stop=False), [KT, QT], [ST[b]])
            P.op("pe", lambda: nc.tensor.matmul(ST[b].ap[:, c0:512], lhsT=SelT.ap[:, n, :], rhs=PenT.ap[:, c0:512], start=False, stop=True), [SelT, PenT], [ST[b]])

        def EPV_(j):
            b = (it + j) % 2
            c0 = max(0, j - 4 * g) * 128
            P.op("act", lambda: nc.scalar.activation(out=PT[b].ap[:, c0:512], in_=ST[b].ap[:, c0:512], func=AF.Exp), [ST[b]], [PT[b]])
            if j >= 4 * g:
                P.op("pool", lambda: nc.gpsimd.tensor_tensor(out=PT[b].ap[:, c0:c0 + 128], in0=PT[b].ap[:, c0:c0 + 128], in1=maskT.ap, op=OP.mult), [PT[b], maskT], [PT[b]])
            P.op("pe", lambda: nc.tensor.matmul(OTp.ap[:, c0:512], lhsT=Vsb.ap[:, j, :], rhs=PT[b].ap[:, c0:512], start=(j == 0), stop=(j == nj - 1)), [Vsb, PT[b]], [OTp])
            P.op("pe", lambda: nc.tensor.matmul(DEN.ap[:, c0:512], lhsT=ones_b.ap, rhs=PT[b].ap[:, c0:512], start=(j == 0), stop=(j == nj - 1)), [ones_b, PT[b]], [DEN])

        S_(0)
        for j in range(nj):
            if j + 1 < nj:
                S_(j + 1)
            EPV_(j)
        it += nj
        P.op("dve", lambda: nc.vector.reciprocal(out=rec.ap, in_=DEN.ap), [DEN], [rec])
        P.op("dve", lambda: nc.vector.tensor_tensor(out=o_sb.ap, in0=OTp.ap, in1=rec.ap, op=OP.mult), [OTp, rec], [o_sb])
        P.dma(View(OT, OT.ap[:, gs]), o_sb, q="sp")


def build_phase_b_moba(S):
    nc = bass.Bass("TRN2", target_bir_lowering=False)
    NBLK = S // 256
    with ExitStack() as st:
        P = Prog(nc, st)
        D = {}
        for nm in ("qT", "qTs", "kT", "kTs"):
            D[nm] = P.dram(nm, [128, S], F32, kind="ExternalInput")
        D["v"] = P.dram("v", [S, 128], F32, kind="ExternalInput")
        D["pos"] = P.dram("pos", [1, S], I32, kind="ExternalInput")
        D["cst"] = P.dram("cst", [128, 2], F32, kind="ExternalInput")
        D["maskT"] = P.dram("maskT", [128, 128], F32, kind="ExternalInput")
        D["selT"] = P.dram("selT", [NBLK, NBLK * 128], F32, kind="ExternalInput")
        D["ident"] = P.dram("ident", [128, 128], F32, kind="ExternalInput")
        D["OT"] = P.dram("OT", [128, S], F32, kind="ExternalOutput")
        with P.scope():
            moba_body(P, nc, S, D)
        P.barrier()
        P.finish([D["OT"]])
        print("phase B moba: ninst", P.ninst, "nwait", P.nwait)
    return nc


EPS = 1e-6


def gdn_body(P, nc, S, D):
    NG = S // 512
    NCH = S // 128
    op = P.op
    V = nc.vector
    A = nc.scalar
    PE = nc.tensor
    ident = P.sb([128, 128], F32, "g_ident"); tri = P.sb([128, 128], F32, "g_tri")
    maskS = P.sb([128, 128], F32, "g_maskS"); maskI = P.sb([128, 128], F32, "g_maskI")
    ones_f = P.sb([128, 128], F32, "g_ones")
    cw = P.sb([128, 12], F32, "g_cw"); hc = P.sb([128, 2], F32, "g_hc"); onb = P.sb([128, 128], F32, "g_onb")
    ba = P.sb([128, NCH, 2], F32, "g_ba")
    beta = P.sb([128, NCH], F32, "g_beta"); nbeta = P.sb([128, NCH], F32, "g_nbeta"); graw = P.sb([128, NCH], F32, "g_graw")
    tmpc = P.sb([128, NCH], F32, "g_tmpc")
    for b_, d_ in ((ident, "ident"), (tri, "tri"), (maskS, "maskS"), (maskI, "maskI"), (cw, "cw"), (hc, "hc"), (onb, "onorm")):
        P.dma(b_, D[d_], q="sp")
    P.dma(ba, D["ba"], q="act")
    op("dve", lambda: V.memset(ones_f.ap, 1.0), [], [ones_f])
    op("act", lambda: A.activation(out=beta.ap, in_=ba.ap[:, :, 0], func=AF.Sigmoid), [ba], [beta])
    op("dve", lambda: V.tensor_scalar(out=nbeta.ap, in0=beta.ap, scalar1=-1.0, scalar2=None, op0=OP.mult), [beta], [nbeta])
    op("act", lambda: A.activation(out=tmpc.ap, in_=ba.ap[:, :, 1], func=AF.Exp, bias=hc.ap[:, 1:2]), [ba, hc], [tmpc])
    op("act", lambda: A.activation(out=tmpc.ap, in_=tmpc.ap, func=AF.Ln, bias=ones_f.ap[:, 0:1]), [tmpc, ones_f], [tmpc])
    op("act", lambda: A.activation(out=hc.ap[:, 0:1], in_=hc.ap[:, 0:1], func=AF.Exp), [hc], [hc])
    op("dve", lambda: V.tensor_scalar(out=graw.ap, in0=tmpc.ap, scalar1=hc.ap[:, 0:1], scalar2=-1.0, op0=OP.mult, op1=OP.mult), [tmpc, hc], [graw])

    St = P.sb([128, 128], F32, "g_S")
    op("dve", lambda: V.memset(St.ap, 0.0), [], [St])
    xin = [P.sb([128, 515], F32, f"g_xin{i}") for i in range(3)]
    yc = [P.sb([128, 512], F32, f"g_yc{i}") for i in range(3)]
    sq = P.sb([128, 512], F32, "g_sq")
    rs = P.sb([128, 512], F32, "g_rs")
    names = ["GrB", "Gm_sb", "t1", "Dm", "EG", "Am", "Bm", "A2", "B2", "Ym", "qk", "qkT", "kbg", "kd", "vb", "wT", "u", "qdT", "vnew", "zt", "ot", "o2"]
    W = {n: P.sb([128, 128], F32, "g_" + n) for n in names}
    cols = P.sb([128, 8], F32, "g_cols")
    PG = P.ps([128, 512], F32, "g_PG"); PTr = P.ps([128, 512], F32, "g_PTr"); PD = P.ps([128, 512], F32, "g_PD")
    PW = P.ps([128, 512], F32, "g_PW"); PS = P.ps([128, 512], F32, "g_PS"); PC = P.ps([128, 512], F32, "g_PC")
    c_ = lambda i: slice(i * 128, (i + 1) * 128)
    xs = (D["xq"], D["xk"], D["xv"])
    z_v = D["z"].ap.rearrange("(n p) d -> n p d", p=128)
    oa_v = D["OA"].ap.rearrange("(n p) d -> n p d", p=128)
    for g in range(NG):
        for i in range(3):
            P.dma(xin[i], View(xs[i], xs[i].ap[:, g * 512:g * 512 + 515]), q="sp" if i != 1 else "act")
            op("dve", lambda: V.tensor_scalar(out=yc[i].ap, in0=xin[i].ap[:, 0:512], scalar1=cw.ap[:, 4 * i:4 * i + 1], scalar2=None, op0=OP.mult), [xin[i], cw], [yc[i]])
            for j in range(1, 4):
                op("dve", lambda: V.scalar_tensor_tensor(out=yc[i].ap, in0=xin[i].ap[:, j:j + 512], scalar=cw.ap[:, 4 * i + j:4 * i + j + 1], in1=yc[i].ap, op0=OP.mult, op1=OP.add), [xin[i], cw, yc[i]], [yc[i]])
            op("act", lambda: A.activation(out=yc[i].ap, in_=yc[i].ap, func=AF.Silu), [yc[i]], [yc[i]])
            if i < 2:
                op("act", lambda: A.activation(out=sq.ap, in_=yc[i].ap, func=AF.Square), [yc[i]], [sq])
                op("pe", lambda: PE.matmul(PC.ap, lhsT=ones_f.ap, rhs=sq.ap, start=True, stop=True), [ones_f, sq], [PC])
                op("dve", lambda: V.tensor_scalar(out=rs.ap, in0=PC.ap, scalar1=EPS, scalar2=None, op0=OP.add), [PC], [rs])
                op("act", lambda: A.activation(out=rs.ap, in_=rs.ap, func=AF.Sqrt), [rs], [rs])
                op("dve", lambda: V.reciprocal(out=rs.ap, in_=rs.ap), [rs], [rs])
                sc_ = (128 ** -0.5) if i == 0 else 1.0
                op("dve", lambda: V.scalar_tensor_tensor(out=yc[i].ap, in0=yc[i].ap, scalar=sc_, in1=rs.ap, op0=OP.mult, op1=OP.mult), [yc[i], rs], [yc[i]])
        for cc in range(4):
            n = g * 4 + cc
            qTn = View(yc[0], yc[0].ap[:, c_(cc)]); kTn = View(yc[1], yc[1].ap[:, c_(cc)]); vTn = View(yc[2], yc[2].ap[:, c_(cc)])
            gr = graw.ap[:, n:n + 1]; be = beta.ap[:, n:n + 1]; nbe = nbeta.ap[:, n:n + 1]
            op("dve", lambda: V.tensor_scalar(out=W["GrB"].ap, in0=ones_f.ap, scalar1=gr, scalar2=None, op0=OP.mult), [ones_f, graw], [W["GrB"]])
            op("pe", lambda: PE.matmul(PG.ap[:, c_(0)], lhsT=W["GrB"].ap, rhs=tri.ap, start=True, stop=True), [W["GrB"], tri], [PG])
            op("pe", lambda: PE.matmul(PG.ap[:, 128:129], lhsT=tri.ap, rhs=gr, start=True, stop=True), [tri, graw], [PG])
            op("pe", lambda: PE.matmul(PG.ap[:, c_(2)], lhsT=kTn.ap, rhs=kTn.ap, start=True, stop=True), [kTn], [PG])
            op("pe", lambda: PE.matmul(PG.ap[:, c_(3)], lhsT=qTn.ap, rhs=kTn.ap, start=True, stop=True), [qTn, kTn], [PG])
            op("act", lambda: A.copy(out=W["Gm_sb"].ap, in_=PG.ap[:, c_(0)]), [PG], [W["Gm_sb"]])
            op("act", lambda: A.copy(out=cols.ap[:, 0:1], in_=PG.ap[:, 128:129]), [PG], [cols])
            op("dve", lambda: V.tensor_scalar(out=W["t1"].ap, in0=W["Gm_sb"].ap, scalar1=cols.ap[:, 0:1], scalar2=0.0, op0=OP.subtract, op1=OP.max), [W["Gm_sb"], cols], [W["t1"]])
            op("act", lambda: A.activation(out=W["Dm"].ap, in_=W["t1"].ap, func=AF.Exp, scale=-1.0), [W["t1"]], [W["Dm"]])
            op("act", lambda: A.activation(out=W["EG"].ap, in_=W["Gm_sb"].ap, func=AF.Exp), [W["Gm_sb"]], [W["EG"]])
            op("act", lambda: A.activation(out=cols.ap[:, 1:2], in_=cols.ap[:, 0:1], func=AF.Exp), [cols], [cols])
            op("act", lambda: A.activation(out=cols.ap[:, 2:3], in_=cols.ap[:, 0:1], func=AF.Exp, scale=-1.0, bias=W["Gm_sb"].ap[:, 127:128]), [cols, W["Gm_sb"]], [cols])
            op("dve", lambda: V.tensor_tensor(out=cols.ap[:, 3:4], in0=cols.ap[:, 1:2], in1=be, op=OP.mult), [cols, beta], [cols])
            op("dve", lambda: V.tensor_tensor(out=W["t1"].ap, in0=PG.ap[:, c_(2)], in1=W["Dm"].ap, op=OP.mult), [PG, W["Dm"]], [W["t1"]])
            op("dve", lambda: V.scalar_tensor_tensor(out=W["Am"].ap, in0=W["t1"].ap, scalar=nbe, in1=maskS.ap, op0=OP.mult, op1=OP.mult), [W["t1"], nbeta, maskS], [W["Am"]])
            op("dve", lambda: V.tensor_tensor(out=W["t1"].ap, in0=PG.ap[:, c_(3)], in1=W["Dm"].ap, op=OP.mult), [PG, W["Dm"]], [W["t1"]])
            op("dve", lambda: V.tensor_tensor(out=W["qk"].ap, in0=W["t1"].ap, in1=maskI.ap, op=OP.mult), [W["t1"], maskI], [W["qk"]])
            op("pe", lambda: PE.transpose(PTr.ap[:, c_(0)], W["Am"].ap, ident.ap), [W["Am"], ident], [PTr])
            op("pe", lambda: PE.transpose(PTr.ap[:, c_(1)], W["qk"].ap, ident.ap), [W["qk"], ident], [PTr])
            op("pe", lambda: PE.transpose(PTr.ap[:, c_(2)], kTn.ap, ident.ap), [kTn, ident], [PTr])
            op("pe", lambda: PE.transpose(PTr.ap[:, c_(3)], vTn.ap, ident.ap), [vTn, ident], [PTr])
            op("act", lambda: A.copy(out=W["Bm"].ap, in_=PTr.ap[:, c_(0)]), [PTr], [W["Bm"]])
            op("act", lambda: A.copy(out=W["qkT"].ap, in_=PTr.ap[:, c_(1)]), [PTr], [W["qkT"]])
            op("act", lambda: A.activation(out=W["kbg"].ap, in_=PTr.ap[:, c_(2)], func=AF.Copy, scale=cols.ap[:, 3:4]), [PTr, cols], [W["kbg"]])
            op("act", lambda: A.activation(out=W["kd"].ap, in_=PTr.ap[:, c_(2)], func=AF.Copy, scale=cols.ap[:, 2:3]), [PTr, cols], [W["kd"]])
            op("act", lambda: A.activation(out=W["vb"].ap, in_=PTr.ap[:, c_(3)], func=AF.Copy, scale=be), [PTr, beta], [W["vb"]])
            op("dve", lambda: V.tensor_tensor(out=W["Ym"].ap, in0=W["Bm"].ap, in1=ident.ap, op=OP.add), [W["Bm"], ident], [W["Ym"]])
            Ac, Bc, An, Bn = W["Am"], W["Bm"], W["A2"], W["B2"]
            for stp in range(6):
                op("pe", lambda: PE.matmul(PD.ap[:, c_(0)], lhsT=Bc.ap, rhs=Ac.ap, start=True, stop=True), [Bc, Ac], [PD])
                if stp < 5:
                    op("pe", lambda: PE.matmul(PD.ap[:, c_(1)], lhsT=Ac.ap, rhs=Bc.ap, start=True, stop=True), [Ac, Bc], [PD])
                op("act", lambda: A.copy(out=An.ap, in_=PD.ap[:, c_(0)]), [PD], [An])
                if stp < 5:
                    op("act", lambda: A.copy(out=Bn.ap, in_=PD.ap[:, c_(1)]), [PD], [Bn])
                op("pe", lambda: PE.matmul(PD.ap[:, c_(2)], lhsT=An.ap, rhs=W["Ym"].ap, start=True, stop=True), [An, W["Ym"]], [PD])
                op("dve", lambda: V.tensor_tensor(out=W["Ym"].ap, in0=W["Ym"].ap, in1=PD.ap[:, c_(2)], op=OP.add), [W["Ym"], PD], [W["Ym"]])
                Ac, Bc, An, Bn = An, Bn, Ac, Bc
            op("pe", lambda: PE.matmul(PW.ap[:, c_(0)], lhsT=W["kbg"].ap, rhs=W["Ym"].ap, start=True, stop=True), [W["kbg"], W["Ym"]], [PW])
            op("pe", lambda: PE.matmul(PW.ap[:, c_(1)], lhsT=W["Ym"].ap, rhs=W["vb"].ap, start=True, stop=True), [W["Ym"], W["vb"]], [PW])
            op("act", lambda: A.copy(out=W["wT"].ap, in_=PW.ap[:, c_(0)]), [PW], [W["wT"]])
            op("act", lambda: A.copy(out=W["u"].ap, in_=PW.ap[:, c_(1)]), [PW], [W["u"]])
            op("dve", lambda: V.tensor_tensor(out=W["qdT"].ap, in0=qTn.ap, in1=W["EG"].ap, op=OP.mult), [qTn, W["EG"]], [W["qdT"]])
            op("pe", lambda: PE.matmul(PS.ap[:, c_(0)], lhsT=W["wT"].ap, rhs=St.ap, start=True, stop=True), [W["wT"], St], [PS])
            op("dve", lambda: V.tensor_tensor(out=W["vnew"].ap, in0=W["u"].ap, in1=PS.ap[:, c_(0)], op=OP.subtract), [W["u"], PS], [W["vnew"]])
            op("pe", lambda: PE.matmul(PS.ap[:, c_(1)], lhsT=W["qdT"].ap, rhs=St.ap, start=True, stop=False), [W["qdT"], St], [PS])
            op("pe", lambda: PE.matmul(PS.ap[:, c_(1)], lhsT=W["qkT"].ap, rhs=W["vnew"].ap, start=False, stop=True), [W["qkT"], W["vnew"]], [PS])
            op("pe", lambda: PE.matmul(PS.ap[:, c_(2)], lhsT=W["kd"].ap, rhs=W["vnew"].ap, start=True, stop=True), [W["kd"], W["vnew"]], [PS])
            op("act", lambda: A.copy(out=W["ot"].ap, in_=PS.ap[:, c_(1)]), [PS], [W["ot"]])
            op("dve", lambda: V.scalar_tensor_tensor(out=St.ap, in0=St.ap, scalar=W["EG"].ap[:, 127:128], in1=PS.ap[:, c_(2)], op0=OP.mult, op1=OP.add), [St, W["EG"], PS], [St])
            P.dma(W["zt"], View(D["z"], z_v[n]), q="act")
            op("act", lambda: A.activation(out=W["o2"].ap, in_=W["ot"].ap, func=AF.Square, accum_out=cols.ap[:, 4:5]), [W["ot"]], [W["o2"], cols])
            op("dve", lambda: V.tensor_scalar(out=cols.ap[:, 5:6], in0=cols.ap[:, 4:5], scalar1=1.0 / 128, scalar2=EPS, op0=OP.mult, op1=OP.add), [cols], [cols])
            op("act", lambda: A.activation(out=cols.ap[:, 5:6], in_=cols.ap[:, 5:6], func=AF.Sqrt), [cols], [cols])
            op("dve", lambda: V.reciprocal(out=cols.ap[:, 5:6], in_=cols.ap[:, 5:6]), [cols], [cols])
            op("act", lambda: A.activation(out=W["zt"].ap, in_=W["zt"].ap, func=AF.Silu), [W["zt"]], [W["zt"]])
            op("dve", lambda: V.scalar_tensor_tensor(out=W["o2"].ap, in0=W["ot"].ap, scalar=cols.ap[:, 5:6], in1=onb.ap, op0=OP.mult, op1=OP.mult), [W["ot"], cols, onb], [W["o2"]])
            op("dve", lambda: V.tensor_tensor(out=W["o2"].ap, in0=W["o2"].ap, in1=W["zt"].ap, op=OP.mult), [W["o2"], W["zt"]], [W["o2"]])
            P.dma(View(D["OA"], oa_v[n]), W["o2"], q="sp")


def build_phase_b_gdn(S):
    nc = bass.Bass("TRN2", target_bir_lowering=False)
    with ExitStack() as st:
        P = Prog(nc, st)
        D = {}
        for nm in ("xq", "xk", "xv"):
            D[nm] = P.dram(nm, [128, S + 3], F32, kind="ExternalInput")
        D["cw"] = P.dram("cw", [128, 12], F32, kind="ExternalInput")
        D["z"] = P.dram("z", [S, 128], F32, kind="ExternalInput")
        D["ba"] = P.dram("ba", [128, S // 128, 2], F32, kind="ExternalInput")
        D["hc"] = P.dram("hc", [128, 2], F32, kind="ExternalInput")
        D["onorm"] = P.dram("onorm", [128, 128], F32, kind="ExternalInput")
        for nm in ("ident", "tri", "maskS", "maskI"):
            D[nm] = P.dram(nm, [128, 128], F32, kind="ExternalInput")
        D["OA"] = P.dram("OA", [S, 128], F32, kind="ExternalOutput")
        with P.scope():
            gdn_body(P, nc, S, D)
        P.barrier()
        P.finish([D["OA"]])
        print("phase B gdn: ninst", P.ninst, "nwait", P.nwait)
    return nc


EPS = 1e-6
NEG = -3.0e38


def bcast_rows(P, nc, ones_row, row, dst, ps, ncols):
    for c0 in range(0, ncols, 512):
        P.op("pe", lambda: nc.tensor.matmul(ps.ap[:, 0:512], lhsT=ones_row.ap[0:1, 0:128], rhs=row.ap[0:1, c0:c0 + 512], start=True, stop=True), [ones_row, row], [ps])
        P.op("act", lambda: nc.scalar.copy(out=dst.ap[:, c0:c0 + 512], in_=ps.ap[:, 0:512]), [ps], [dst])


def build_phase_c(T, final=False, NCH=32):
    nc = bass.Bass("TRN2", target_bir_lowering=False)
    NT = T // 128
    with ExitStack() as st:
        P = Prog(nc, st)
        x = P.dram("x", [T, 1024], F32, kind="ExternalInput")
        oT = P.dram("oT", [1024, T], F32, kind="ExternalInput")
        w_out = P.dram("w_out", [1024, 1024], F32, kind="ExternalInput")
        cT = P.dram("cT", [128, 8], F32, kind="ExternalInput")
        modw = P.dram("modw", [1024, 4096], F32, kind="ExternalInput")
        modb = P.dram("modb", [1, 4096], F32, kind="ExternalInput")
        nrm = P.dram("nrm", [1, 1024], F32, kind="ExternalInput")
        fnrm = P.dram("fnrm", [1, 1024], F32, kind="ExternalInput")
        w_q = P.dram("w_q", [1024, 2048], F32, kind="ExternalInput")
        skT = P.dram("skT", [128, 16, 128], F32, kind="ExternalInput")
        uT = P.dram("uT", [1024, NCH * 512], F32, kind="ExternalInput")
        vv = P.dram("vv", [NCH * 512, 1024], F32, kind="ExternalInput")
        ident_d = P.dram("ident", [128, 128], F32, kind="ExternalInput")
        xo = P.dram("xo", [T, 1024], F32, kind="ExternalOutput")
        sc_s = P.dram("sc_s", [NT, 128, 3, 8, 128], F32)
        sc_x1 = P.dram("sc_x1", [T, 1024], F32)
        sc_h2T = P.dram("sc_h2T", [NT, 128, 8, 128], BF16)
        uT_bf = P.dram("uT_bf", [1024, NCH * 512], BF16)
        vv_bf = P.dram("vv_bf", [NCH * 512, 1024], BF16)

        ident = P.sb([128, 128], F32, "ident_sb")
        identb = P.sb([128, 128], BF16, "identb")
        ones_row = P.sb([1, 128], F32, "ones_row")
        G1b = P.sb([128, 1024], F32, "G1b")
        A2b = P.sb([128, 1024], F32, "A2b")
        B2b = P.sb([128, 1024], F32, "B2b")
        G2b = P.sb([128, 1024], F32, "G2b")
        FNb = P.sb([128, 1024], F32, "FNb")
        P.dma(ident, ident_d)
        P.op("dve", lambda: nc.vector.tensor_copy(out=identb.ap, in_=ident.ap), [ident], [identb])
        P.op("dve", lambda: nc.vector.memset(ones_row.ap, 1.0), [], [ones_row])

        for i in range(8):
            P.dma(View(uT_bf, uT_bf.ap[i * 128:(i + 1) * 128, :]), View(uT, uT.ap[i * 128:(i + 1) * 128, :]), q="pool")
        NV = NCH * 512 // 8
        for i in range(8):
            P.dma(View(vv_bf, vv_bf.ap[i * NV:(i + 1) * NV, :]), View(vv, vv.ap[i * NV:(i + 1) * NV, :]), q="pool")
        with P.scope():
            P1 = P.ps([128, 1024], F32, "P1a")
            cond = P.sb([128, 8], F32, "cond")
            modrow = P.sb([1, 4096], F32, "modrow")
            mb = P.sb([1, 4096], F32, "mb")
            nr = P.sb([1, 1024], F32, "nr")
            fn = P.sb([1, 1024], F32, "fn")
            a2row = P.sb([1, 1024], F32, "a2row")
            mwc = [P.sb([128, 8, 512], F32, f"mwc{i}") for i in range(2)]
            P.dma(cond, cT)
            P.dma(mb, modb, q="act")
            P.dma(nr, nrm, q="act")
            P.dma(fn, fnrm, q="act")
            P.op("act", lambda: nc.scalar.activation(out=cond.ap, in_=cond.ap, func=AF.Silu), [cond], [cond])
            mw_v = modw.ap.rearrange("(kc p) o -> p kc o", p=128)
            for ch in range(8):
                buf = mwc[ch % 2]
                P.dma(buf, View(modw, mw_v[:, :, ch * 512:(ch + 1) * 512]), q="sp" if ch % 2 == 0 else "act")
                for kc in range(8):
                    P.op("pe", lambda: nc.tensor.matmul(P1.ap[0:1, 0:512], lhsT=cond.ap[:, kc:kc + 1], rhs=buf.ap[:, kc, :], start=(kc == 0), stop=(kc == 7)), [cond, buf], [P1])
                P.op("dve", lambda: nc.vector.tensor_tensor(out=modrow.ap[0:1, ch * 512:(ch + 1) * 512], in0=P1.ap[0:1, 0:512], in1=mb.ap[0:1, ch * 512:(ch + 1) * 512], op=OP.add), [P1, mb], [modrow])
            P.op("dve", lambda: nc.vector.scalar_tensor_tensor(out=a2row.ap, in0=modrow.ap[0:1, 2048:3072], scalar=1.0, in1=nr.ap, op0=OP.add, op1=OP.mult), [modrow, nr], [a2row])
            g1row = View(modrow, modrow.ap[0:1, 0:1024])
            b2row = View(modrow, modrow.ap[0:1, 1024:2048])
            g2row = View(modrow, modrow.ap[0:1, 3072:4096])
            for (row, dst) in ((g1row, G1b), (a2row, A2b), (b2row, B2b), (g2row, G2b), (fn, FNb)):
                for c0 in range(0, 1024, 512):
                    rap = _ap(row)[0:1, c0:c0 + 512]
                    P.op("pe", lambda: nc.tensor.matmul(P1.ap[:, 0:512], lhsT=ones_row.ap[0:1, 0:128], rhs=rap, start=True, stop=True), [ones_row, row], [P1])
                    P.op("act", lambda: nc.scalar.copy(out=dst.ap[:, c0:c0 + 512], in_=P1.ap[:, 0:512]), [P1], [dst])

        with P.scope():
            P1 = P.ps([128, 1024], F32, "P1b")
            wo = P.sb([128, 8, 1024], BF16, "wo")
            wq = P.sb([128, 8, 2048], F32, "wq")
            sk = P.sb([128, 16, 128], F32, "sk")
            P.dma(wo, View(w_out, w_out.ap.rearrange("(kc p) o -> p kc o", p=128)), q="pool")
            for i in range(4):
                P.dma(View(wq, wq.ap[:, 2 * i:2 * i + 2, :]), View(w_q, w_q.ap.rearrange("(kc p) o -> p kc o", p=128)[:, 2 * i:2 * i + 2, :]), q="act" if i % 2 else "sp")
            P.dma(sk, skT, q="sp")
            xt = P.sb([128, 1024], F32, "xt")
            ot = P.sb([128, 8, 128], BF16, "ot")
            h2Tb = P.sb([128, 8, 128], BF16, "h2Tb")
            x1 = P.sb([128, 1024], F32, "x1")
            h2 = P.sb([128, 1024], F32, "h2")
            h2Tf = P.sb([128, 8, 128], F32, "h2Tf")
            qT = P.sb([128, 16, 128], F32, "qT")
            S3 = P.sb([128, 3, 8, 128], F32, "S3")
            s_sb = P.sb([128, 16, 128], F32, "s_sb")
            s_r = P.sb([128, 16, 128], F32, "s_r")
            v16 = P.sb([128, 16, 16], F32, "v16")
            cand = P.sb([128, 8, 256], F32, "cand")
            c16 = P.sb([128, 8, 16], F32, "c16")
            ec = P.sb([128, 8, 16], F32, "ec")
            st4 = P.sb([128, 4, 8], F32, "st4")
            ss = P.sb([128, 2], F32, "ss")
            oT_v = oT.ap.rearrange("(kc p) t -> p kc t", p=128)
            for ti in range(NT):
              if True:
                tsl = slice(ti * 128, (ti + 1) * 128)
                P.dma(xt, View(x, x.ap[tsl, :]), q="sp")
                P.dma(ot, View(oT, oT_v[:, :, tsl]), q="pool")
                for hf in range(2):
                    for kc in range(8):
                        P.op("pe", lambda: nc.tensor.matmul(P1.ap[:, hf * 512:(hf + 1) * 512], lhsT=ot.ap[:, kc, :], rhs=wo.ap[:, kc, hf * 512:(hf + 1) * 512], start=(kc == 0), stop=(kc == 7)), [ot, wo], [P1])
                P.op("dve", lambda: nc.vector.tensor_tensor(out=x1.ap, in0=P1.ap, in1=G1b.ap, op=OP.mult), [P1, G1b], [x1])
                P.op("pool", lambda: nc.gpsimd.tensor_tensor(out=x1.ap, in0=x1.ap, in1=xt.ap, op=OP.add), [x1, xt], [x1])
                P.dma(View(sc_x1, sc_x1.ap[tsl, :]), x1, q="sp")
                P.op("act", lambda: nc.scalar.activation(out=h2.ap, in_=x1.ap, func=AF.Square, accum_out=ss.ap[:, 0:1]), [x1], [h2, ss])
                P.op("dve", lambda: nc.vector.tensor_scalar(out=ss.ap[:, 1:2], in0=ss.ap[:, 0:1], scalar1=1.0 / 1024, scalar2=EPS, op0=OP.mult, op1=OP.add), [ss], [ss])
                P.op("act", lambda: nc.scalar.activation(out=ss.ap[:, 1:2], in_=ss.ap[:, 1:2], func=AF.Sqrt), [ss], [ss])
                P.op("dve", lambda: nc.vector.reciprocal(out=ss.ap[:, 1:2], in_=ss.ap[:, 1:2]), [ss], [ss])
                P.op("dve", lambda: nc.vector.scalar_tensor_tensor(out=h2.ap, in0=x1.ap, scalar=ss.ap[:, 1:2], in1=A2b.ap, op0=OP.mult, op1=OP.mult), [x1, ss, A2b], [h2])
                P.op("pool", lambda: nc.gpsimd.tensor_tensor(out=h2.ap, in0=h2.ap, in1=B2b.ap, op=OP.add), [h2, B2b], [h2])
                for kc in range(8):
                    P.op("pe", lambda: nc.tensor.transpose(P1.ap[:, kc * 128:(kc + 1) * 128], h2.ap[:, kc * 128:(kc + 1) * 128], ident.ap), [h2, ident], [P1])
                P.op("act", lambda: nc.scalar.copy(out=h2Tf.ap.rearrange("p a b -> p (a b)"), in_=P1.ap), [P1], [h2Tf])
                P.op("dve", lambda: nc.vector.tensor_copy(out=h2Tb.ap, in_=h2Tf.ap), [h2Tf], [h2Tb])
                P.dma(View(sc_h2T, sc_h2T.ap[ti]), h2Tb, q="sp")
                for rnd in range(2):
                    for j in range(8):
                        hp = rnd * 8 + j
                        for kc in range(8):
                            P.op("pe", lambda: nc.tensor.matmul(P1.ap[:, j * 128:(j + 1) * 128], lhsT=wq.ap[:, kc, hp * 128:(hp + 1) * 128], rhs=h2Tf.ap[:, kc, :], start=(kc == 0), stop=(kc == 7)), [wq, h2Tf], [P1])
                    P.op("act", lambda: nc.scalar.copy(out=qT.ap[:, rnd * 8:(rnd + 1) * 8, :].rearrange("p a b -> p (a b)"), in_=P1.ap), [P1], [qT])
                for rnd in range(2):
                    for j in range(8):
                        hp = rnd * 8 + j
                        P.op("pe", lambda: nc.tensor.matmul(P1.ap[:, j * 128:(j + 1) * 128], lhsT=qT.ap[:, hp, :], rhs=sk.ap[:, hp, :], start=True, stop=True), [qT, sk], [P1])
                    P.op("act", lambda: nc.scalar.copy(out=s_sb.ap[:, rnd * 8:(rnd + 1) * 8, :].rearrange("p a b -> p (a b)"), in_=P1.ap), [P1], [s_sb])
                for hp in range(16):
                    P.op("dve", lambda: nc.vector.max(out=v16.ap[:, hp, 0:8], in_=s_sb.ap[:, hp, :]), [s_sb], [v16])
                    P.op("dve", lambda: nc.vector.match_replace(out=s_r.ap[:, hp, :], in_to_replace=v16.ap[:, hp, 0:8], in_values=s_sb.ap[:, hp, :], imm_value=NEG), [s_sb, v16], [s_r])
                    P.op("dve", lambda: nc.vector.max(out=v16.ap[:, hp, 8:16], in_=s_r.ap[:, hp, :]), [s_r], [v16])
                v16v = v16.ap.rearrange("p (h two) k -> p h two k", two=2)
                P.op("dve", lambda: nc.vector.tensor_tensor(out=cand.ap.rearrange("p h (i j) -> p h i j", i=16), in0=v16v[:, :, 0, :].unsqueeze(3).to_broadcast([128, 8, 16, 16]), in1=v16v[:, :, 1, :].unsqueeze(2).to_broadcast([128, 8, 16, 16]), op=OP.add), [v16], [cand])
                for h in range(8):
                    P.op("dve", lambda: nc.vector.max(out=c16.ap[:, h, 0:8], in_=cand.ap[:, h, :]), [cand], [c16])
                    P.op("dve", lambda: nc.vector.match_replace(out=s_r.ap.rearrange("p a b -> p (a b)").rearrange("p (h c) -> p h c", h=8)[:, h, :], in_to_replace=c16.ap[:, h, 0:8], in_values=cand.ap[:, h, :], imm_value=NEG), [cand, c16], [s_r])
                    P.op("dve", lambda: nc.vector.max(out=c16.ap[:, h, 8:16], in_=s_r.ap.rearrange("p a b -> p (a b)").rearrange("p (h c) -> p h c", h=8)[:, h, :]), [s_r], [c16])
                P.op("dve", lambda: nc.vector.tensor_tensor(out=ec.ap, in0=c16.ap, in1=c16.ap[:, :, 0:1].to_broadcast([128, 8, 16]), op=OP.subtract), [c16], [ec])
                P.op("act", lambda: nc.scalar.activation(out=ec.ap, in_=ec.ap, func=AF.Exp), [ec], [ec])
                P.op("dve", lambda: nc.vector.tensor_reduce(out=st4.ap[:, 1, :], in_=ec.ap, axis=AX.X, op=OP.add), [ec], [st4])
                P.op("act", lambda: nc.scalar.activation(out=st4.ap[:, 2, :], in_=st4.ap[:, 1, :], func=AF.Ln), [st4], [st4])
                P.op("dve", lambda: nc.vector.tensor_tensor(out=st4.ap[:, 2, :], in0=st4.ap[:, 2, :], in1=c16.ap[:, :, 0], op=OP.add), [st4, c16], [st4])
                P.op("dve", lambda: nc.vector.tensor_scalar(out=st4.ap[:, 3, :], in0=c16.ap[:, :, 15], scalar1=-1e-3, scalar2=None, op0=OP.add), [c16], [st4])
                s_v = s_sb.ap.rearrange("p (h two) n -> p h two n", two=2)
                P.op("dve", lambda: nc.vector.tensor_tensor(out=S3.ap[:, 0], in0=s_v[:, :, 0, :], in1=st4.ap[:, 2, :].unsqueeze(2).to_broadcast([128, 8, 128]), op=OP.subtract), [s_sb, st4], [S3])
                P.op("dve", lambda: nc.vector.tensor_tensor(out=st4.ap[:, 0, :], in0=st4.ap[:, 3, :], in1=st4.ap[:, 2, :], op=OP.subtract), [st4], [st4])
                P.op("dve", lambda: nc.vector.memset(S3.ap[:, 1], 0.0), [], [S3])
                P.op("act", lambda: nc.scalar.activation(out=S3.ap[:, 1, :, 0], in_=st4.ap[:, 0, :], func=AF.Exp), [st4], [S3])
                P.op("act", lambda: nc.scalar.activation(out=S3.ap[:, 2], in_=s_v[:, :, 1, :], func=AF.Exp), [s_sb], [S3])
                P.op("act", lambda: nc.scalar.activation(out=S3.ap[:, 0], in_=S3.ap[:, 0], func=AF.Exp), [S3], [S3])
                P.dma(View(sc_s, sc_s.ap[ti]), S3, q="act")

        with P.scope():
            uc = [P.sb([128, 8, 512], BF16, f"uc{i}") for i in range(2)]
            vc = [P.sb([128, 4, 1024], BF16, f"vc{i}") for i in range(3)]
            S3s = [P.sb([128, 3, 8, 128], F32, f"S3b{i}") for i in range(4)]
            x1s = [P.sb([128, 1024], F32, f"x1b{i}") for i in range(4)]
            h2Ts = [P.sb([128, 8, 128], BF16, f"h2Tt{i}") for i in range(4)]
            sumE = [P.sb([128, 8, 4, 128], F32, f"sumE{i}") for i in range(3)]
            Gh = [P.sb([128, 8, 512], BF16, f"Gh{i}") for i in range(2)]
            actT = [P.sb([128, 512], BF16, f"actT{i}") for i in range(3)]
            gaT = [P.sb([128, 4, 128], BF16, f"gaT{i}") for i in range(2)]
            xo_t = P.sb([128, 1024], F32, "xo_t")
            ss = P.sb([128, 2], F32, "ss2")
            ACCs = [P.ps([128, 1024], F32, f"ACC{i}") for i in range(2)]
            PA = [P.ps([128, 512], F32, f"PA{i}") for i in range(2)]
            GTs = [P.ps([128, 512], F32, "GT0")] * 2
            PT = P.ps([128, 512], BF16, "PT")
            ga = P.sb([128, 512], BF16, "ga")
            uT_v = uT_bf.ap.rearrange("(kc p) e -> p kc e", p=128)
            vv_v = vv_bf.ap.rearrange("(b p) d -> p b d", p=128)
            assert NT % 2 == 0
            items = []
            for tp in range(NT // 2):
                for ci in range(NCH):
                    items.append((2 * tp, ci, 0))
                    items.append((2 * tp + 1, ci, 1))

            def load_tile(ti):
                P.dma(S3s[ti % 4], View(sc_s, sc_s.ap[ti]), q="sp")
                P.dma(x1s[ti % 4], View(sc_x1, sc_x1.ap[ti * 128:(ti + 1) * 128, :]), q="sp")
                P.dma(h2Ts[ti % 4], View(sc_h2T, sc_h2T.ap[ti]), q="sp")

            def stage1(idx):
                ti, ci, sub = items[idx]
                b = idx % 2
                b3 = idx % 3
                k = idx // 2
                S3 = S3s[ti % 4]
                h2T = h2Ts[ti % 4]
                if ci == 2 and sub == 0 and ti + 2 < NT:
                    load_tile(ti + 2)
                    load_tile(ti + 3)
                if sub == 0:
                    P.dma(uc[k % 2], View(uT_bf, uT_v[:, :, ci * 512:(ci + 1) * 512]), q="sp")
                    P.dma(vc[k % 3], View(vv_bf, vv_v[:, ci * 4:(ci + 1) * 4, :]), q="act")
                for kc in range(8):
                    P.op("pe", lambda: nc.tensor.matmul(PA[b].ap, lhsT=h2T.ap[:, kc, :], rhs=uc[k % 2].ap[:, kc, :], start=(kc == 0), stop=(kc == 7)), [h2T, uc[k % 2]], [PA[b]])
                P.op("act", lambda: nc.scalar.activation(out=actT[b3].ap, in_=PA[b].ap, func=AF.Gelu), [PA[b]], [actT[b3]])
                s1e_b = S3.ap[:, 0, :, ci * 4:(ci + 1) * 4].unsqueeze(3).to_broadcast([128, 8, 4, 128])
                s2_b = S3.ap[:, 2].unsqueeze(2).to_broadcast([128, 8, 4, 128])
                P.op("pool", lambda: nc.gpsimd.tensor_tensor(out=sumE[b3].ap, in0=s1e_b, in1=s2_b, op=OP.mult), [S3], [sumE[b3]])

            def stage2a(idx):
                ti, ci, sub = items[idx]
                b = idx % 2
                S3 = S3s[ti % 4]
                GT = GTs[b]
                Ev = sumE[idx % 3].ap.rearrange("p h a n -> p h (a n)")
                for h in range(8):
                    P.op("dve", lambda: nc.vector.scalar_tensor_tensor(out=Gh[b].ap[:, h, :], in0=Ev[:, h, :], scalar=S3.ap[:, 1, h, 0:1], in1=Ev[:, h, :], op0=OP.is_ge, op1=OP.mult), [sumE[idx % 3], S3], [Gh[b]], indep=True)
                for h in range(8):
                    P.op("pe", lambda: nc.tensor.matmul(GT.ap, lhsT=identb.ap, rhs=Gh[b].ap[:, h, :], start=(h == 0), stop=(h == 7)), [Gh[b], identb], [GT])

            def stage2b(idx):
                ti, ci, sub = items[idx]
                b = idx % 2
                b3 = idx % 3
                k = idx // 2
                GT = GTs[b]
                ACC = ACCs[sub]
                P.op("dve", lambda: nc.vector.tensor_tensor(out=ga.ap, in0=GT.ap, in1=actT[b3].ap, op=OP.mult), [GT, actT[b3]], [ga])
                for bb in range(4):
                    P.op("pe", lambda: nc.tensor.transpose(PT.ap[:, bb * 128:(bb + 1) * 128], ga.ap[:, bb * 128:(bb + 1) * 128], identb.ap), [ga, identb], [PT])
                P.op("dve", lambda: nc.vector.tensor_copy(out=gaT[b].ap.rearrange("p a b -> p (a b)"), in_=PT.ap), [PT], [gaT[b]])
                for bb in range(4):
                    for hf in range(2):
                        P.op("pe", lambda: nc.tensor.matmul(ACC.ap[:, hf * 512:(hf + 1) * 512], lhsT=gaT[b].ap[:, bb, :], rhs=vc[k % 3].ap[:, bb, hf * 512:(hf + 1) * 512], start=(ci == 0 and bb == 0), stop=(ci == NCH - 1 and bb == 3)), [gaT[b], vc[k % 3]], [ACC])

            def epilogue(ti, sub, idx):
                x1 = x1s[ti % 4]
                jb = sumE[idx % 3]
                ACC = ACCs[sub]
                P.op("dve", lambda: nc.vector.tensor_tensor(out=xo_t.ap, in0=ACC.ap, in1=G2b.ap, op=OP.mult), [ACC, G2b], [xo_t])
                P.op("pool", lambda: nc.gpsimd.tensor_tensor(out=xo_t.ap, in0=xo_t.ap, in1=x1.ap, op=OP.add), [xo_t, x1], [xo_t])
                if final:
                    P.op("act", lambda: nc.scalar.activation(out=jb.ap.rearrange("p h a n -> p (h a n)")[:, 0:1024], in_=xo_t.ap, func=AF.Square, accum_out=ss.ap[:, 0:1]), [xo_t], [jb, ss])
                    P.op("dve", lambda: nc.vector.tensor_scalar(out=ss.ap[:, 1:2], in0=ss.ap[:, 0:1], scalar1=1.0 / 1024, scalar2=EPS, op0=OP.mult, op1=OP.add), [ss], [ss])
                    P.op("act", lambda: nc.scalar.activation(out=ss.ap[:, 1:2], in_=ss.ap[:, 1:2], func=AF.Sqrt), [ss], [ss])
                    P.op("dve", lambda: nc.vector.reciprocal(out=ss.ap[:, 1:2], in_=ss.ap[:, 1:2]), [ss], [ss])
                    P.op("dve", lambda: nc.vector.scalar_tensor_tensor(out=xo_t.ap, in0=xo_t.ap, scalar=ss.ap[:, 1:2], in1=FNb.ap, op0=OP.mult, op1=OP.mult), [xo_t, ss, FNb], [xo_t])
                P.dma(View(xo, xo.ap[ti * 128:(ti + 1) * 128, :]), xo_t, q="sp")

            load_tile(0)
            load_tile(1)
            NI = len(items)
            stage1(0)
            stage1(1)
            stage2a(0)
            for idx in range(NI):
                if idx + 2 < NI:
                    stage1(idx + 2)
                stage2b(idx)
                if items[idx][1] == NCH - 1:
                    epilogue(items[idx][0], items[idx][2], idx)
                if idx + 1 < NI:
                    stage2a(idx + 1)
        P.barrier()
        P.finish([xo])
        print("phase C: ninst", P.ninst, "nwait", P.nwait)
    return nc


SEQ = 16384
NCORE = 8
TOK = SEQ // NCORE
_PROGS = {}
f32 = np.float32


def build_phase_b_even(S):
    nc = bass.Bass("TRN2", target_bir_lowering=False)
    NBLK = S // 256
    with ExitStack() as st:
        P = Prog(nc, st)
        D = {}
        for nm in ("xq", "xk", "xv"):
            D[nm] = P.dram(nm, [128, S + 3], F32, kind="ExternalInput")
        D["cw"] = P.dram("cw", [128, 12], F32, kind="ExternalInput")
        D["z"] = P.dram("z", [S, 128], F32, kind="ExternalInput")
        D["ba"] = P.dram("ba", [128, S // 128, 2], F32, kind="ExternalInput")
        D["hc"] = P.dram("hc", [128, 2], F32, kind="ExternalInput")
        D["onorm"] = P.dram("onorm", [128, 128], F32, kind="ExternalInput")
        for nm in ("ident", "tri", "maskS", "maskI"):
            D[nm] = P.dram(nm, [128, 128], F32, kind="ExternalInput")
        D["OA"] = P.dram("OA", [S, 128], F32, kind="ExternalOutput")
        for nm in ("qT", "qTs", "kT", "kTs"):
            D[nm] = P.dram(nm, [128, S], F32, kind="ExternalInput")
        D["v"] = P.dram("v", [S, 128], F32, kind="ExternalInput")
        D["pos"] = P.dram("pos", [1, S], I32, kind="ExternalInput")
        D["cst"] = P.dram("cst", [128, 2], F32, kind="ExternalInput")
        D["maskT"] = P.dram("maskT", [128, 128], F32, kind="ExternalInput")
        D["selT"] = P.dram("selT", [NBLK, NBLK * 128], F32, kind="ExternalInput")
        D["OT"] = P.dram("OT", [128, S], F32, kind="ExternalOutput")
        with P.scope():
            gdn_body(P, nc, S, D)
        with P.scope():
            moba_body(P, nc, S, D)
        P.barrier()
        P.finish([D["OA"], D["OT"]])
    return nc


def _prog(key):
    if key not in _PROGS:
        if key == "A_even":
            _PROGS[key] = build_phase_a(TOK, 3592)
        elif key == "A_odd":
            _PROGS[key] = build_phase_a(TOK, 832)
        elif key == "B_even":
            _PROGS[key] = build_phase_b_even(SEQ)
        elif key == "B_odd":
            _PROGS[key] = build_phase_b_mla(SEQ)
        elif key == "C":
            _PROGS[key] = build_phase_c(TOK, final=False)
        elif key == "C_final":
            _PROGS[key] = build_phase_c(TOK, final=True)
    return _PROGS[key]


def _run(key, in_maps):
    res = run_bass_kernel_spmd(_prog(key), in_maps, core_ids=list(range(NCORE)))
    return res.results


def _c(a):
    return np.ascontiguousarray(a)


def kernel(x, c, positions, mod_w, mod_b, norm_mix, norm_ffn, hy_w_in, gdn_conv, gdn_a_log,
           gdn_dt_bias, gdn_o_norm, hy_w_out, mla_w_in, mla_q_norm, mla_kv_norm, mla_w_uq,
           mla_w_ukv, mla_w_out, peer_w_q, peer_sub_keys, peer_u, peer_v, final_norm):
    A_ = lambda a: np.asarray(a)
    x, c, positions, mod_w, mod_b = A_(x), A_(c), A_(positions), A_(mod_w), A_(mod_b)
    norm_mix, norm_ffn, hy_w_in, gdn_conv = A_(norm_mix), A_(norm_ffn), A_(hy_w_in), A_(gdn_conv)
    gdn_a_log, gdn_dt_bias, gdn_o_norm, hy_w_out = A_(gdn_a_log), A_(gdn_dt_bias), A_(gdn_o_norm), A_(hy_w_out)
    mla_w_in, mla_q_norm, mla_kv_norm, mla_w_uq = A_(mla_w_in), A_(mla_q_norm), A_(mla_kv_norm), A_(mla_w_uq)
    mla_w_ukv, mla_w_out, peer_w_q, peer_sub_keys = A_(mla_w_ukv), A_(mla_w_out), A_(peer_w_q), A_(peer_sub_keys)
    peer_u, peer_v, final_norm = A_(peer_u), A_(peer_v), A_(final_norm)
    S = SEQ
    xcur = _c(x[0].astype(f32, copy=False))
    cT = _c(c.reshape(8, 128).T)
    pos = _c(positions.reshape(1, S).astype(np.int32, copy=False))
    I = np.eye(128, dtype=f32)
    tri = np.triu(np.ones((128, 128), f32))
    maskS = np.tril(np.ones((128, 128), f32), -1)
    maskI = np.tril(np.ones((128, 128), f32))
    maskT = np.triu(np.ones((128, 128), f32))
    NBLK = S // 256
    selT = np.kron(np.eye(NBLK, dtype=f32), np.ones((1, 128), f32))
    invf_b = (10000.0 ** (-np.arange(0, 128, 2, dtype=np.float32) / 128)).astype(f32)
    cst_b = np.zeros((128, 2), f32); cst_b[:, 0] = np.concatenate([invf_b, invf_b]); cst_b[:64, 1] = -1; cst_b[64:, 1] = 1
    invf_c = (10000.0 ** (-np.arange(0, 64, 2, dtype=np.float32) / 64)).astype(f32)
    cst_c = np.zeros((128, 2), f32); cst_c[:64, 0] = np.concatenate([invf_c, invf_c]); cst_c[:32, 1] = -1; cst_c[32:64, 1] = 1
    sw = lambda a: _c(np.concatenate([a[a.shape[0] // 2:], a[:a.shape[0] // 2]], 0))

    for l in range(4):
        i = l // 2
        even = (l % 2 == 0)
        W = hy_w_in[i] if even else mla_w_in[i]
        modw_a = _c(mod_w[l][:, 0:2048]); modb_a = _c(mod_b[l][None, 0:2048]); nrm_a = _c(norm_mix[l][None])
        in_maps = [dict(x=xcur[k * TOK:(k + 1) * TOK], cT=cT, modw=modw_a, modb=modb_a, nrm=nrm_a, W=_c(W), ident=I) for k in range(NCORE)]
        res = _run("A_even" if even else "A_odd", in_maps)
        Y = np.concatenate([r["Y"] for r in res], 0)
        if even:
            in_maps = []
            for k in range(NCORE):
                h = k % 4
                def xT(off):
                    a = Y[:, off + h * 128: off + (h + 1) * 128].T
                    return _c(np.concatenate([np.zeros((128, 3), f32), a], 1))
                cw = _c(np.concatenate([gdn_conv[i][:, off + h * 128: off + (h + 1) * 128].T for off in (0, 512, 1024)], 1))
                ba = _c(np.stack([Y[:, 2048 + h], Y[:, 2052 + h]], -1).reshape(S // 128, 128, 2).transpose(1, 0, 2))
                hc = _c(np.tile(np.array([[gdn_a_log[i][h], gdn_dt_bias[i][h]]], f32), (128, 1)))
                qT = _c(Y[:, 2056 + h * 128: 2056 + (h + 1) * 128].T)
                kT = _c(Y[:, 2568 + h * 128: 2568 + (h + 1) * 128].T)
                in_maps.append(dict(xq=xT(0), xk=xT(512), xv=xT(1024), cw=cw, z=_c(Y[:, 1536 + h * 128: 1536 + (h + 1) * 128]),
                                    ba=ba, hc=hc, onorm=_c(np.tile(gdn_o_norm[i][None], (128, 1))), ident=I, tri=tri, maskS=maskS, maskI=maskI,
                                    qT=qT, qTs=sw(qT), kT=kT, kTs=sw(kT), v=_c(Y[:, 3080 + h * 128: 3080 + (h + 1) * 128]),
                                    pos=pos, cst=cst_b, maskT=maskT, selT=selT))
            res = _run("B_even", in_maps)
            oT = np.concatenate([res[h]["OA"].T for h in range(4)] + [res[h]["OT"] for h in range(4)], 0)
            w_out = hy_w_out[i]
        else:
            YT = _c(Y.T)
            qn = _c(mla_q_norm[i].reshape(4, 128).T); kvn = _c(mla_kv_norm[i].reshape(2, 128).T)
            krT = _c(YT[768:832]); krTs = sw(krT)
            in_maps = []
            for h in range(NCORE):
                wq = mla_w_uq[i][:, h * 192:(h + 1) * 192]; wkv = mla_w_ukv[i][:, h * 256:(h + 1) * 256]
                wr = wq[:, 128:]
                in_maps.append(dict(cqT=YT[:512], ckvT=YT[512:768], krT=krT, krTs=krTs, pos=pos, qn=qn, kvn=kvn,
                                    wuq_n=_c(wq[:, :128]), wuq_r=_c(wr), wuq_rs=_c(np.concatenate([wr[:, 32:], wr[:, :32]], 1)),
                                    wukv_k=_c(wkv[:, :128]), wukv_v=_c(wkv[:, 128:]), cst=cst_c, maskT=maskT))
            res = _run("B_odd", in_maps)
            oT = np.concatenate([res[h]["OT"] for h in range(NCORE)], 0)
            w_out = mla_w_out[i]
        modw_c = _c(mod_w[l][:, 2048:6144]); modb_c = _c(mod_b[l][None, 2048:6144])
        skT = _c(peer_sub_keys[l].reshape(16, 128, 128).transpose(2, 0, 1))
        uT = _c(peer_u[l].T)
        vv = _c(peer_v[l])
        in_maps = [dict(x=xcur[k * TOK:(k + 1) * TOK], oT=_c(oT[:, k * TOK:(k + 1) * TOK]), w_out=_c(w_out), cT=cT, modw=modw_c, modb=modb_c,
                        nrm=_c(norm_ffn[l][None]), fnrm=_c(final_norm[None]), w_q=_c(peer_w_q[l]), skT=skT, uT=uT, vv=vv, ident=I) for k in range(NCORE)]
        res = _run("C_final" if l == 3 else "C", in_maps)
        xcur = np.concatenate([r["xo"] for r in res], 0)
    return xcur.reshape(1, S, 1024).astype(f32, copy=False)
```
